# Optimizing a Trainium2 kernel written in Bass

```python
import math
import jax, jax.numpy as jnp
from jax import lax
import numpy as np


D_MODEL = 2048
BATCH = 4
SEQ = 4096
DEPTH = 2

A_HEADS = 8
A_HEAD_DIM = 128
A_KV_RANK = 256
IDX_HEADS = 16
IDX_HEAD_DIM = 64
IDX_ROPE_DIM = 32
ROPE_THETA = 10000.0
DSA_TOPK = 256
Q_BLOCK = 128
B_GROUPS = ((128, 1), (512, 4), (2048, 16))
B_HEADS_PER_GROUP = 4
B_HEAD_DIM = 128
B_HEADS = B_HEADS_PER_GROUP * len(B_GROUPS)
BAND_BLOCK = 128
REL_BUCKETS = 32
REL_MAX_DISTANCE = 2048
N_BIAS_HEADS = A_HEADS + B_HEADS
D_FF = -(-8 * D_MODEL // (3 * 256)) * 256
NORM_EPS = 1e-6
NEG_INF = -1e30

A_Q_COLS = A_HEADS * A_HEAD_DIM
A_KV_COLS = A_KV_RANK
IDX_Q_COLS = IDX_HEADS * IDX_HEAD_DIM
IDX_K_COLS = IDX_HEAD_DIM
IDX_W_COLS = IDX_HEADS
B_QKV_COLS = 3 * B_HEADS * B_HEAD_DIM
GATE_COLS = 2 * D_MODEL
IN_SIZES = (A_Q_COLS, A_KV_COLS, IDX_Q_COLS, IDX_K_COLS, IDX_W_COLS, B_QKV_COLS, GATE_COLS)
N_IN = sum(IN_SIZES)
A_OUT = A_HEADS * A_HEAD_DIM
B_OUT = B_HEADS_PER_GROUP * B_HEAD_DIM

kernel_name = 'hybrid_dsa_dilated_gated_block'


def rms_norm(x, g):
    xf = x.astype(jnp.float32)
    y = xf * lax.rsqrt(jnp.mean(xf * xf, axis=-1, keepdims=True) + NORM_EPS)
    return y.astype(x.dtype) * g


def layer_norm(x, g, b):
    xf = x.astype(jnp.float32)
    mu = jnp.mean(xf, axis=-1, keepdims=True)
    var = jnp.mean(jnp.square(xf - mu), axis=-1, keepdims=True)
    return ((xf - mu) * lax.rsqrt(var + NORM_EPS)).astype(x.dtype) * g + b


def rel_bucket(dist):
    n = jnp.maximum(dist, 0)
    exact = REL_BUCKETS // 2
    nf = jnp.maximum(n, 1).astype(jnp.float32)
    large = exact + (jnp.log(nf / exact) / math.log(REL_MAX_DISTANCE / exact)
                     * (REL_BUCKETS - exact)).astype(jnp.int32)
    return jnp.where(n < exact, n, jnp.minimum(large, REL_BUCKETS - 1))


def rope_partial(x, pos):
    half = IDX_ROPE_DIM // 2
    freqs = ROPE_THETA ** (-jnp.arange(half, dtype=jnp.float32) / half)
    ang = pos.astype(jnp.float32)[:, None] * freqs[None, :]
    cos = jnp.cos(ang)[None, :, None, :]
    sin = jnp.sin(ang)[None, :, None, :]
    xr = x[..., :IDX_ROPE_DIM].astype(jnp.float32)
    x1, x2 = xr[..., :half], xr[..., half:]
    rot = jnp.concatenate([x1 * cos - x2 * sin, x1 * sin + x2 * cos], axis=-1).astype(x.dtype)
    return jnp.concatenate([rot, x[..., IDX_ROPE_DIM:]], axis=-1)


def dsa_attention(q, c_kv, q_idx, k_idx, w_idx, w_uk, w_uv, bias_a):
    B, L = q.shape[0], q.shape[1]
    topk = min(DSA_TOPK, L // 4)
    nq = L // Q_BLOCK
    q_lat = jnp.einsum('blhd,hdc->blhc', q, w_uk) * (A_HEAD_DIM ** -0.5)
    key_pos = jnp.arange(L)

    def to_blocks(a):
        return jnp.swapaxes(a.reshape(B, nq, Q_BLOCK, *a.shape[2:]), 0, 1)

    def block(args):
        ql, qi, wi, t0 = args
        qpos = t0 + jnp.arange(Q_BLOCK)
        logits = jnp.einsum('bphd,bsd->bphs', qi, k_idx)
        score = jnp.einsum('bph,bphs->bps', wi, jax.nn.relu(logits)).astype(jnp.float32)
        causal = key_pos[None, :] <= qpos[:, None]
        score = jnp.where(causal[None], score, -jnp.inf)
        _, idx = lax.top_k(score, topk)
        kv_sel = jax.vmap(lambda c, i: c[i])(c_kv, idx)
        s = jnp.einsum('bphc,bpkc->bphk', ql, kv_sel).astype(jnp.float32)
        dist = qpos[None, :, None] - idx
        s = s + jnp.moveaxis(bias_a[rel_bucket(dist)], -1, 2).astype(jnp.float32)
        s = jnp.where((dist >= 0)[:, :, None, :], s, NEG_INF)
        p = jax.nn.softmax(s, axis=-1).astype(kv_sel.dtype)
        return jnp.einsum('bphk,bpkc->bphc', p, kv_sel)

    t0s = jnp.arange(nq, dtype=jnp.int32) * Q_BLOCK
    o_lat = lax.map(block, (to_blocks(q_lat), to_blocks(q_idx), to_blocks(w_idx), t0s))
    o_lat = jnp.swapaxes(o_lat, 0, 1).reshape(B, L, A_HEADS, A_KV_RANK)
    return jnp.einsum('blhc,hcd->blhd', o_lat, w_uv).reshape(B, L, A_OUT)


def dilated_group(q, k, v, bias_g, dilation, window):
    B, L, H, dh = q.shape
    steps = window // dilation
    P = BAND_BLOCK
    M = L // dilation
    nb = -(-M // P)
    Mp = nb * P

    def strided(a):
        return a.reshape(B, M, dilation, H, dh).transpose(0, 2, 1, 3, 4)

    qs = jnp.pad(strided(q), ((0, 0), (0, 0), (0, Mp - M), (0, 0), (0, 0))).reshape(B, dilation, nb, P, H, dh)

    def key_blocks(a):
        a = jnp.pad(strided(a), ((0, 0), (0, 0), (P, Mp - M), (0, 0), (0, 0))).reshape(B, dilation, nb + 1, P, H, dh)
        return jnp.concatenate([a[:, :, :-1], a[:, :, 1:]], axis=3)

    ks, vs = key_blocks(k), key_blocks(v)
    i = jnp.arange(P)[:, None]
    u = jnp.arange(2 * P)[None, :]
    back = i + P - u
    m_key = (jnp.arange(nb) * P)[:, None, None] - P + u[None]
    valid = (back >= 0)[None] & (back <= steps)[None] & (m_key >= 0)
    bias = jnp.transpose(bias_g[rel_bucket(back * dilation)], (2, 0, 1)).astype(jnp.float32)
    s = jnp.einsum('bdnihe,bdnuhe->bdnhiu', qs, ks).astype(jnp.float32) * (dh ** -0.5) + bias
    s = jnp.where(valid[None, None, :, None], s, NEG_INF)
    mx = jnp.max(s, axis=-1, keepdims=True)
    p = jnp.exp(s - mx)
    den = jnp.sum(p, axis=-1, keepdims=True)
    o = jnp.einsum('bdnhiu,bdnuhe->bdnihe', (p / den).astype(v.dtype), vs)
    lse = (mx + jnp.log(den))[..., 0]
    o = o.reshape(B, dilation, Mp, H, dh)[:, :, :M].transpose(0, 2, 1, 3, 4).reshape(B, L, H, dh)
    lse = lse.transpose(0, 1, 2, 4, 3).reshape(B, dilation, Mp, H)[:, :, :M].transpose(0, 2, 1, 3).reshape(B, L, H)
    return o, lse


def dilated_mixture(qkv_b, bias_b):
    B, L = qkv_b.shape[0], qkv_b.shape[1]
    outs, lses = [], []
    for g, (window, dil) in enumerate(B_GROUPS):
        o, lse = dilated_group(qkv_b[:, :, 0, g], qkv_b[:, :, 1, g], qkv_b[:, :, 2, g],
                               bias_b[:, g * B_HEADS_PER_GROUP:(g + 1) * B_HEADS_PER_GROUP], dil, window)
        outs.append(o)
        lses.append(lse)
    alpha = jax.nn.softmax(jnp.stack(lses), axis=0)
    out = jnp.sum(alpha[..., None] * jnp.stack(outs).astype(jnp.float32), axis=0)
    return out.astype(qkv_b.dtype).reshape(B, L, B_OUT)


def hybrid_layer(x, c, rel_bias, w_ada, b_ada, norm1_g, w_in, kv_norm_g, idx_ln_g, idx_ln_b,
                 w_uk, w_uv, w_a_up, w_b_up, w_out, norm2_g, w_ff_gate, w_ff_up, w_ff_down):
    B, L, _ = x.shape
    mod = jax.nn.silu(c) @ w_ada + b_ada
    shift1, scale1, gate1, shift2, scale2, gate2 = [m[:, None, :] for m in jnp.split(mod, 6, axis=-1)]

    h = rms_norm(x, norm1_g) * (1 + scale1) + shift1
    z = h @ w_in
    split_pts = [int(p) for p in np.cumsum(IN_SIZES)[:-1]]
    q_a, ckv, q_i, k_i, w_i, qkv_b, gates = jnp.split(z, split_pts, axis=-1)
    pos = jnp.arange(L)
    q_a = q_a.reshape(B, L, A_HEADS, A_HEAD_DIM)
    ckv = rms_norm(ckv, kv_norm_g)
    q_i = rope_partial(q_i.reshape(B, L, IDX_HEADS, IDX_HEAD_DIM), pos)
    k_i = rope_partial(layer_norm(k_i, idx_ln_g, idx_ln_b)[:, :, None, :], pos)[:, :, 0, :]
    w_i = w_i * (IDX_HEADS ** -0.5 * IDX_HEAD_DIM ** -0.5)
    y_a = dsa_attention(q_a, ckv, q_i, k_i, w_i, w_uk, w_uv, rel_bias[:, :A_HEADS])
    y_b = dilated_mixture(qkv_b.reshape(B, L, 3, len(B_GROUPS), B_HEADS_PER_GROUP, B_HEAD_DIM),
                          rel_bias[:, A_HEADS:])
    g_a, g_b = jnp.split(jax.nn.sigmoid(gates), 2, axis=-1)
    merged = g_a * (y_a @ w_a_up) + g_b * (y_b @ w_b_up)
    x = x + gate1 * (merged @ w_out)

    h2 = rms_norm(x, norm2_g) * (1 + scale2) + shift2
    ff = (jax.nn.silu(h2 @ w_ff_gate) * (h2 @ w_ff_up)) @ w_ff_down
    return x + gate2 * ff


def setup_inputs(seed: int = 0) -> dict:
    key = jax.random.key(seed)
    ks = jax.random.split(key, 22)

    def nrm(k, shape, scale):
        return jax.random.normal(k, shape, jnp.float32) * scale

    return {
        'x': nrm(ks[0], (BATCH, SEQ, D_MODEL), 1.0),
        'c': nrm(ks[1], (BATCH, D_MODEL), 1.0),
        'rel_bias': nrm(ks[2], (REL_BUCKETS, N_BIAS_HEADS), 0.5),
        'w_ada': nrm(ks[3], (DEPTH, D_MODEL, 6 * D_MODEL), 0.5 * D_MODEL ** -0.5),
        'b_ada': nrm(ks[4], (DEPTH, 6 * D_MODEL), 0.02),
        'norm1_g': 1.0 + nrm(ks[5], (DEPTH, D_MODEL), 0.02),
        'w_in': nrm(ks[6], (DEPTH, D_MODEL, N_IN), D_MODEL ** -0.5),
        'kv_norm_g': 1.0 + nrm(ks[7], (DEPTH, A_KV_RANK), 0.02),
        'idx_ln_g': 1.0 + nrm(ks[8], (DEPTH, IDX_HEAD_DIM), 0.02),
        'idx_ln_b': nrm(ks[9], (DEPTH, IDX_HEAD_DIM), 0.02),
        'w_uk': nrm(ks[10], (DEPTH, A_HEADS, A_HEAD_DIM, A_KV_RANK), A_HEAD_DIM ** -0.5),
        'w_uv': nrm(ks[11], (DEPTH, A_HEADS, A_KV_RANK, A_HEAD_DIM), A_KV_RANK ** -0.5),
        'w_a_up': nrm(ks[12], (DEPTH, A_OUT, D_MODEL), A_OUT ** -0.5),
        'w_b_up': nrm(ks[13], (DEPTH, B_OUT, D_MODEL), B_OUT ** -0.5),
        'w_out': nrm(ks[14], (DEPTH, D_MODEL, D_MODEL), D_MODEL ** -0.5),
        'norm2_g': 1.0 + nrm(ks[15], (DEPTH, D_MODEL), 0.02),
        'w_ff_gate': nrm(ks[16], (DEPTH, D_MODEL, D_FF), D_MODEL ** -0.5),
        'w_ff_up': nrm(ks[17], (DEPTH, D_MODEL, D_FF), D_MODEL ** -0.5),
        'w_ff_down': nrm(ks[18], (DEPTH, D_FF, D_MODEL), D_FF ** -0.5),
        'final_g': 1.0 + nrm(ks[19], (D_MODEL,), 0.02),
    }


def reference(x, c, rel_bias, w_ada, b_ada, norm1_g, w_in, kv_norm_g, idx_ln_g, idx_ln_b,
              w_uk, w_uv, w_a_up, w_b_up, w_out, norm2_g, w_ff_gate, w_ff_up, w_ff_down, final_g):
    for l in range(DEPTH):
        x = hybrid_layer(x, c, rel_bias, w_ada[l], b_ada[l], norm1_g[l], w_in[l], kv_norm_g[l],
                         idx_ln_g[l], idx_ln_b[l], w_uk[l], w_uv[l], w_a_up[l], w_b_up[l], w_out[l],
                         norm2_g[l], w_ff_gate[l], w_ff_up[l], w_ff_down[l])
    return rms_norm(x, final_g)
```

```python
import math
from contextlib import ExitStack
import numpy as np
import concourse.bass as bass
import concourse.mybir as mybir
from concourse.bass_utils import run_bass_kernel_spmd

F32 = mybir.dt.float32
BF16 = mybir.dt.bfloat16
ALU = mybir.AluOpType
AF = mybir.ActivationFunctionType

D = 2048
KC = 16
NIN = 11088
DFF = 5632
FC = 44
NEG = -30000.0
EPS = 1e-6
C_QA, C_CKV, C_QI, C_KI, C_WI, C_QB, C_KB, C_VB, C_GATE = 0, 1024, 1280, 2304, 2368, 2384, 3920, 5456, 6992
B_GROUPS = ((128, 1), (512, 4), (2048, 16))
NBLK_G = (2, 5, 17)
OFF_G = (0, 2, 7)
NRING = (3, 6, 18)


class KB:
    def __init__(self, nc):
        self.nc = nc
        self.eng = {"pe": nc.tensor, "dve": nc.vector, "act": nc.scalar, "pool": nc.gpsimd, "sp": nc.sync}
        self.esem = {e: nc.alloc_semaphore("prog_" + e) for e in ("pe", "dve", "act", "pool")}
        self.cnt = {e: 0 for e in self.esem}
        self.dsem, self.dcnt = {}, {}
        self.waited, self.lastw, self.readers = {}, {}, {}
        self.ninst = 0

    def _sem(self, key):
        return self.esem[key[1]] if key[0] == "e" else self.dsem[key[1]]

    def _wait(self, e, tok):
        if tok is None:
            return
        key, val = tok
        if key == ("e", "pe") and e == "pe":
            return
        if key[0] == "d":
            val = self.dcnt[key[1]]
        k = (e, key)
        if self.waited.get(k, 0) >= val:
            return
        self.waited[k] = val
        self.eng[e].wait_ge(self._sem(key), val)
        self.ninst += 1

    def _deps(self, e, reads, writes):
        for r in reads:
            self._wait(e, self.lastw.get(r))
        for w in writes:
            self._wait(e, self.lastw.get(w))
            for t in self.readers.get(w, ()):
                self._wait(e, t)

    def _commit(self, tok, reads, writes):
        for r in reads:
            lst = self.readers.setdefault(r, [])
            lst.append(tok)
            if len(lst) > 12:
                best = {}
                for k, v in lst:
                    best[k] = max(best.get(k, 0), v)
                self.readers[r] = list(best.items())
        for w in writes:
            self.lastw[w] = tok
            self.readers[w] = []

    def op(self, e, fn, reads=(), writes=()):
        self._deps(e, reads, writes)
        ins = fn(self.eng[e])
        self.cnt[e] += 1
        ins.then_inc(self.esem[e], 1)
        tok = (("e", e), self.cnt[e])
        self._commit(tok, reads, writes)
        self.ninst += 1
        return tok

    def mm(self, out, lhsT, rhs, start, stop, reads=(), writes=(), last=True):
        e = "pe"
        self._deps(e, reads, writes)
        ins = self.nc.tensor.matmul(out, lhsT, rhs, start=start, stop=stop)
        self.ninst += 1
        if last:
            self.cnt[e] += 1
            ins.then_inc(self.esem[e], 1)
            tok = (("e", e), self.cnt[e])
        else:
            tok = (("e", e), self.cnt[e] + 1)
        self._commit(tok, reads, writes)
        return tok

    def dma(self, q, out, in_, slot, reads=(), writes=()):
        if slot not in self.dsem:
            self.dsem[slot] = self.nc.alloc_semaphore("d_" + slot)
            self.dcnt[slot] = 0
        self._deps(q, reads, writes)
        ins = self.eng[q].dma_start(out=out, in_=in_)
        self.dcnt[slot] += 16
        ins.then_inc(self.dsem[slot], 16)
        tok = (("d", slot), self.dcnt[slot])
        self._commit(tok, reads, writes)
        self.ninst += 1
        return tok

    def cc(self, fn, name, reads=(), writes=()):
        self.dsem[name] = self.nc.alloc_semaphore("cc_" + name)
        self._deps("pool", reads, writes)
        ins = fn(self.eng["pool"])
        ins.then_inc(self.dsem[name], 1)
        self.dcnt[name] = 1
        tok = (("d", name), 1)
        self._commit(tok, reads, writes)
        self.ninst += 1
        return tok

    def barrier(self):
        for e in self.eng:
            for e2 in self.esem:
                if e2 != e and self.cnt[e2] > 0:
                    self._wait(e, (("e", e2), self.cnt[e2]))
            for s, v in self.dcnt.items():
                if v > 0:
                    self._wait(e, (("d", s), v))
        self.lastw.clear()
        self.readers.clear()


def V(t, off, dims, p0=0, npart=128):
    base = t[:]
    ps = base.ap[0][0]
    return bass.AP(base.tensor, base.offset + p0 * ps + off, [[ps, npart]] + [[int(s), int(c)] for s, c in dims])


class Prog:
    def __init__(self, LG, nl=2, dbg=(), phases=None, NR=1):
        L = LG // NR
        self.LG, self.NR = LG, NR
        self.L, self.NB, self.NL = L, L // 128, nl
        self.NTA = 17 + NR
        self.NTB = [n + NR - 1 for n in NBLK_G]
        self.OFFB = [0, self.NTB[0], self.NTB[0] + self.NTB[1]]
        self.NRING = [n - 1 + 2 * NR for n in NBLK_G]
        self.O_KIT, self.O_KB, self.O_CKVA = 2 * L, 3 * L, 15 * L
        self.O_VB = 15 * L + (L // 128) * 256
        self.KCOLS = 15 * L + (L // 128) * 1792
        self.QT = min(1024, L)
        self.dbg = set(dbg)
        self.phases = phases
        nc = self.nc = bass.Bass("TRN2", target_bir_lowering=False)
        self.kb = KB(nc)
        self.ev_i = 0
        self.wr_i = 0
        NB = self.NB

        def din(name, shape):
            return nc.dram_tensor(name, list(shape), F32, kind="ExternalInput").ap()

        def dscr(name, shape, dt):
            kind = "ExternalOutput" if name in self.dbg else "Internal"
            return nc.dram_tensor(name, list(shape), dt, kind=kind).ap()

        self.d_xT = din("xT", [KC, 128, L])
        self.d_c = din("c_pk", [128, KC])
        self.d_wada = din("w_ada", [nl, D, 6 * D])
        self.d_bada = din("b_adaT", [nl, 128, 96])
        self.d_n1g = din("n1g", [nl, 128, KC])
        self.d_n2g = din("n2g", [nl, 128, KC])
        self.d_fg = din("fg", [128, KC])
        self.d_win = din("w_in", [nl, D, NIN])
        self.d_kvg = din("kvg", [nl, 128, 256])
        self.d_ilg = din("ilg", [nl, 128, 64])
        self.d_ilb = din("ilb", [nl, 128, 64])
        self.d_wuk = din("w_uk", [nl, 8, 128, 256])
        self.d_wuv = din("w_uv", [nl, 8, 256, 128])
        self.d_waup = din("w_a_up", [nl, 1024, D])
        self.d_wbup = din("w_b_up", [nl, 512, D])
        self.d_wout = din("w_out", [nl, D, D])
        self.d_wfg = din("w_ffg", [nl, D, DFF])
        self.d_wfu = din("w_ffu", [nl, D, DFF])
        self.d_wfd = din("w_ffd", [nl, DFF, D])
        self.d_ropeC = din("ropeC", [128, NB, 16])
        self.d_ropeS = din("ropeS", [128, NB, 16])
        self.d_bta = din("bta", [128, self.NTA * 8 * 128])
        self.d_btb = din("btb", [128, sum(self.NTB) * 4 * 128])
        self.d_cm = din("cmr", [128, NR * 128])
        self.d_ident = din("ident", [128, 128])
        self.d_out = nc.dram_tensor("outT", [KC, 128, L], F32, kind="ExternalOutput").ap()
        self.d_xs = dscr("xs", [KC, 128, L], F32)
        self.d_qlat = dscr("qlat", [128, 16, L], BF16)
        self.d_qiT = dscr("qiT", [128, 8, L], BF16)
        self.d_wi = dscr("wi", [NB, 128, 16], F32)
        self.d_qb = dscr("qb", [128, 12, L], BF16)
        self.d_gates = dscr("gates", [128, 32, L], BF16)
        def kt(name, cols):
            loc = dscr(name + "_l", [128, cols], BF16)
            full = loc if NR == 1 else dscr(name + "_f", [NR * 128, cols], BF16)
            return loc, full
        self.kA = [kt("kA%d" % l, 3 * L) for l in range(nl)]
        self.kC = [kt("kC%d" % l, NB * 256) for l in range(nl)]
        self.kK = [[kt("kK%d_%d" % (l, g), 4 * L) for g in range(3)] for l in range(nl)]
        self.kV = [[kt("kV%d_%d" % (l, g), NB * 512) for g in range(3)] for l in range(nl)]
        self.d_ya = dscr("yaT", [128, 8, L], BF16)
        self.d_yb = dscr("ybT", [128, 4, L], BF16)

        def sbp(name, f, dt):
            return nc.alloc_sbuf_tensor(name, [128, f], dt)

        self.ident = sbp("identb", 128, BF16)
        self.ones = sbp("onesb", 128, BF16)
        self.cm = sbp("cmf", NR * 128, F32)
        self.ropeC = sbp("ropeCs", NB * 16, F32)
        self.ropeS = sbp("ropeSs", NB * 16, F32)
        self.mod = [sbp("mod%d" % l, 96, F32) for l in range(nl)]
        self.A1 = [sbp("A1_%d" % l, 16, F32) for l in range(nl)]
        self.A2 = [sbp("A2_%d" % l, 16, F32) for l in range(nl)]
        self.fg = sbp("fgs", 16, F32)
        self.wring = [sbp("wring%d" % i, 8192, BF16) for i in range(2)]
        self.ps = [nc.alloc_psum_tensor("psb%d" % i, [128, 512], F32) for i in range(8)]

    def sb(self, es, name, f, dt):
        self.sb_n = getattr(self, "sb_n", 0) + 1
        return es.enter_context(self.nc.sbuf_tensor("s%d_%s" % (self.sb_n, name), [128, f], dt))

    def evq(self):
        self.ev_i += 1
        return "act" if self.ev_i % 2 else "dve"

    def evac(self, out, in_, scale, reads, writes, eng=None):
        e = eng or self.evq()
        if e == "act":
            return self.kb.op("act", lambda g: g.activation(out=out, in_=in_, func=AF.Identity, scale=float(scale)),
                              reads=reads, writes=writes)
        if scale == 1.0:
            return self.kb.op("dve", lambda g: g.tensor_copy(out=out, in_=in_), reads=reads, writes=writes)
        return self.kb.op("dve", lambda g: g.tensor_scalar(out=out, in0=in_, scalar1=float(scale), scalar2=None,
                                                            op0=ALU.mult), reads=reads, writes=writes)

    def load_w(self, dram_ap, dims):
        s = self.wr_i % 2
        self.wr_i += 1
        key = "wring%d" % s
        view = V(self.wring[s], 0, dims)
        self.kb.dma("pool", view, dram_ap, key, writes=[key])
        return view, key, s

    def wslice(self, s, off, dims):
        return V(self.wring[s], off, dims)

    def phase_const(self):
        kb = self.kb
        kb.dma("pool", self.ident[:], self.d_ident, "c_ident", writes=["ident"])
        kb.op("pool", lambda g: g.memset(self.ones[:], 1.0), writes=["ones"])
        kb.dma("sp", self.cm[:], self.d_cm, "c_misc", writes=["cm"])
        kb.dma("sp", self.ropeC[:], self.d_ropeC.rearrange("p b f -> p (b f)"), "c_misc", writes=["ropeC"])
        kb.dma("sp", self.ropeS[:], self.d_ropeS.rearrange("p b f -> p (b f)"), "c_misc", writes=["ropeS"])
        kb.dma("sp", self.fg[:], self.d_fg, "c_misc", writes=["fg"])

    def phase_mod(self):
        kb, ps = self.kb, self.ps
        with ExitStack() as es:
            sc = self.sb(es, "sc", 16, F32)
            scb = self.sb(es, "scb", 16, BF16)
            bada = self.sb(es, "bada", 96, F32)
            ng = self.sb(es, "ng", 32, F32)
            W = [self.sb(es, "wada%d" % i, 16 * 1536, BF16) for i in range(2)]
            kb.dma("sp", sc[:], self.d_c, "m_c", writes=["sc"])
            kb.op("act", lambda g: g.activation(out=scb[:], in_=sc[:], func=AF.Silu), reads=["sc"], writes=["scb"])
            n = 0
            for l in range(self.NL):
                for cb in range(8):
                    s = n % 2
                    n += 1
                    wk = "wada%d" % s
                    kb.dma("pool", V(W[s], 0, [(1536, 16), (1, 1536)]),
                           self.d_wada[l, :, cb * 1536:(cb + 1) * 1536].rearrange("(k p) c -> p k c", p=128),
                           wk, writes=[wk])
                    for jj in range(12):
                        j = cb * 12 + jj
                        for k in range(16):
                            kb.mm(V(ps[0], j, [(1, 1)]), V(W[s], k * 1536 + jj * 128, [(1, 128)]),
                                  V(scb, k, [(1, 1)]), k == 0, k == 15, reads=[wk, "scb"], writes=["ps0"],
                                  last=(k == 15))
                kb.dma("sp", bada[:], self.d_bada[l], "m_b", writes=["bada"])
                kb.dma("sp", V(ng, 0, [(1, 16)]), self.d_n1g[l], "m_b", writes=["ng"])
                kb.dma("sp", V(ng, 16, [(1, 16)]), self.d_n2g[l], "m_b", writes=["ng"])
                mod = self.mod[l]
                kb.op("dve", lambda g: g.tensor_tensor(out=mod[:], in0=V(ps[0], 0, [(1, 96)]), in1=bada[:], op=ALU.add),
                      reads=["ps0", "bada"], writes=["mod%d" % l])
                kb.op("dve", lambda g: g.scalar_tensor_tensor(out=self.A1[l][:], in0=V(mod, 16, [(1, 16)]), scalar=1.0,
                                                              in1=V(ng, 0, [(1, 16)]), op0=ALU.add, op1=ALU.mult),
                      reads=["mod%d" % l, "ng"], writes=["A1_%d" % l])
                kb.op("dve", lambda g: g.scalar_tensor_tensor(out=self.A2[l][:], in0=V(mod, 64, [(1, 16)]), scalar=1.0,
                                                              in1=V(ng, 16, [(1, 16)]), op0=ALU.add, op1=ALU.mult),
                      reads=["mod%d" % l, "ng"], writes=["A2_%d" % l])
            kb.barrier()

    def norm_tiles(self, es, xsrc, A, shift, t0, QT, hT, final=False):
        kb, ps = self.kb, self.ps
        xt = [self.sb(es, "nx%d" % i, 16 * 512, F32) for i in range(2)]
        sq = self.sb(es, "nsq", 16 * 512, BF16)
        rs = self.sb(es, "nrs", 512, F32)
        toks = []
        for ti in range(QT // 512):
            b = ti % 2
            tt = t0 + ti * 512
            xk = "nx%d" % b
            kb.dma("sp", V(xt[b], 0, [(512, 16), (1, 512)]), xsrc[:, :, tt:tt + 512].rearrange("k p t -> p k t"),
                   xk, reads=["x_t%d" % (tt // 512)], writes=[xk])
            kb.op("act", lambda g: g.activation(out=sq[:], in_=xt[b][:], func=AF.Square), reads=[xk], writes=["nsq"])
            for k in range(16):
                kb.mm(ps[1][:], self.ones[:], V(sq, k * 512, [(1, 512)]), k == 0, k == 15, reads=["nsq", "ones"],
                      writes=["ps1"], last=(k == 15))
            kb.op("act", lambda g: g.activation(out=rs[:], in_=ps[1][:], func=AF.Sqrt, scale=1.0 / D, bias=self.epsb[:]),
                  reads=["ps1"], writes=["nrs"])
            kb.op("dve", lambda g: g.reciprocal(out=rs[:], in_=rs[:]), reads=["nrs"], writes=["nrs"])
            kb.op("dve", lambda g: g.tensor_tensor(out=V(xt[b], 0, [(512, 16), (1, 512)]),
                                                   in0=V(xt[b], 0, [(512, 16), (1, 512)]),
                                                   in1=V(rs, 0, [(0, 16), (1, 512)]), op=ALU.mult),
                  reads=[xk, "nrs"], writes=[xk])
            if final:
                for k in range(16):
                    kb.op("dve" if k % 2 else "act",
                          (lambda g: g.tensor_scalar(out=V(xt[b], k * 512, [(1, 512)]), in0=V(xt[b], k * 512, [(1, 512)]),
                                                     scalar1=V(A, k, [(1, 1)]), scalar2=None, op0=ALU.mult)) if k % 2 else
                          (lambda g: g.activation(out=V(xt[b], k * 512, [(1, 512)]), in_=V(xt[b], k * 512, [(1, 512)]),
                                                  func=AF.Identity, scale=V(A, k, [(1, 1)]))),
                          reads=[xk, "fg"], writes=[xk])
                toks.append(kb.dma("sp", self.d_out[:, :, tt:tt + 512].rearrange("k p t -> p k t"),
                                   V(xt[b], 0, [(512, 16), (1, 512)]), "outst", reads=[xk]))
                continue
            for k in range(16):
                o = V(hT, k * QT + ti * 512, [(1, 512)])
                i_ = V(xt[b], k * 512, [(1, 512)])
                if k % 2:
                    kb.op("dve", lambda g: g.tensor_scalar(out=o, in0=i_, scalar1=V(A, k, [(1, 1)]),
                                                           scalar2=V(shift[0], shift[1] + k, [(1, 1)]),
                                                           op0=ALU.mult, op1=ALU.add), reads=[xk], writes=["hT"])
                else:
                    kb.op("act", lambda g: g.activation(out=o, in_=i_, func=AF.Identity, scale=V(A, k, [(1, 1)]),
                                                        bias=V(shift[0], shift[1] + k, [(1, 1)])), reads=[xk], writes=["hT"])
        return toks

    def fm_proj(self, hT, QT, t0, wv, wk, c0, ncol, out_cb):
        kb, ps = self.kb, self.ps
        for c in range(ncol // 128):
            for ti in range(QT // 512):
                bank = 2 + (self.bank_i % 2)
                self.bank_i += 1
                pk = "ps%d" % bank
                for k in range(16):
                    kb.mm(ps[bank][:], V(wv[0], wv[1] + k * wv[2] + c * 128, [(1, 128)]),
                          V(hT, k * QT + ti * 512, [(1, 512)]), k == 0, k == 15, reads=[wk, "hT"], writes=[pk],
                          last=(k == 15))
                out_cb(c0 + c, ti, bank)

    def phase_p1(self, l, t0, QT, hT, es, part):
        kb, ps, L = self.kb, self.ps, self.L
        self.bank_i = 0
        win = self.d_win[l]
        self.d_ckvT = self.kA[l][0][:, 0:2 * L].rearrange("p (c t) -> p c t", c=2)
        self.d_kiT = self.kA[l][0][:, 2 * L:3 * L]
        self.d_ckvA = self.kC[l][0].rearrange("p (b c) -> b p c", c=256)
        kKl = [self.kK[l][g][0].rearrange("p (h t) -> p h t", h=4) for g in range(3)]
        kVl = [self.kV[l][g][0] for g in range(3)]
        stg = [self.sb(es, "p1stg%d" % i, 512, BF16) for i in range(4)]
        self.stg_i = 0

        def nstg():
            i = self.stg_i % 4
            self.stg_i += 1
            return stg[i], "p1stg%d" % i

        def wcols(c0, n):
            return win[:, c0:c0 + n].rearrange("(k p) c -> p k c", p=128)

        if part == "q":
            self._p1_q(l, t0, QT, hT, es, wcols, nstg)
        else:
            self._p1_k(l, t0, QT, hT, es, wcols, nstg, kKl, kVl)

    def _p1_q(self, l, t0, QT, hT, es, wcols, nstg):
        kb, ps, L = self.kb, self.ps, self.L
        wuk = self.sb(es, "wuk", 8 * 256, BF16)
        kb.dma("pool", V(wuk, 0, [(256, 8), (1, 256)]), self.d_wuk[l].rearrange("h d c -> d h c"), "p1wuk", writes=["wuk"])
        qa = [self.sb(es, "qa%d" % i, 512, BF16) for i in range(2)]
        self.qa_i = 0
        for grp in range(2):
            wv_, wk, s = self.load_w(wcols(C_QA + grp * 512, 512), [(512, 16), (1, 512)])

            def cb_qa(h, ti, bank):
                qi = self.qa_i % 2
                self.qa_i += 1
                qk = "qa%d" % qi
                self.evac(qa[qi][:], ps[bank][:], 1.0, ["ps%d" % bank], [qk])
                for cc in range(2):
                    b2 = 4 + cc
                    kb.mm(ps[b2][:], V(wuk, h * 256 + cc * 128, [(1, 128)]), qa[qi][:], True, True,
                          reads=["wuk", qk], writes=["ps%d" % b2])
                    st, sk = nstg()
                    self.evac(st[:], ps[b2][:], 128 ** -0.5, ["ps%d" % b2], [sk])
                    tt = t0 + ti * 512
                    kb.dma("sp", self.d_qlat[:, cc * 8 + h, tt:tt + 512], st[:], sk, reads=[sk], writes=["qlat"])

            self.fm_proj(hT, QT, t0, (self.wring[s], 0, 512), wk, grp * 4, 512, cb_qa)

        self._p1_fm(t0, QT, hT, wcols, nstg, C_QB, self.d_qb, 128 ** -0.5, "qb", None)
        self._p1_gates_qi(l, t0, QT, hT, es, wcols, nstg)

    def _p1_fm(self, t0, QT, hT, wcols, nstg, cbase, dst, scl, dk, kKl):
        kb, ps = self.kb, self.ps
        if True:
            for grp in range(3):
                wv_, wk, s = self.load_w(wcols(cbase + grp * 512, 512), [(512, 16), (1, 512)])

                def cb_b(h, ti, bank, dst=dst, scl=scl, dk=dk):
                    st, sk = nstg()
                    self.evac(st[:], ps[bank][:], scl, ["ps%d" % bank], [sk])
                    tt = t0 + ti * 512
                    dap = dst[:, h, tt:tt + 512] if dst is not None else kKl[h // 4][:, h % 4, tt:tt + 512]
                    kb.dma("sp", dap, st[:], sk, reads=[sk], writes=[dk])

                self.fm_proj(hT, QT, t0, (self.wring[s], 0, 512), wk, grp * 4, 512, cb_b)

    def _p1_gates_qi(self, l, t0, QT, hT, es, wcols, nstg):
        kb, ps, L = self.kb, self.ps, self.L
        for grp in range(8):
            wv_, wk, s = self.load_w(wcols(C_GATE + grp * 512, 512), [(512, 16), (1, 512)])

            def cb_g(c, ti, bank):
                st, sk = nstg()
                kb.op("act", lambda g: g.activation(out=st[:], in_=ps[bank][:], func=AF.Sigmoid),
                      reads=["ps%d" % bank], writes=[sk])
                tt = t0 + ti * 512
                kb.dma("sp", self.d_gates[:, c, tt:tt + 512], st[:], sk, reads=[sk], writes=["gates"])

            self.fm_proj(hT, QT, t0, (self.wring[s], 0, 512), wk, grp * 4, 512, cb_g)

        self._p1_qi(l, t0, QT, hT, es, wcols)

    def _p1_k(self, l, t0, QT, hT, es, wcols, nstg, kKl, kVl):
        kb, ps, L = self.kb, self.ps, self.L
        self._p1_fm(t0, QT, hT, wcols, nstg, C_KB, None, 1.0, "kbT", kKl)
        vst = [self.sb(es, "vst%d" % i, 512, BF16) for i in range(2)]
        n = 0
        for grp in range(3):
            wv_, wk, s = self.load_w(wcols(C_VB + grp * 512, 512), [(512, 16), (1, 512)])
            for bi in range(QT // 128):
                blk = t0 // 128 + bi
                bank = 2 + (self.bank_i % 2)
                self.bank_i += 1
                for k in range(16):
                    kb.mm(ps[bank][:], V(hT, k * QT + bi * 128, [(1, 128)]), V(self.wring[s], k * 512, [(1, 512)]),
                          k == 0, k == 15, reads=[wk, "hT"], writes=["ps%d" % bank], last=(k == 15))
                v = vst[n % 2]
                vk = "vst%d" % (n % 2)
                n += 1
                self.evac(v[:], ps[bank][:], 1.0, ["ps%d" % bank], [vk])
                kb.dma("sp", kVl[grp][:, blk * 512:(blk + 1) * 512], v[:], vk, reads=[vk], writes=["vb"])

        kvg = self.sb(es, "kvg", 256, F32)
        ilg = self.sb(es, "ilg", 128, F32)
        kb.dma("sp", kvg[:], self.d_kvg[l], "p1c", writes=["kvg"])
        kb.dma("sp", V(ilg, 0, [(1, 64)]), self.d_ilg[l], "p1c", writes=["ilg"])
        kb.dma("sp", V(ilg, 64, [(1, 64)]), self.d_ilb[l], "p1c", writes=["ilg"])
        s = self.wr_i % 2
        self.wr_i += 1
        wk = "wring%d" % s
        kb.dma("pool", V(self.wring[s], 0, [(336, 16), (1, 256)]), wcols(C_CKV, 256), wk, writes=[wk])
        kb.dma("pool", V(self.wring[s], 256, [(336, 16), (1, 80)]), wcols(C_KI, 80), wk, writes=[wk])
        junk = self.sb(es, "junk", 256, F32)
        st8 = self.sb(es, "st8", 16, F32)
        ckvb = [self.sb(es, "ckvb%d" % i, 256, BF16) for i in range(2)]
        ckt = [self.sb(es, "ckt%d" % i, 256, BF16) for i in range(2)]
        kn = self.sb(es, "kn", 64, F32)
        kt = self.sb(es, "ktmp", 64, F32)
        kr = [self.sb(es, "kr%d" % i, 128, BF16) for i in range(2)]
        kit = [self.sb(es, "kit%d" % i, 128, BF16) for i in range(2)]
        wis = [self.sb(es, "wis%d" % i, 16, F32) for i in range(2)]
        for bi in range(QT // 128):
            blk = t0 // 128 + bi
            p = bi % 2
            bank = 2 + (self.bank_i % 2)
            self.bank_i += 1
            pk = "ps%d" % bank
            for k in range(16):
                kb.mm(V(ps[bank], 0, [(1, 336)]), V(hT, k * QT + bi * 128, [(1, 128)]), V(self.wring[s], k * 336, [(1, 336)]),
                      k == 0, k == 15, reads=[wk, "hT"], writes=[pk], last=(k == 15))
            kb.op("act", lambda g: g.activation(out=junk[:], in_=V(ps[bank], 0, [(1, 256)]), func=AF.Square),
                  reads=[pk], writes=["junk"])
            kb.op("dve", lambda g: g.reduce_sum(out=V(st8, 0, [(1, 1)]), in_=junk[:], axis=mybir.AxisListType.X),
                  reads=["junk"], writes=["st8a"])
            kb.op("act", lambda g: g.activation(out=V(st8, 1, [(1, 1)]), in_=V(st8, 0, [(1, 1)]), func=AF.Sqrt,
                                                scale=1.0 / 256, bias=self.epsb[:]), reads=["st8a"], writes=["st8b"])
            kb.op("dve", lambda g: g.reciprocal(out=V(st8, 2, [(1, 1)]), in_=V(st8, 1, [(1, 1)])), reads=["st8b"], writes=["st8c"])
            cb_, ck = ckvb[p], "ckvb%d" % p
            kb.op("dve", lambda g: g.scalar_tensor_tensor(out=cb_[:], in0=V(ps[bank], 0, [(1, 256)]), scalar=V(st8, 2, [(1, 1)]),
                                                          in1=kvg[:], op0=ALU.mult, op1=ALU.mult),
                  reads=[pk, "st8c", "kvg"], writes=[ck])
            kb.dma("sp", self.d_ckvA[blk], cb_[:], ck, reads=[ck], writes=["ckvA"])
            for cc in range(2):
                kb.mm(V(ps[6], cc * 128, [(1, 128)]), V(cb_, cc * 128, [(1, 128)]), self.ident[:], True, True,
                      reads=[ck, "ident"], writes=["ps6"])
            ct, ctk = ckt[p], "ckt%d" % p
            self.evac(ct[:], V(ps[6], 0, [(1, 256)]), 1.0, ["ps6"], [ctk])
            kb.dma("sp", self.d_ckvT[:, :, blk * 128:(blk + 1) * 128], V(ct, 0, [(128, 2), (1, 128)]), ctk,
                   reads=[ctk], writes=["ckvT"])
            kb.op("dve", lambda g: g.reduce_sum(out=V(st8, 3, [(1, 1)]), in_=V(ps[bank], 256, [(1, 64)]), axis=mybir.AxisListType.X),
                  reads=[pk], writes=["st8d"])
            kb.op("act", lambda g: g.activation(out=V(junk, 0, [(1, 64)]), in_=V(ps[bank], 256, [(1, 64)]), func=AF.Square),
                  reads=[pk], writes=["junk"])
            kb.op("dve", lambda g: g.reduce_sum(out=V(st8, 4, [(1, 1)]), in_=V(junk, 0, [(1, 64)]), axis=mybir.AxisListType.X),
                  reads=["junk"], writes=["st8e"])
            kb.op("dve", lambda g: g.tensor_scalar(out=V(st8, 5, [(1, 1)]), in0=V(st8, 3, [(1, 1)]), scalar1=1.0 / 64, scalar2=None,
                                                   op0=ALU.mult), reads=["st8d"], writes=["st8f"])
            kb.op("dve", lambda g: g.tensor_tensor(out=V(st8, 6, [(1, 1)]), in0=V(st8, 5, [(1, 1)]), in1=V(st8, 5, [(1, 1)]),
                                                   op=ALU.mult), reads=["st8f"], writes=["st8g"])
            kb.op("dve", lambda g: g.scalar_tensor_tensor(out=V(st8, 7, [(1, 1)]), in0=V(st8, 4, [(1, 1)]), scalar=1.0 / 64,
                                                          in1=V(st8, 6, [(1, 1)]), op0=ALU.mult, op1=ALU.subtract),
                  reads=["st8e", "st8g"], writes=["st8h"])
            kb.op("act", lambda g: g.activation(out=V(st8, 8, [(1, 1)]), in_=V(st8, 7, [(1, 1)]), func=AF.Sqrt,
                                                bias=self.epsb[:]), reads=["st8h"], writes=["st8i"])
            kb.op("dve", lambda g: g.reciprocal(out=V(st8, 9, [(1, 1)]), in_=V(st8, 8, [(1, 1)])), reads=["st8i"], writes=["st8j"])
            kb.op("dve", lambda g: g.tensor_scalar(out=kn[:], in0=V(ps[bank], 256, [(1, 64)]), scalar1=V(st8, 5, [(1, 1)]),
                                                   scalar2=V(st8, 9, [(1, 1)]), op0=ALU.subtract, op1=ALU.mult),
                  reads=[pk, "st8f", "st8j"], writes=["kn"])
            kb.op("dve", lambda g: g.tensor_tensor(out=kn[:], in0=kn[:], in1=V(ilg, 0, [(1, 64)]), op=ALU.mult),
                  reads=["kn", "ilg"], writes=["kn"])
            kb.op("dve", lambda g: g.tensor_tensor(out=kn[:], in0=kn[:], in1=V(ilg, 64, [(1, 64)]), op=ALU.add),
                  reads=["kn", "ilg"], writes=["kn"])
            cosb = V(self.ropeC, blk * 16, [(1, 16)])
            sinb = V(self.ropeS, blk * 16, [(1, 16)])
            x1, x2 = V(kn, 0, [(1, 16)]), V(kn, 16, [(1, 16)])
            kb.op("dve", lambda g: g.tensor_tensor(out=V(kt, 0, [(1, 16)]), in0=x1, in1=cosb, op=ALU.mult), reads=["kn", "ropeC"], writes=["kt0"])
            kb.op("dve", lambda g: g.tensor_tensor(out=V(kt, 16, [(1, 16)]), in0=x2, in1=sinb, op=ALU.mult), reads=["kn", "ropeS"], writes=["kt1"])
            kb.op("dve", lambda g: g.tensor_tensor(out=V(kt, 32, [(1, 16)]), in0=x1, in1=sinb, op=ALU.mult), reads=["kn", "ropeS"], writes=["kt2"])
            kb.op("dve", lambda g: g.tensor_tensor(out=V(kt, 48, [(1, 16)]), in0=x2, in1=cosb, op=ALU.mult), reads=["kn", "ropeC"], writes=["kt3"])
            krp, krk = kr[p], "kr%d" % p
            for half in range(2):
                kb.op("dve", lambda g: g.tensor_tensor(out=V(krp, half * 64, [(1, 16)]), in0=V(kt, 0, [(1, 16)]), in1=V(kt, 16, [(1, 16)]),
                                                       op=ALU.subtract), reads=["kt0", "kt1"], writes=[krk])
                kb.op("dve", lambda g: g.tensor_tensor(out=V(krp, half * 64 + 16, [(1, 16)]), in0=V(kt, 32, [(1, 16)]), in1=V(kt, 48, [(1, 16)]),
                                                       op=ALU.add), reads=["kt2", "kt3"], writes=[krk])
                kb.op("dve", lambda g: g.tensor_copy(out=V(krp, half * 64 + 32, [(1, 32)]), in_=V(kn, 32, [(1, 32)])),
                      reads=["kn"], writes=[krk])
            kb.mm(V(ps[7], 0, [(1, 128)]), krp[:], self.ident[:], True, True, reads=[krk, "ident"], writes=["ps7"])
            kip, kik = kit[p], "kit%d" % p
            self.evac(kip[:], V(ps[7], 0, [(1, 128)]), 1.0, ["ps7"], [kik])
            kb.dma("sp", self.d_kiT[:, blk * 128:(blk + 1) * 128], kip[:], kik, reads=[kik], writes=["kiT"])
            wp, wpk = wis[p], "wis%d" % p
            kb.op("dve", lambda g: g.tensor_scalar(out=wp[:], in0=V(ps[bank], 320, [(1, 16)]), scalar1=1.0 / 32.0, scalar2=None,
                                                   op0=ALU.mult), reads=[pk], writes=[wpk])
            kb.dma("sp", self.d_wi[blk], wp[:], wpk, reads=[wpk], writes=["wi"])

    def _p1_qi(self, l, t0, QT, hT, es, wcols):
        kb, ps, L = self.kb, self.ps, self.L
        qr = [self.sb(es, "qr%d" % i, 512, BF16) for i in range(2)]
        qt4 = [self.sb(es, "qt4%d" % i, 512, F32) for i in range(1)]
        qit = [self.sb(es, "qit%d" % i, 512, BF16) for i in range(2)]
        n = 0
        for grp in range(2):
            wv_, wk, s = self.load_w(wcols(C_QI + grp * 512, 512), [(512, 16), (1, 512)])
            for bi in range(QT // 128):
                blk = t0 // 128 + bi
                bank = 2 + (self.bank_i % 2)
                self.bank_i += 1
                pk = "ps%d" % bank
                for k in range(16):
                    kb.mm(ps[bank][:], V(hT, k * QT + bi * 128, [(1, 128)]), V(self.wring[s], k * 512, [(1, 512)]),
                          k == 0, k == 15, reads=[wk, "hT"], writes=[pk], last=(k == 15))
                p = n % 2
                n += 1
                q, qk = qr[p], "qr%d" % p
                t4 = qt4[0]
                cosb = V(self.ropeC, blk * 16, [(0, 8), (1, 16)])
                sinb = V(self.ropeS, blk * 16, [(0, 8), (1, 16)])
                x1 = V(ps[bank], 0, [(64, 8), (1, 16)])
                x2 = V(ps[bank], 16, [(64, 8), (1, 16)])
                kb.op("dve", lambda g: g.tensor_tensor(out=V(t4, 0, [(16, 8), (1, 16)]), in0=x1, in1=cosb, op=ALU.mult), reads=[pk, "ropeC"], writes=["qt0"])
                kb.op("dve", lambda g: g.tensor_tensor(out=V(t4, 128, [(16, 8), (1, 16)]), in0=x2, in1=sinb, op=ALU.mult), reads=[pk, "ropeS"], writes=["qt1"])
                kb.op("dve", lambda g: g.tensor_tensor(out=V(t4, 256, [(16, 8), (1, 16)]), in0=x1, in1=sinb, op=ALU.mult), reads=[pk, "ropeS"], writes=["qt2"])
                kb.op("dve", lambda g: g.tensor_tensor(out=V(t4, 384, [(16, 8), (1, 16)]), in0=x2, in1=cosb, op=ALU.mult), reads=[pk, "ropeC"], writes=["qt3"])
                kb.op("dve", lambda g: g.tensor_tensor(out=V(q, 0, [(64, 8), (1, 16)]), in0=V(t4, 0, [(16, 8), (1, 16)]),
                                                       in1=V(t4, 128, [(16, 8), (1, 16)]), op=ALU.subtract), reads=["qt0", "qt1"], writes=[qk])
                kb.op("dve", lambda g: g.tensor_tensor(out=V(q, 16, [(64, 8), (1, 16)]), in0=V(t4, 256, [(16, 8), (1, 16)]),
                                                       in1=V(t4, 384, [(16, 8), (1, 16)]), op=ALU.add), reads=["qt2", "qt3"], writes=[qk])
                kb.op("act", lambda g: g.activation(out=V(q, 32, [(64, 8), (1, 32)]), in_=V(ps[bank], 32, [(64, 8), (1, 32)]),
                                                    func=AF.Identity), reads=[pk], writes=[qk])
                for pr in range(4):
                    kb.mm(V(ps[6], pr * 128, [(1, 128)]), V(q, pr * 128, [(1, 128)]), self.ident[:], True, True,
                          reads=[qk, "ident"], writes=["ps6"])
                qi_, qik = qit[p], "qit%d" % p
                self.evac(qi_[:], ps[6][:], 1.0, ["ps6"], [qik])
                kb.dma("sp", self.d_qiT[:, grp * 4:(grp + 1) * 4, blk * 128:(blk + 1) * 128], V(qi_, 0, [(128, 4), (1, 128)]),
                       qik, reads=[qik], writes=["qiT"])

    def phase_att_a(self, l):
        kb, ps, L, NB, NR = self.kb, self.ps, self.L, self.NB, self.NR
        kAf, kCf = self.kA[l][1], self.kC[l][1]
        NTA = self.NTA
        with ExitStack() as es:
            ckvT = self.sb(es, "ckvTs", NR * 2 * L, BF16)
            ckvA = self.sb(es, "ckvAs", NR * NB * 256, BF16)
            kiT = self.sb(es, "kiTs", NR * L, BF16)
            bta = self.sb(es, "btas", NTA * 8 * 128, BF16)
            wuv = self.sb(es, "wuvs", 8 * 2 * 128, BF16)
            for rk in range(NR):
                kb.dma("sp", V(ckvT, rk * 2 * L, [(1, 2 * L)]), kAf[rk * 128:(rk + 1) * 128, 0:2 * L], "aa_k", reads=["kfull"], writes=["ckvTs"])
                kb.dma("sp", V(kiT, rk * L, [(1, L)]), kAf[rk * 128:(rk + 1) * 128, 2 * L:3 * L], "aa_k", reads=["kfull"], writes=["kiTs"])
                kb.dma("sp", V(ckvA, rk * NB * 256, [(1, NB * 256)]), kCf[rk * 128:(rk + 1) * 128, :], "aa_k",
                       reads=["kfull"], writes=["ckvAs"])
            kb.dma("pool", bta[:], self.d_bta, "aa_c", writes=["btas"])
            kb.dma("pool", V(wuv, 0, [(256, 8), (128, 2), (1, 128)]), self.d_wuv[l].rearrange("h (cc c) d -> c h cc d", c=128),
                   "aa_c", writes=["wuvs"])
            qiT = [self.sb(es, "aqi%d" % i, 8 * 128, BF16) for i in range(2)]
            wi = [self.sb(es, "awi%d" % i, 16, F32) for i in range(2)]
            qlat = [self.sb(es, "aql%d" % i, 16 * 128, BF16) for i in range(4)]
            dg = self.sb(es, "adg", 16 * 128, BF16)
            R = [self.sb(es, "aR%d" % i, 512, BF16) for i in range(4)]
            NK = NR * L
            scs = [self.sb(es, "asc%d" % i, NK, F32) for i in range(2)]
            m8s = [self.sb(es, "am8%d" % i, 8, F32) for i in range(2)]
            assert NK <= 4096
            negm = [(self.wring[i // 2], (i % 2) * 4096) for i in range(4)]
            Pst = [self.sb(es, "aP%d" % i, 512, BF16) for i in range(3)]
            olat = self.sb(es, "aol", 2 * 512, BF16)
            densb = self.sb(es, "adn", 512, F32)
            ysb = self.sb(es, "ays", 512, F32)
            yst = [self.sb(es, "ayst%d" % i, 512, BF16) for i in range(2)]
            st = dict(rn=0, pn=0, yn=0, ln=0)
            LB = (4, 5, 7)

            def nkeys(s):
                return NR * (s + 1) * 128

            def stage1(s):
                b = s % 2
                t0 = s * 128
                scA, sck = scs[b], "asc%d" % b
                kb.dma("sp", V(qlat[s % 4], 0, [(128, 16), (1, 128)]), self.d_qlat[:, :, t0:t0 + 128], "aa_ql%d" % (s % 4),
                       reads=["qlat"], writes=["aql%d" % (s % 4)])
                if nkeys(s) <= 256:
                    return
                kb.dma("sp", V(qiT[b], 0, [(128, 8), (1, 128)]), self.d_qiT[:, :, t0:t0 + 128], "aa_q%d" % b,
                       reads=["qiT"], writes=["aqi%d" % b])
                kb.dma("sp", wi[b][:], self.d_wi[s], "aa_q%d" % b, reads=["wi"], writes=["awi%d" % b])
                kb.op("dve", lambda g: g.tensor_tensor(out=V(dg, 0, [(128, 16), (1, 128)]), in0=V(self.ident, 0, [(0, 16), (1, 128)]),
                                                       in1=V(wi[b], 0, [(1, 16), (0, 128)]), op=ALU.mult),
                      reads=["ident", "awi%d" % b], writes=["adg"])
                nb_r = s + 1
                for rk in range(NR):
                    cbase = rk * nb_r * 128
                    for kc in range((nb_r + 3) // 4):
                        w = min(512, nb_r * 128 - kc * 512)
                        for h in range(16):
                            pr, hf = h // 2, h % 2
                            bank = LB[st["ln"] % 3]
                            st["ln"] += 1
                            kb.mm(V(ps[bank], 0, [(1, w)]), V(qiT[b], pr * 128, [(1, 128)], p0=hf * 64, npart=64),
                                  V(kiT, rk * L + kc * 512, [(1, w)], p0=hf * 64, npart=64), True, True,
                                  reads=["aqi%d" % b, "kiTs"], writes=["ps%d" % bank])
                            r = R[st["rn"] % 4]
                            rkey = "aR%d" % (st["rn"] % 4)
                            st["rn"] += 1
                            if h % 2:
                                kb.op("act", lambda g: g.activation(out=V(r, 0, [(1, w)]), in_=V(ps[bank], 0, [(1, w)]), func=AF.Relu),
                                      reads=["ps%d" % bank], writes=[rkey])
                            else:
                                kb.op("dve", lambda g: g.tensor_scalar(out=V(r, 0, [(1, w)]), in0=V(ps[bank], 0, [(1, w)]), scalar1=0.0,
                                                                       scalar2=None, op0=ALU.max), reads=["ps%d" % bank], writes=[rkey])
                            kb.mm(V(ps[6], 0, [(1, w)]), V(dg, h * 128, [(1, 128)]), V(r, 0, [(1, w)]), h == 0, h == 15,
                                  reads=["adg", rkey], writes=["ps6"], last=(h == 15))
                        kb.op("act", lambda g: g.activation(out=V(scA, cbase + kc * 512, [(1, w)]), in_=V(ps[6], 0, [(1, w)]),
                                                            func=AF.Identity), reads=["ps6"], writes=[sck])
                    kb.op("dve", lambda g: g.tensor_tensor(out=V(scA, cbase + s * 128, [(1, 128)]), in0=V(scA, cbase + s * 128, [(1, 128)]),
                                                           in1=V(self.cm, rk * 128, [(1, 128)]), op=ALU.add), reads=[sck, "cm"], writes=[sck])

            def stage2(s):
                n = nkeys(s)
                nm, nk = negm[s % 4], "anegm%d" % (s % 4)
                sc, sck = scs[s % 2], "asc%d" % (s % 2)
                m8, mk = m8s[s % 2], "am8%d" % (s % 2)
                if n <= 256:
                    kb.op("pool", lambda g: g.memset(V(nm[0], nm[1], [(1, n)]), 0.0), writes=[nk])
                    return
                for r_ in range(32):
                    kb.op("dve", lambda g: g.max(out=m8[:], in_=V(sc, 0, [(1, n)])), reads=[sck], writes=[mk])
                    yield
                    kb.op("dve", lambda g: g.match_replace(out=V(sc, 0, [(1, n)]), in_to_replace=m8[:],
                                                           in_values=V(sc, 0, [(1, n)]), imm_value=-3.0e38),
                          reads=[mk, sck], writes=[sck])
                    yield
                kb.op("dve", lambda g: g.tensor_scalar(out=V(nm[0], nm[1], [(1, n)]), in0=V(sc, 0, [(1, n)]), scalar1=-1.0e38,
                                                       scalar2=NEG, op0=ALU.is_gt, op1=ALU.mult),
                      reads=[sck], writes=[nk])
                yield

            def inter(*gens):
                gens = list(gens)
                while gens:
                    for g_ in list(gens):
                        try:
                            next(g_)
                        except StopIteration:
                            gens.remove(g_)

            def stage3(s):
                b = s % 4
                t0 = s * 128
                nm, nk = negm[b], "anegm%d" % b
                nb_r = s + 1
                keys = [(rk, sj) for rk in range(NR) for sj in range(nb_r)]
                for g4 in range(2):
                    for idx, (rk, sj) in enumerate(keys):
                        j = NR * sj + rk
                        dl = min(NR * s + NR - 1 - j, NTA - 1)
                        first, lastk = idx == 0, idx == len(keys) - 1
                        sb_ = LB[st["pn"] % 3]
                        sk = "ps%d" % sb_
                        kb.mm(ps[sb_][:], self.ident[:], V(bta, (dl * 8 + g4 * 4) * 128, [(1, 512)]), True, False,
                              reads=["ident", "btas"], writes=[sk], last=False)
                        kb.mm(ps[sb_][:], V(nm[0], nm[1] + (rk * nb_r + sj) * 128, [(1, 128)]), V(self.ident, 0, [(0, 4), (1, 128)]), False, False,
                              reads=[nk, "ident"], writes=[sk], last=False)
                        for cc in range(2):
                            kb.mm(ps[sb_][:], V(ckvT, (rk * 2 + cc) * L + sj * 128, [(1, 128)]),
                                  V(qlat[b], (cc * 8 + g4 * 4) * 128, [(1, 512)]), False, cc == 1,
                                  reads=["ckvTs", "aql%d" % b], writes=[sk], last=(cc == 1))
                        pt = Pst[st["pn"] % 3]
                        pk_ = "aP%d" % (st["pn"] % 3)
                        st["pn"] += 1
                        kb.op("act", lambda g: g.activation(out=pt[:], in_=ps[sb_][:], func=AF.Exp), reads=[sk], writes=[pk_])
                        for cc in range(2):
                            kb.mm(ps[cc][:], V(ckvA, (rk * NB + sj) * 256 + cc * 128, [(1, 128)]), pt[:], first, lastk,
                                  reads=["ckvAs", pk_], writes=["ps%d" % cc], last=lastk)
                        kb.mm(ps[2][:], self.ones[:], pt[:], first, lastk, reads=["ones", pk_], writes=["ps2"], last=lastk)
                    for cc in range(2):
                        self.evac(V(olat, cc * 512, [(1, 512)]), ps[cc][:], 1.0, ["ps%d" % cc], ["aol"], eng="act")
                    kb.op("act", lambda g: g.activation(out=densb[:], in_=ps[2][:], func=AF.Ln), reads=["ps2"], writes=["adn"])
                    kb.op("act", lambda g: g.activation(out=densb[:], in_=densb[:], func=AF.Exp, scale=-1.0), reads=["adn"], writes=["adn"])
                    for h in range(4):
                        hh = g4 * 4 + h
                        for cc in range(2):
                            kb.mm(V(ps[3], h * 128, [(1, 128)]), V(wuv, hh * 256 + cc * 128, [(1, 128)]),
                                  V(olat, cc * 512 + h * 128, [(1, 128)]), cc == 0, cc == 1, reads=["wuvs", "aol"], writes=["ps3"],
                                  last=(cc == 1))
                    self.evac(ysb[:], ps[3][:], 1.0, ["ps3"], ["ays"], eng="act")
                    y = yst[st["yn"] % 2]
                    yk = "ayst%d" % (st["yn"] % 2)
                    st["yn"] += 1
                    kb.op("pool", lambda g: g.tensor_tensor(out=y[:], in0=ysb[:], in1=densb[:], op=ALU.mult),
                          reads=["ays", "adn"], writes=[yk])
                    kb.dma("sp", self.d_ya[:, g4 * 4:(g4 + 1) * 4, t0:t0 + 128], V(y, 0, [(128, 4), (1, 128)]), yk,
                           reads=[yk], writes=["yaT"])

            stage1(0)
            stage1(1)
            inter(stage2(0), stage2(1))
            for s in range(0, NB, 2):
                if s + 2 < NB:
                    stage1(s + 2)
                    stage1(s + 3)
                stage3(s)
                stage3(s + 1)
                if s + 2 < NB:
                    inter(stage2(s + 2), stage2(s + 3))
            kb.barrier()

    def phase_att_b(self, l):
        kb, ps, L, NB, NR = self.kb, self.ps, self.L, self.NB, self.NR
        kKf = [self.kK[l][g][1] for g in range(3)]
        kVf = [self.kV[l][g][1] for g in range(3)]
        NRING, NTB, OFFB = self.NRING, self.NTB, self.OFFB
        with ExitStack() as es:
            btb = self.sb(es, "btbs", sum(NTB) * 4 * 128, BF16)
            kb.dma("pool", btb[:], self.d_btb, "ab_c", writes=["btbs"])
            kr = [self.sb(es, "bkr%d" % g, NRING[g] * 512, BF16) for g in range(3)]
            vr = [self.sb(es, "bvr%d" % g, NRING[g] * 512, BF16) for g in range(3)]
            qb = [self.sb(es, "bq%d" % i, 12 * 128, BF16) for i in range(2)]
            Pst = [self.sb(es, "bP%d" % i, 512, BF16) for i in range(3)]
            rden = self.sb(es, "brd", 512, F32)
            yst = [self.sb(es, "byst%d" % i, 512, BF16) for i in range(2)]
            pn = 0
            for s in range(NB):
                b = s % 2
                t0 = s * 128
                kb.dma("sp", V(qb[b], 0, [(128, 12), (1, 128)]), self.d_qb[:, :, t0:t0 + 128], "ab_q%d" % b, reads=["qb"],
                       writes=["bq%d" % b])
                for rk in range(NR):
                    j = NR * s + rk
                    for g in range(3):
                        sl = j % NRING[g]
                        kb.dma("sp", V(kr[g], sl * 512, [(128, 4), (1, 128)]),
                               kKf[g][rk * 128:(rk + 1) * 128, :].rearrange("p (h t) -> p h t", h=4)[:, :, t0:t0 + 128],
                               "ab_k%d_%d" % (g, s % 2), reads=["kfull"], writes=["bkr%d_%d" % (g, sl)])
                        kb.dma("sp", V(vr[g], sl * 512, [(1, 512)]), kVf[g][rk * 128:(rk + 1) * 128, s * 512:(s + 1) * 512],
                               "ab_v%d_%d" % (g, s % 2), reads=["kfull"], writes=["bvr%d_%d" % (g, sl)])
                jtop = NR * s + NR - 1
                pairs = [(g, dl) for g in range(3) for dl in range(NTB[g]) if jtop - dl >= 0]
                for idx, (g, dl) in enumerate(pairs):
                    j = jtop - dl
                    sl = j % NRING[g]
                    first, lastp = idx == 0, idx == len(pairs) - 1
                    sb_ = 5 + (pn % 2)
                    sk = "ps%d" % sb_
                    kb.mm(ps[sb_][:], self.ident[:], V(btb, (OFFB[g] + dl) * 512, [(1, 512)]), True, False,
                          reads=["ident", "btbs"], writes=[sk], last=False)
                    for hg in range(4):
                        kb.mm(V(ps[sb_], hg * 128, [(1, 128)]), V(kr[g], sl * 512 + hg * 128, [(1, 128)]),
                              V(qb[b], (g * 4 + hg) * 128, [(1, 128)]), False, hg == 3,
                              reads=["bkr%d_%d" % (g, sl), "bq%d" % b], writes=[sk], last=(hg == 3))
                    pt = Pst[pn % 3]
                    pk_ = "bP%d" % (pn % 3)
                    pn += 1
                    kb.op("act", lambda g_: g_.activation(out=pt[:], in_=ps[sb_][:], func=AF.Exp), reads=[sk], writes=[pk_])
                    for hg in range(4):
                        kb.mm(V(ps[0], hg * 128, [(1, 128)]), V(vr[g], sl * 512 + hg * 128, [(1, 128)]),
                              V(pt, hg * 128, [(1, 128)]), first and hg == 0, lastp, reads=["bvr%d_%d" % (g, sl), pk_], writes=["ps0"],
                              last=(lastp and hg == 3))
                    kb.mm(ps[1][:], self.ones[:], pt[:], first, lastp, reads=["ones", pk_], writes=["ps1"], last=lastp)
                kb.op("dve", lambda g_: g_.reciprocal(out=rden[:], in_=ps[1][:]), reads=["ps1"], writes=["brd"])
                y = yst[s % 2]
                yk = "byst%d" % (s % 2)
                kb.op("dve", lambda g_: g_.tensor_tensor(out=y[:], in0=ps[0][:], in1=rden[:], op=ALU.mult),
                      reads=["ps0", "brd"], writes=[yk])
                kb.dma("sp", self.d_yb[:, :, t0:t0 + 128], V(y, 0, [(128, 4), (1, 128)]), yk, reads=[yk], writes=["ybT"])
            kb.barrier()

    def phase_p4(self, l, xsrc, t0, QT, es):
        kb, ps, L = self.kb, self.ps, self.L
        ya = self.sb(es, "p4ya", 8 * QT, BF16)
        yb = self.sb(es, "p4yb", 4 * QT, BF16)
        mg = self.sb(es, "p4mg", 16 * QT, BF16)
        wa = self.sb(es, "p4wa", 8 * D, BF16)
        wb = self.sb(es, "p4wb", 4 * D, BF16)
        gt = [self.sb(es, "p4g%d" % i, 2 * QT, BF16) for i in range(2)]
        m1 = [self.sb(es, "p4m%d" % i, 512, F32) for i in range(2)]
        xo = [self.sb(es, "p4x%d" % i, 512, F32) for i in range(2)]
        kb.dma("sp", V(ya, 0, [(QT, 8), (1, QT)]), self.d_ya[:, :, t0:t0 + QT], "p4y", reads=["yaT"], writes=["p4ya"])
        kb.dma("sp", V(yb, 0, [(QT, 4), (1, QT)]), self.d_yb[:, :, t0:t0 + QT], "p4y", reads=["ybT"], writes=["p4yb"])
        kb.dma("pool", V(wa, 0, [(D, 8), (1, D)]), self.d_waup[l].rearrange("(h p) c -> p h c", p=128), "p4w", writes=["p4wa"])
        kb.dma("pool", V(wb, 0, [(D, 4), (1, D)]), self.d_wbup[l].rearrange("(h p) c -> p h c", p=128), "p4w", writes=["p4wb"])
        NT = QT // 512
        for oc in range(16):
            gb_ = oc % 2
            gk = "p4g%d" % gb_
            kb.dma("sp", V(gt[gb_], 0, [(1, QT)]), self.d_gates[:, oc, t0:t0 + QT], gk, reads=["gates"], writes=[gk])
            kb.dma("sp", V(gt[gb_], QT, [(1, QT)]), self.d_gates[:, 16 + oc, t0:t0 + QT], gk, reads=["gates"], writes=[gk])
            for ti in range(NT):
                for h in range(8):
                    kb.mm(ps[0][:], V(wa, h * D + oc * 128, [(1, 128)]), V(ya, h * QT + ti * 512, [(1, 512)]), h == 0, h == 7,
                          reads=["p4wa", "p4ya"], writes=["ps0"], last=(h == 7))
                for h in range(4):
                    kb.mm(ps[1][:], V(wb, h * D + oc * 128, [(1, 128)]), V(yb, h * QT + ti * 512, [(1, 512)]), h == 0, h == 3,
                          reads=["p4wb", "p4yb"], writes=["ps1"], last=(h == 3))
                kb.op("dve", lambda g: g.tensor_tensor(out=m1[0][:], in0=ps[0][:], in1=V(gt[gb_], ti * 512, [(1, 512)]), op=ALU.mult),
                      reads=["ps0", gk], writes=["p4m0"])
                kb.op("dve", lambda g: g.tensor_tensor(out=m1[1][:], in0=ps[1][:], in1=V(gt[gb_], QT + ti * 512, [(1, 512)]), op=ALU.mult),
                      reads=["ps1", gk], writes=["p4m1"])
                kb.op("pool", lambda g: g.tensor_tensor(out=V(mg, oc * QT + ti * 512, [(1, 512)]), in0=m1[0][:], in1=m1[1][:], op=ALU.add),
                      reads=["p4m0", "p4m1"], writes=["p4mg"])
        n = 0
        for grp in range(4):
            wv_, wk, s = self.load_w(self.d_wout[l][:, grp * 512:(grp + 1) * 512].rearrange("(k p) c -> p k c", p=128),
                                     [(512, 16), (1, 512)])
            for c in range(4):
                oc = grp * 4 + c
                for ti in range(NT):
                    tt = t0 + ti * 512
                    bank = 2 + n % 2
                    xb = n % 2
                    n += 1
                    xk = "p4x%d" % xb
                    kb.dma("sp", xo[xb][:], xsrc[oc, :, tt:tt + 512], xk, reads=["x_t%d" % (tt // 512)], writes=[xk])
                    for k in range(16):
                        kb.mm(ps[bank][:], V(self.wring[s], k * 512 + c * 128, [(1, 128)]), V(mg, k * QT + ti * 512, [(1, 512)]),
                              k == 0, k == 15, reads=[wk, "p4mg"], writes=["ps%d" % bank], last=(k == 15))
                    kb.op("dve", lambda g: g.scalar_tensor_tensor(out=xo[xb][:], in0=ps[bank][:], scalar=V(self.mod[l], 32 + oc, [(1, 1)]),
                                                                  in1=xo[xb][:], op0=ALU.mult, op1=ALU.add),
                          reads=["ps%d" % bank, xk], writes=[xk])
                    kb.dma("sp", self.d_xs[oc, :, tt:tt + 512], xo[xb][:], xk, reads=[xk], writes=["x_t%d" % (tt // 512)])

    def phase_ffn(self, l, t0, QT, hT, es):
        kb, ps, L = self.kb, self.ps, self.L
        act = self.sb(es, "fact", FC * QT, BF16)
        sg = [self.sb(es, "fsg%d" % i, 512, F32) for i in range(2)]
        xo = [self.sb(es, "fx%d" % i, 512, F32) for i in range(2)]
        NT = QT // 512
        n = 0
        for cg in range(DFF // 256):
            s = self.wr_i % 2
            self.wr_i += 1
            wk = "wring%d" % s
            kb.dma("pool", V(self.wring[s], 0, [(256, 16), (1, 256)]),
                   self.d_wfg[l][:, cg * 256:(cg + 1) * 256].rearrange("(k p) c -> p k c", p=128), wk, writes=[wk])
            kb.dma("pool", V(self.wring[s], 4096, [(256, 16), (1, 256)]),
                   self.d_wfu[l][:, cg * 256:(cg + 1) * 256].rearrange("(k p) c -> p k c", p=128), wk, writes=[wk])
            for c in range(2):
                f = cg * 2 + c
                for ti in range(NT):
                    bg, bu = 2 + 2 * (n % 2), 3 + 2 * (n % 2)
                    sgi = n % 2
                    n += 1
                    for k in range(16):
                        kb.mm(ps[bg][:], V(self.wring[s], k * 256 + c * 128, [(1, 128)]), V(hT, k * QT + ti * 512, [(1, 512)]),
                              k == 0, k == 15, reads=[wk, "hT"], writes=["ps%d" % bg], last=(k == 15))
                    for k in range(16):
                        kb.mm(ps[bu][:], V(self.wring[s], 4096 + k * 256 + c * 128, [(1, 128)]), V(hT, k * QT + ti * 512, [(1, 512)]),
                              k == 0, k == 15, reads=[wk, "hT"], writes=["ps%d" % bu], last=(k == 15))
                    kb.op("act", lambda g: g.activation(out=sg[sgi][:], in_=ps[bg][:], func=AF.Silu), reads=["ps%d" % bg],
                          writes=["fsg%d" % sgi])
                    kb.op("dve", lambda g: g.tensor_tensor(out=V(act, f * QT + ti * 512, [(1, 512)]), in0=ps[bu][:], in1=sg[sgi][:],
                                                           op=ALU.mult), reads=["ps%d" % bu, "fsg%d" % sgi], writes=["fact"])
        n = 0
        for oc in range(16):
            s = self.wr_i % 2
            self.wr_i += 1
            wk = "wring%d" % s
            kb.dma("pool", V(self.wring[s], 0, [(128, FC), (1, 128)]),
                   self.d_wfd[l][:, oc * 128:(oc + 1) * 128].rearrange("(f p) c -> p f c", p=128), wk, writes=[wk])
            for ti in range(NT):
                tt = t0 + ti * 512
                bank = n % 2
                xb = n % 2
                n += 1
                xk = "fx%d" % xb
                kb.dma("sp", xo[xb][:], self.d_xs[oc, :, tt:tt + 512], xk, reads=["x_t%d" % (tt // 512)], writes=[xk])
                for f in range(FC):
                    kb.mm(ps[bank][:], V(self.wring[s], f * 128, [(1, 128)]), V(act, f * QT + ti * 512, [(1, 512)]), f == 0,
                          f == FC - 1, reads=[wk, "fact"], writes=["ps%d" % bank], last=(f == FC - 1))
                kb.op("dve", lambda g: g.scalar_tensor_tensor(out=xo[xb][:], in0=ps[bank][:], scalar=V(self.mod[l], 80 + oc, [(1, 1)]),
                                                              in1=xo[xb][:], op0=ALU.mult, op1=ALU.add),
                      reads=["ps%d" % bank, xk], writes=[xk])
                kb.dma("sp", self.d_xs[oc, :, tt:tt + 512], xo[xb][:], xk, reads=[xk], writes=["x_t%d" % (tt // 512)])

    def build(self):
        kb, L, QT = self.kb, self.L, self.QT
        self.epsb = self.nc.alloc_sbuf_tensor("epsb", [128, 1], F32)
        kb.op("pool", lambda g: g.memset(self.epsb[:], EPS), writes=["epsb"])
        self.phase_const()
        self.phase_mod()
        ph = self.phases
        for l in range(self.NL):
            xsrc = self.d_xT if l == 0 else self.d_xs
            if ph is None or "p1" in ph:
                with ExitStack() as es:
                    QT1 = L
                    hT = self.sb(es, "hT", 16 * QT1, BF16)
                    with ExitStack() as es2:
                        self.norm_tiles(es2, xsrc, self.A1[l], (self.mod[l], 0), 0, QT1, hT)
                        kb.barrier()
                    with ExitStack() as es2:
                        self.phase_p1(l, 0, QT1, hT, es2, "k")
                        kb.barrier()
                    if self.NR > 1:
                        rg = [[c * self.NR + i for i in range(self.NR)] for c in range(8 // self.NR)]
                        for ci, (loc, full) in enumerate([self.kA[l], self.kC[l]] + self.kK[l] + self.kV[l]):
                            kb.cc(lambda g: g.collective_compute("AllGather", ALU.bypass, replica_groups=rg, ins=[loc], outs=[full]),
                                  "ag%d_%d" % (l, ci), reads=["kloc"], writes=["kfull"])
                    with ExitStack() as es2:
                        self.phase_p1(l, 0, QT1, hT, es2, "q")
                        kb.barrier()
            if ph is None or "atta" in ph:
                self.phase_att_a(l)
            if ph is None or "attb" in ph:
                self.phase_att_b(l)
            if ph is None or "p4" in ph:
                for q in range(L // QT):
                    with ExitStack() as es:
                        self.phase_p4(l, xsrc, q * QT, QT, es)
                        kb.barrier()
                    with ExitStack() as es:
                        hT = self.sb(es, "hT", 16 * QT, BF16)
                        with ExitStack() as es2:
                            self.norm_tiles(es2, self.d_xs, self.A2[l], (self.mod[l], 48), q * QT, QT, hT)
                            kb.barrier()
                        self.phase_ffn(l, q * QT, QT, hT, es)
                        kb.barrier()
        toks = []
        with ExitStack() as es:
            toks = self.norm_tiles(es, self.d_xs, self.fg, None, 0, L, None, final=True)
        for t in toks:
            kb._wait("sp", t)
        for nm in self.dbg:
            pass
        kb.barrier()
        return self.nc


def _rel_bucket(n):
    n = np.maximum(n, 0)
    nf = np.maximum(n, 1).astype(np.float32)
    large = 16 + (np.log(nf / np.float32(16)) / np.float32(math.log(2048 / 16)) * np.float32(16)).astype(np.int32)
    return np.where(n < 16, n, np.minimum(large, 31))


def _static_tables(LG, NR, r, rel_bias):
    L = LG // NR
    NB = L // 128
    k = np.arange(128)[:, None]
    q = np.arange(128)[None, :]
    negf = np.float32(NEG)
    sh = NR - 1 - r
    NTA = 17 + NR
    bta = np.full((128, NTA, 8, 128), negf, np.float32)
    for dl in range(NTA):
        d = dl - sh
        if d < 0:
            continue
        dist = min(d, 17) * 128 + q - k
        val = rel_bias[_rel_bucket(dist)][:, :, :8]
        val = np.where((dist >= 0)[:, :, None], val, negf)
        bta[:, dl] = np.transpose(val, (0, 2, 1))
    NTB = [n + NR - 1 for n in NBLK_G]
    btb = np.full((128, sum(NTB), 4, 128), negf, np.float32)
    off = 0
    for g, (win, dil) in enumerate(B_GROUPS):
        for dl in range(NTB[g]):
            d = dl - sh
            if 0 <= d < NBLK_G[g]:
                dist = d * 128 + q - k
                ok = (dist >= 0) & (dist <= win) & (dist % dil == 0)
                val = rel_bias[_rel_bucket(dist)][:, :, 8 + g * 4: 8 + g * 4 + 4]
                val = np.where(ok[:, :, None], val, negf)
                btb[:, off + dl] = np.transpose(val, (0, 2, 1))
        off += NTB[g]
    cm = np.where(np.arange(128)[None, :] <= np.arange(128)[:, None], np.float32(0), np.float32(-1e30)).astype(np.float32)
    cmr = np.zeros((128, NR, 128), np.float32)
    for rk in range(NR):
        if rk == r:
            cmr[:, rk] = cm
        elif rk > r:
            cmr[:, rk] = np.float32(-1e30)
    blocks = np.array([NR * s + r for s in range(NB)])
    pos = (blocks[:, None] * 128 + np.arange(128)[None, :]).astype(np.float32)
    freqs = (np.float32(10000.0) ** (-np.arange(16, dtype=np.float32) / np.float32(16))).astype(np.float32)
    ang = pos[:, :, None] * freqs[None, None, :]
    ropeC = np.cos(ang).astype(np.float32).transpose(1, 0, 2)
    ropeS = np.sin(ang).astype(np.float32).transpose(1, 0, 2)
    return dict(bta=np.ascontiguousarray(bta.reshape(128, -1)), btb=np.ascontiguousarray(btb.reshape(128, -1)),
                cmr=np.ascontiguousarray(cmr.reshape(128, -1)), ropeC=np.ascontiguousarray(ropeC),
                ropeS=np.ascontiguousarray(ropeS), ident=np.eye(128, dtype=np.float32))


def _pk(v):
    v = np.asarray(v, np.float32)
    return np.ascontiguousarray(np.swapaxes(v.reshape(v.shape[:-1] + (-1, 128)), -1, -2))


def make_in_maps(inp, LG, NR, cores):
    f = lambda a: np.ascontiguousarray(np.asarray(a, np.float32))
    shared = dict(
        w_ada=f(inp["w_ada"]), b_adaT=_pk(inp["b_ada"]), n1g=_pk(inp["norm1_g"]), n2g=_pk(inp["norm2_g"]), fg=_pk(inp["final_g"]),
        w_in=f(inp["w_in"]),
        kvg=np.ascontiguousarray(np.broadcast_to(f(inp["kv_norm_g"])[:, None, :], (len(inp["kv_norm_g"]), 128, 256))),
        ilg=np.ascontiguousarray(np.broadcast_to(f(inp["idx_ln_g"])[:, None, :], (len(inp["idx_ln_g"]), 128, 64))),
        ilb=np.ascontiguousarray(np.broadcast_to(f(inp["idx_ln_b"])[:, None, :], (len(inp["idx_ln_b"]), 128, 64))),
        w_uk=f(inp["w_uk"]), w_uv=f(inp["w_uv"]), w_a_up=f(inp["w_a_up"]), w_b_up=f(inp["w_b_up"]), w_out=f(inp["w_out"]),
        w_ffg=f(inp["w_ff_gate"]), w_ffu=f(inp["w_ff_up"]), w_ffd=f(inp["w_ff_down"]),
    )
    tabs = [_static_tables(LG, NR, r, f(inp["rel_bias"])) for r in range(NR)]
    x = f(inp["x"])
    c = f(inp["c"])
    L = LG // NR
    maps = []
    for b, r in cores:
        m = dict(shared)
        m.update(tabs[r])
        xb = x[b].reshape(LG // 128, 128, D)[r::NR].reshape(L, D)
        m["xT"] = np.ascontiguousarray(xb.T.reshape(KC, 128, L))
        m["c_pk"] = _pk(c[b])
        maps.append(m)
    return maps


_CACHE = {}
NR_FULL = 2


def kernel(**inputs):
    x = np.asarray(inputs["x"])
    B, LG, _ = x.shape
    nl = np.asarray(inputs["w_ada"]).shape[0]
    NR = NR_FULL
    key = (LG, nl, NR)
    if key not in _CACHE:
        _CACHE[key] = Prog(LG, nl, NR=NR).build()
    nc = _CACHE[key]
    cores = [((c // NR) % B, c % NR) for c in range(8)]
    in_maps = make_in_maps(inputs, LG, NR, cores)
    res = run_bass_kernel_spmd(nc, in_maps, core_ids=list(range(8)))
    L = LG // NR
    out = np.empty((B, LG // 128, 128, D), np.float32)
    for c, (b, r) in enumerate(cores):
        if c // NR >= B:
            continue
        o = np.asarray(res.results[c]["outT"]).reshape(D, L).T
        out[b, r::NR] = o.reshape(L // 128, 128, D)
    return out.reshape(B, LG, D)
```

```python
import math
from contextlib import ExitStack
import numpy as np
import concourse.bass as bass
import concourse.mybir as mybir
from concourse.bass_utils import run_bass_kernel_spmd

F32 = mybir.dt.float32
BF16 = mybir.dt.bfloat16
ALU = mybir.AluOpType
AF = mybir.ActivationFunctionType

D = 2048
KC = 16
NIN = 11088
DFF = 5632
FC = 44
NEG = -30000.0
EPS = 1e-6
C_QA, C_CKV, C_QI, C_KI, C_WI, C_QB, C_KB, C_VB, C_GATE = 0, 1024, 1280, 2304, 2368, 2384, 3920, 5456, 6992
B_GROUPS = ((128, 1), (512, 4), (2048, 16))
NBLK_G = (2, 5, 17)
OFF_G = (0, 2, 7)
NRING = (3, 6, 18)


class KB:
    def __init__(self, nc):
        self.nc = nc
        self.eng = {"pe": nc.tensor, "dve": nc.vector, "act": nc.scalar, "pool": nc.gpsimd, "sp": nc.sync}
        self.esem = {e: nc.alloc_semaphore("prog_" + e) for e in ("pe", "dve", "act", "pool")}
        self.cnt = {e: 0 for e in self.esem}
        self.dsem, self.dcnt = {}, {}
        self.waited, self.lastw, self.readers = {}, {}, {}
        self.ninst = 0

    def _sem(self, key):
        return self.esem[key[1]] if key[0] == "e" else self.dsem[key[1]]

    def _wait(self, e, tok):
        if tok is None:
            return
        key, val = tok
        if key == ("e", "pe") and e == "pe":
            return
        if key[0] == "d":
            val = self.dcnt[key[1]]
        k = (e, key)
        if self.waited.get(k, 0) >= val:
            return
        self.waited[k] = val
        self.eng[e].wait_ge(self._sem(key), val)
        self.ninst += 1

    def _deps(self, e, reads, writes):
        for r in reads:
            self._wait(e, self.lastw.get(r))
        for w in writes:
            self._wait(e, self.lastw.get(w))
            for t in self.readers.get(w, ()):
                self._wait(e, t)

    def _commit(self, tok, reads, writes):
        for r in reads:
            lst = self.readers.setdefault(r, [])
            lst.append(tok)
            if len(lst) > 12:
                best = {}
                for k, v in lst:
                    best[k] = max(best.get(k, 0), v)
                self.readers[r] = list(best.items())
        for w in writes:
            self.lastw[w] = tok
            self.readers[w] = []

    def op(self, e, fn, reads=(), writes=()):
        self._deps(e, reads, writes)
        ins = fn(self.eng[e])
        self.cnt[e] += 1
        ins.then_inc(self.esem[e], 1)
        tok = (("e", e), self.cnt[e])
        self._commit(tok, reads, writes)
        self.ninst += 1
        return tok

    def mm(self, out, lhsT, rhs, start, stop, reads=(), writes=(), last=True):
        e = "pe"
        self._deps(e, reads, writes)
        ins = self.nc.tensor.matmul(out, lhsT, rhs, start=start, stop=stop)
        self.ninst += 1
        if last:
            self.cnt[e] += 1
            ins.then_inc(self.esem[e], 1)
            tok = (("e", e), self.cnt[e])
        else:
            tok = (("e", e), self.cnt[e] + 1)
        self._commit(tok, reads, writes)
        return tok

    def dma(self, q, out, in_, slot, reads=(), writes=()):
        if slot not in self.dsem:
            self.dsem[slot] = self.nc.alloc_semaphore("d_" + slot)
            self.dcnt[slot] = 0
        self._deps(q, reads, writes)
        ins = self.eng[q].dma_start(out=out, in_=in_)
        self.dcnt[slot] += 16
        ins.then_inc(self.dsem[slot], 16)
        tok = (("d", slot), self.dcnt[slot])
        self._commit(tok, reads, writes)
        self.ninst += 1
        return tok

    def cc(self, fn, name, reads=(), writes=()):
        self.dsem[name] = self.nc.alloc_semaphore("cc_" + name)
        self._deps("pool", reads, writes)
        ins = fn(self.eng["pool"])
        ins.then_inc(self.dsem[name], 1)
        self.dcnt[name] = 1
        tok = (("d", name), 1)
        self._commit(tok, reads, writes)
        self.ninst += 1
        return tok

    def barrier(self):
        for e in self.eng:
            for e2 in self.esem:
                if e2 != e and self.cnt[e2] > 0:
                    self._wait(e, (("e", e2), self.cnt[e2]))
            for s, v in self.dcnt.items():
                if v > 0:
                    self._wait(e, (("d", s), v))
        self.lastw.clear()
        self.readers.clear()


def V(t, off, dims, p0=0, npart=128):
    base = t[:]
    ps = base.ap[0][0]
    return bass.AP(base.tensor, base.offset + p0 * ps + off, [[ps, npart]] + [[int(s), int(c)] for s, c in dims])


class Prog:
    def __init__(self, LG, nl=2, dbg=(), phases=None, NR=1):
        L = LG // NR
        self.LG, self.NR = LG, NR
        self.L, self.NB, self.NL = L, L // 128, nl
        self.NTA = 17 + NR
        self.NTB = [n + NR - 1 for n in NBLK_G]
        self.OFFB = [0, self.NTB[0], self.NTB[0] + self.NTB[1]]
        self.NRING = [n - 1 + 2 * NR for n in NBLK_G]
        self.O_KIT, self.O_KB, self.O_CKVA = 2 * L, 3 * L, 15 * L
        self.O_VB = 15 * L + (L // 128) * 256
        self.KCOLS = 15 * L + (L // 128) * 1792
        self.QT = min(1024, L)
        self.dbg = set(dbg)
        self.phases = phases
        nc = self.nc = bass.Bass("TRN2", target_bir_lowering=False)
        self.kb = KB(nc)
        self.ev_i = 0
        self.wr_i = 0
        NB = self.NB

        def din(name, shape):
            return nc.dram_tensor(name, list(shape), F32, kind="ExternalInput").ap()

        def dscr(name, shape, dt):
            kind = "ExternalOutput" if name in self.dbg else "Internal"
            return nc.dram_tensor(name, list(shape), dt, kind=kind).ap()

        self.d_xT = din("xT", [KC, 128, L])
        self.d_c = din("c_pk", [128, KC])
        self.d_wada = din("w_ada", [nl, D, 6 * D])
        self.d_bada = din("b_adaT", [nl, 128, 96])
        self.d_n1g = din("n1g", [nl, 128, KC])
        self.d_n2g = din("n2g", [nl, 128, KC])
        self.d_fg = din("fg", [128, KC])
        self.d_win = din("w_in", [nl, D, NIN])
        self.d_kvg = din("kvg", [nl, 128, 256])
        self.d_ilg = din("ilg", [nl, 128, 64])
        self.d_ilb = din("ilb", [nl, 128, 64])
        self.d_wuk = din("w_uk", [nl, 8, 128, 256])
        self.d_wuv = din("w_uv", [nl, 8, 256, 128])
        self.d_waup = din("w_a_up", [nl, 1024, D])
        self.d_wbup = din("w_b_up", [nl, 512, D])
        self.d_wout = din("w_out", [nl, D, D])
        self.d_wfg = din("w_ffg", [nl, D, DFF])
        self.d_wfu = din("w_ffu", [nl, D, DFF])
        self.d_wfd = din("w_ffd", [nl, DFF, D])
        self.d_ropeC = din("ropeC", [128, NB, 16])
        self.d_ropeS = din("ropeS", [128, NB, 16])
        self.d_bta = din("bta", [128, self.NTA * 8 * 128])
        self.d_btb = din("btb", [128, sum(self.NTB) * 4 * 128])
        self.d_cm = din("cmr", [128, NR * 128])
        self.d_ident = din("ident", [128, 128])
        self.d_out = nc.dram_tensor("outT", [KC, 128, L], F32, kind="ExternalOutput").ap()
        self.d_xs = dscr("xs", [KC, 128, L], F32)
        self.d_qlat = dscr("qlat", [128, 16, L], BF16)
        self.d_qiT = dscr("qiT", [128, 8, L], BF16)
        self.d_wi = dscr("wi", [NB, 128, 16], F32)
        self.d_qb = dscr("qb", [128, 12, L], BF16)
        self.d_gates = dscr("gates", [128, 32, L], BF16)
        def kt(name, cols):
            loc = dscr(name + "_l", [128, cols], BF16)
            full = loc if NR == 1 else dscr(name + "_f", [NR * 128, cols], BF16)
            return loc, full
        self.kA = [kt("kA%d" % l, 3 * L) for l in range(nl)]
        self.kC = [kt("kC%d" % l, NB * 256) for l in range(nl)]
        self.kK = [[kt("kK%d_%d" % (l, g), 4 * L) for g in range(3)] for l in range(nl)]
        self.kV = [[kt("kV%d_%d" % (l, g), NB * 512) for g in range(3)] for l in range(nl)]
        self.d_ya = dscr("yaT", [128, 8, L], BF16)
        self.d_yb = dscr("ybT", [128, 4, L], BF16)

        def sbp(name, f, dt):
            return nc.alloc_sbuf_tensor(name, [128, f], dt)

        self.ident = sbp("identb", 128, BF16)
        self.ones = sbp("onesb", 128, BF16)
        self.cm = sbp("cmf", NR * 128, F32)
        self.ropeC = sbp("ropeCs", NB * 16, F32)
        self.ropeS = sbp("ropeSs", NB * 16, F32)
        self.mod = [sbp("mod%d" % l, 96, F32) for l in range(nl)]
        self.A1 = [sbp("A1_%d" % l, 16, F32) for l in range(nl)]
        self.A2 = [sbp("A2_%d" % l, 16, F32) for l in range(nl)]
        self.fg = sbp("fgs", 16, F32)
        self.wring = [sbp("wring%d" % i, 8192, BF16) for i in range(2)]
        self.ps = [nc.alloc_psum_tensor("psb%d" % i, [128, 512], F32) for i in range(8)]

    def sb(self, es, name, f, dt):
        self.sb_n = getattr(self, "sb_n", 0) + 1
        return es.enter_context(self.nc.sbuf_tensor("s%d_%s" % (self.sb_n, name), [128, f], dt))

    def evq(self):
        self.ev_i += 1
        return "act" if self.ev_i % 2 else "dve"

    def evac(self, out, in_, scale, reads, writes, eng=None):
        e = eng or self.evq()
        if e == "act":
            return self.kb.op("act", lambda g: g.activation(out=out, in_=in_, func=AF.Identity, scale=float(scale)),
                              reads=reads, writes=writes)
        if scale == 1.0:
            return self.kb.op("dve", lambda g: g.tensor_copy(out=out, in_=in_), reads=reads, writes=writes)
        return self.kb.op("dve", lambda g: g.tensor_scalar(out=out, in0=in_, scalar1=float(scale), scalar2=None,
                                                            op0=ALU.mult), reads=reads, writes=writes)

    def load_w(self, dram_ap, dims):
        s = self.wr_i % 2
        self.wr_i += 1
        key = "wring%d" % s
        view = V(self.wring[s], 0, dims)
        self.kb.dma("pool", view, dram_ap, key, writes=[key])
        return view, key, s

    def wslice(self, s, off, dims):
        return V(self.wring[s], off, dims)

    def phase_const(self):
        kb = self.kb
        kb.dma("pool", self.ident[:], self.d_ident, "c_ident", writes=["ident"])
        kb.op("pool", lambda g: g.memset(self.ones[:], 1.0), writes=["ones"])
        kb.dma("sp", self.cm[:], self.d_cm, "c_misc", writes=["cm"])
        kb.dma("sp", self.ropeC[:], self.d_ropeC.rearrange("p b f -> p (b f)"), "c_misc", writes=["ropeC"])
        kb.dma("sp", self.ropeS[:], self.d_ropeS.rearrange("p b f -> p (b f)"), "c_misc", writes=["ropeS"])
        kb.dma("sp", self.fg[:], self.d_fg, "c_misc", writes=["fg"])

    def phase_mod(self):
        kb, ps = self.kb, self.ps
        with ExitStack() as es:
            sc = self.sb(es, "sc", 16, F32)
            scb = self.sb(es, "scb", 16, BF16)
            bada = self.sb(es, "bada", 96, F32)
            ng = self.sb(es, "ng", 32, F32)
            W = [self.sb(es, "wada%d" % i, 16 * 1536, BF16) for i in range(2)]
            kb.dma("sp", sc[:], self.d_c, "m_c", writes=["sc"])
            kb.op("act", lambda g: g.activation(out=scb[:], in_=sc[:], func=AF.Silu), reads=["sc"], writes=["scb"])
            n = 0
            for l in range(self.NL):
                for cb in range(8):
                    s = n % 2
                    n += 1
                    wk = "wada%d" % s
                    kb.dma("pool", V(W[s], 0, [(1536, 16), (1, 1536)]),
                           self.d_wada[l, :, cb * 1536:(cb + 1) * 1536].rearrange("(k p) c -> p k c", p=128),
                           wk, writes=[wk])
                    for jj in range(12):
                        j = cb * 12 + jj
                        for k in range(16):
                            kb.mm(V(ps[0], j, [(1, 1)]), V(W[s], k * 1536 + jj * 128, [(1, 128)]),
                                  V(scb, k, [(1, 1)]), k == 0, k == 15, reads=[wk, "scb"], writes=["ps0"],
                                  last=(k == 15))
                kb.dma("sp", bada[:], self.d_bada[l], "m_b", writes=["bada"])
                kb.dma("sp", V(ng, 0, [(1, 16)]), self.d_n1g[l], "m_b", writes=["ng"])
                kb.dma("sp", V(ng, 16, [(1, 16)]), self.d_n2g[l], "m_b", writes=["ng"])
                mod = self.mod[l]
                kb.op("dve", lambda g: g.tensor_tensor(out=mod[:], in0=V(ps[0], 0, [(1, 96)]), in1=bada[:], op=ALU.add),
                      reads=["ps0", "bada"], writes=["mod%d" % l])
                kb.op("dve", lambda g: g.scalar_tensor_tensor(out=self.A1[l][:], in0=V(mod, 16, [(1, 16)]), scalar=1.0,
                                                              in1=V(ng, 0, [(1, 16)]), op0=ALU.add, op1=ALU.mult),
                      reads=["mod%d" % l, "ng"], writes=["A1_%d" % l])
                kb.op("dve", lambda g: g.scalar_tensor_tensor(out=self.A2[l][:], in0=V(mod, 64, [(1, 16)]), scalar=1.0,
                                                              in1=V(ng, 16, [(1, 16)]), op0=ALU.add, op1=ALU.mult),
                      reads=["mod%d" % l, "ng"], writes=["A2_%d" % l])
            kb.barrier()

    def norm_tiles(self, es, xsrc, A, shift, t0, QT, hT, final=False):
        kb, ps = self.kb, self.ps
        xt = [self.sb(es, "nx%d" % i, 16 * 512, F32) for i in range(2)]
        sq = self.sb(es, "nsq", 16 * 512, BF16)
        rs = self.sb(es, "nrs", 512, F32)
        toks = []
        for ti in range(QT // 512):
            b = ti % 2
            tt = t0 + ti * 512
            xk = "nx%d" % b
            kb.dma("sp", V(xt[b], 0, [(512, 16), (1, 512)]), xsrc[:, :, tt:tt + 512].rearrange("k p t -> p k t"),
                   xk, reads=["x_t%d" % (tt // 512)], writes=[xk])
            kb.op("act", lambda g: g.activation(out=sq[:], in_=xt[b][:], func=AF.Square), reads=[xk], writes=["nsq"])
            for k in range(16):
                kb.mm(ps[1][:], self.ones[:], V(sq, k * 512, [(1, 512)]), k == 0, k == 15, reads=["nsq", "ones"],
                      writes=["ps1"], last=(k == 15))
            kb.op("act", lambda g: g.activation(out=rs[:], in_=ps[1][:], func=AF.Sqrt, scale=1.0 / D, bias=self.epsb[:]),
                  reads=["ps1"], writes=["nrs"])
            kb.op("dve", lambda g: g.reciprocal(out=rs[:], in_=rs[:]), reads=["nrs"], writes=["nrs"])
            kb.op("dve", lambda g: g.tensor_tensor(out=V(xt[b], 0, [(512, 16), (1, 512)]),
                                                   in0=V(xt[b], 0, [(512, 16), (1, 512)]),
                                                   in1=V(rs, 0, [(0, 16), (1, 512)]), op=ALU.mult),
                  reads=[xk, "nrs"], writes=[xk])
            if final:
                for k in range(16):
                    kb.op("dve" if k % 2 else "act",
                          (lambda g: g.tensor_scalar(out=V(xt[b], k * 512, [(1, 512)]), in0=V(xt[b], k * 512, [(1, 512)]),
                                                     scalar1=V(A, k, [(1, 1)]), scalar2=None, op0=ALU.mult)) if k % 2 else
                          (lambda g: g.activation(out=V(xt[b], k * 512, [(1, 512)]), in_=V(xt[b], k * 512, [(1, 512)]),
                                                  func=AF.Identity, scale=V(A, k, [(1, 1)]))),
                          reads=[xk, "fg"], writes=[xk])
                toks.append(kb.dma("sp", self.d_out[:, :, tt:tt + 512].rearrange("k p t -> p k t"),
                                   V(xt[b], 0, [(512, 16), (1, 512)]), "outst", reads=[xk]))
                continue
            for k in range(16):
                o = V(hT, k * QT + ti * 512, [(1, 512)])
                i_ = V(xt[b], k * 512, [(1, 512)])
                if k % 2:
                    kb.op("dve", lambda g: g.tensor_scalar(out=o, in0=i_, scalar1=V(A, k, [(1, 1)]),
                                                           scalar2=V(shift[0], shift[1] + k, [(1, 1)]),
                                                           op0=ALU.mult, op1=ALU.add), reads=[xk], writes=["hT"])
                else:
                    kb.op("act", lambda g: g.activation(out=o, in_=i_, func=AF.Identity, scale=V(A, k, [(1, 1)]),
                                                        bias=V(shift[0], shift[1] + k, [(1, 1)])), reads=[xk], writes=["hT"])
        return toks

    def fm_proj(self, hT, QT, t0, wv, wk, c0, ncol, out_cb):
        kb, ps = self.kb, self.ps
        for c in range(ncol // 128):
            for ti in range(QT // 512):
                bank = 2 + (self.bank_i % 2)
                self.bank_i += 1
                pk = "ps%d" % bank
                for k in range(16):
                    kb.mm(ps[bank][:], V(wv[0], wv[1] + k * wv[2] + c * 128, [(1, 128)]),
                          V(hT, k * QT + ti * 512, [(1, 512)]), k == 0, k == 15, reads=[wk, "hT"], writes=[pk],
                          last=(k == 15))
                out_cb(c0 + c, ti, bank)

    def phase_p1(self, l, t0, QT, hT, es, part):
        kb, ps, L = self.kb, self.ps, self.L
        self.bank_i = 0
        win = self.d_win[l]
        self.d_ckvT = self.kA[l][0][:, 0:2 * L].rearrange("p (c t) -> p c t", c=2)
        self.d_kiT = self.kA[l][0][:, 2 * L:3 * L]
        self.d_ckvA = self.kC[l][0].rearrange("p (b c) -> b p c", c=256)
        kKl = [self.kK[l][g][0].rearrange("p (h t) -> p h t", h=4) for g in range(3)]
        kVl = [self.kV[l][g][0] for g in range(3)]
        stg = [self.sb(es, "p1stg%d" % i, 512, BF16) for i in range(4)]
        self.stg_i = 0

        def nstg():
            i = self.stg_i % 4
            self.stg_i += 1
            return stg[i], "p1stg%d" % i

        def wcols(c0, n):
            return win[:, c0:c0 + n].rearrange("(k p) c -> p k c", p=128)

        if part == "q":
            self._p1_q(l, t0, QT, hT, es, wcols, nstg)
        else:
            self._p1_k(l, t0, QT, hT, es, wcols, nstg, kKl, kVl)

    def _p1_q(self, l, t0, QT, hT, es, wcols, nstg):
        kb, ps, L = self.kb, self.ps, self.L
        wuk = self.sb(es, "wuk", 8 * 256, BF16)
        kb.dma("pool", V(wuk, 0, [(256, 8), (1, 256)]), self.d_wuk[l].rearrange("h d c -> d h c"), "p1wuk", writes=["wuk"])
        qa = [self.sb(es, "qa%d" % i, 512, BF16) for i in range(2)]
        self.qa_i = 0
        for grp in range(2):
            wv_, wk, s = self.load_w(wcols(C_QA + grp * 512, 512), [(512, 16), (1, 512)])

            def cb_qa(h, ti, bank):
                qi = self.qa_i % 2
                self.qa_i += 1
                qk = "qa%d" % qi
                self.evac(qa[qi][:], ps[bank][:], 1.0, ["ps%d" % bank], [qk])
                for cc in range(2):
                    b2 = 4 + cc
                    kb.mm(ps[b2][:], V(wuk, h * 256 + cc * 128, [(1, 128)]), qa[qi][:], True, True,
                          reads=["wuk", qk], writes=["ps%d" % b2])
                    st, sk = nstg()
                    self.evac(st[:], ps[b2][:], 128 ** -0.5, ["ps%d" % b2], [sk])
                    tt = t0 + ti * 512
                    kb.dma("sp", self.d_qlat[:, cc * 8 + h, tt:tt + 512], st[:], sk, reads=[sk], writes=["qlat"])

            self.fm_proj(hT, QT, t0, (self.wring[s], 0, 512), wk, grp * 4, 512, cb_qa)

        self._p1_fm(t0, QT, hT, wcols, nstg, C_QB, self.d_qb, 128 ** -0.5, "qb", None)
        self._p1_gates_qi(l, t0, QT, hT, es, wcols, nstg)

    def _p1_fm(self, t0, QT, hT, wcols, nstg, cbase, dst, scl, dk, kKl):
        kb, ps = self.kb, self.ps
        if True:
            for grp in range(3):
                wv_, wk, s = self.load_w(wcols(cbase + grp * 512, 512), [(512, 16), (1, 512)])

                def cb_b(h, ti, bank, dst=dst, scl=scl, dk=dk):
                    st, sk = nstg()
                    self.evac(st[:], ps[bank][:], scl, ["ps%d" % bank], [sk])
                    tt = t0 + ti * 512
                    dap = dst[:, h, tt:tt + 512] if dst is not None else kKl[h // 4][:, h % 4, tt:tt + 512]
                    kb.dma("sp", dap, st[:], sk, reads=[sk], writes=[dk])

                self.fm_proj(hT, QT, t0, (self.wring[s], 0, 512), wk, grp * 4, 512, cb_b)

    def _p1_gates_qi(self, l, t0, QT, hT, es, wcols, nstg):
        kb, ps, L = self.kb, self.ps, self.L
        for grp in range(8):
            wv_, wk, s = self.load_w(wcols(C_GATE + grp * 512, 512), [(512, 16), (1, 512)])

            def cb_g(c, ti, bank):
                st, sk = nstg()
                kb.op("act", lambda g: g.activation(out=st[:], in_=ps[bank][:], func=AF.Sigmoid),
                      reads=["ps%d" % bank], writes=[sk])
                tt = t0 + ti * 512
                kb.dma("sp", self.d_gates[:, c, tt:tt + 512], st[:], sk, reads=[sk], writes=["gates"])

            self.fm_proj(hT, QT, t0, (self.wring[s], 0, 512), wk, grp * 4, 512, cb_g)

        self._p1_qi(l, t0, QT, hT, es, wcols)

    def _p1_k(self, l, t0, QT, hT, es, wcols, nstg, kKl, kVl):
        kb, ps, L = self.kb, self.ps, self.L
        self._p1_fm(t0, QT, hT, wcols, nstg, C_KB, None, 1.0, "kbT", kKl)
        vst = [self.sb(es, "vst%d" % i, 512, BF16) for i in range(2)]
        n = 0
        for grp in range(3):
            wv_, wk, s = self.load_w(wcols(C_VB + grp * 512, 512), [(512, 16), (1, 512)])
            for bi in range(QT // 128):
                blk = t0 // 128 + bi
                bank = 2 + (self.bank_i % 2)
                self.bank_i += 1
                for k in range(16):
                    kb.mm(ps[bank][:], V(hT, k * QT + bi * 128, [(1, 128)]), V(self.wring[s], k * 512, [(1, 512)]),
                          k == 0, k == 15, reads=[wk, "hT"], writes=["ps%d" % bank], last=(k == 15))
                v = vst[n % 2]
                vk = "vst%d" % (n % 2)
                n += 1
                self.evac(v[:], ps[bank][:], 1.0, ["ps%d" % bank], [vk])
                kb.dma("sp", kVl[grp][:, blk * 512:(blk + 1) * 512], v[:], vk, reads=[vk], writes=["vb"])

        kvg = self.sb(es, "kvg", 256, F32)
        ilg = self.sb(es, "ilg", 128, F32)
        kb.dma("sp", kvg[:], self.d_kvg[l], "p1c", writes=["kvg"])
        kb.dma("sp", V(ilg, 0, [(1, 64)]), self.d_ilg[l], "p1c", writes=["ilg"])
        kb.dma("sp", V(ilg, 64, [(1, 64)]), self.d_ilb[l], "p1c", writes=["ilg"])
        s = self.wr_i % 2
        self.wr_i += 1
        wk = "wring%d" % s
        kb.dma("pool", V(self.wring[s], 0, [(336, 16), (1, 256)]), wcols(C_CKV, 256), wk, writes=[wk])
        kb.dma("pool", V(self.wring[s], 256, [(336, 16), (1, 80)]), wcols(C_KI, 80), wk, writes=[wk])
        junk = self.sb(es, "junk", 256, F32)
        st8 = self.sb(es, "st8", 16, F32)
        ckvb = [self.sb(es, "ckvb%d" % i, 256, BF16) for i in range(2)]
        ckt = [self.sb(es, "ckt%d" % i, 256, BF16) for i in range(2)]
        kn = self.sb(es, "kn", 64, F32)
        kt = self.sb(es, "ktmp", 64, F32)
        kr = [self.sb(es, "kr%d" % i, 128, BF16) for i in range(2)]
        kit = [self.sb(es, "kit%d" % i, 128, BF16) for i in range(2)]
        wis = [self.sb(es, "wis%d" % i, 16, F32) for i in range(2)]
        for bi in range(QT // 128):
            blk = t0 // 128 + bi
            p = bi % 2
            bank = 2 + (self.bank_i % 2)
            self.bank_i += 1
            pk = "ps%d" % bank
            for k in range(16):
                kb.mm(V(ps[bank], 0, [(1, 336)]), V(hT, k * QT + bi * 128, [(1, 128)]), V(self.wring[s], k * 336, [(1, 336)]),
                      k == 0, k == 15, reads=[wk, "hT"], writes=[pk], last=(k == 15))
            kb.op("act", lambda g: g.activation(out=junk[:], in_=V(ps[bank], 0, [(1, 256)]), func=AF.Square),
                  reads=[pk], writes=["junk"])
            kb.op("dve", lambda g: g.reduce_sum(out=V(st8, 0, [(1, 1)]), in_=junk[:], axis=mybir.AxisListType.X),
                  reads=["junk"], writes=["st8a"])
            kb.op("act", lambda g: g.activation(out=V(st8, 1, [(1, 1)]), in_=V(st8, 0, [(1, 1)]), func=AF.Sqrt,
                                                scale=1.0 / 256, bias=self.epsb[:]), reads=["st8a"], writes=["st8b"])
            kb.op("dve", lambda g: g.reciprocal(out=V(st8, 2, [(1, 1)]), in_=V(st8, 1, [(1, 1)])), reads=["st8b"], writes=["st8c"])
            cb_, ck = ckvb[p], "ckvb%d" % p
            kb.op("dve", lambda g: g.scalar_tensor_tensor(out=cb_[:], in0=V(ps[bank], 0, [(1, 256)]), scalar=V(st8, 2, [(1, 1)]),
                                                          in1=kvg[:], op0=ALU.mult, op1=ALU.mult),
                  reads=[pk, "st8c", "kvg"], writes=[ck])
            kb.dma("sp", self.d_ckvA[blk], cb_[:], ck, reads=[ck], writes=["ckvA"])
            for cc in range(2):
                kb.mm(V(ps[6], cc * 128, [(1, 128)]), V(cb_, cc * 128, [(1, 128)]), self.ident[:], True, True,
                      reads=[ck, "ident"], writes=["ps6"])
            ct, ctk = ckt[p], "ckt%d" % p
            self.evac(ct[:], V(ps[6], 0, [(1, 256)]), 1.0, ["ps6"], [ctk])
            kb.dma("sp", self.d_ckvT[:, :, blk * 128:(blk + 1) * 128], V(ct, 0, [(128, 2), (1, 128)]), ctk,
                   reads=[ctk], writes=["ckvT"])
            kb.op("dve", lambda g: g.reduce_sum(out=V(st8, 3, [(1, 1)]), in_=V(ps[bank], 256, [(1, 64)]), axis=mybir.AxisListType.X),
                  reads=[pk], writes=["st8d"])
            kb.op("act", lambda g: g.activation(out=V(junk, 0, [(1, 64)]), in_=V(ps[bank], 256, [(1, 64)]), func=AF.Square),
                  reads=[pk], writes=["junk"])
            kb.op("dve", lambda g: g.reduce_sum(out=V(st8, 4, [(1, 1)]), in_=V(junk, 0, [(1, 64)]), axis=mybir.AxisListType.X),
                  reads=["junk"], writes=["st8e"])
            kb.op("dve", lambda g: g.tensor_scalar(out=V(st8, 5, [(1, 1)]), in0=V(st8, 3, [(1, 1)]), scalar1=1.0 / 64, scalar2=None,
                                                   op0=ALU.mult), reads=["st8d"], writes=["st8f"])
            kb.op("dve", lambda g: g.tensor_tensor(out=V(st8, 6, [(1, 1)]), in0=V(st8, 5, [(1, 1)]), in1=V(st8, 5, [(1, 1)]),
                                                   op=ALU.mult), reads=["st8f"], writes=["st8g"])
            kb.op("dve", lambda g: g.scalar_tensor_tensor(out=V(st8, 7, [(1, 1)]), in0=V(st8, 4, [(1, 1)]), scalar=1.0 / 64,
                                                          in1=V(st8, 6, [(1, 1)]), op0=ALU.mult, op1=ALU.subtract),
                  reads=["st8e", "st8g"], writes=["st8h"])
            kb.op("act", lambda g: g.activation(out=V(st8, 8, [(1, 1)]), in_=V(st8, 7, [(1, 1)]), func=AF.Sqrt,
                                                bias=self.epsb[:]), reads=["st8h"], writes=["st8i"])
            kb.op("dve", lambda g: g.reciprocal(out=V(st8, 9, [(1, 1)]), in_=V(st8, 8, [(1, 1)])), reads=["st8i"], writes=["st8j"])
            kb.op("dve", lambda g: g.tensor_scalar(out=kn[:], in0=V(ps[bank], 256, [(1, 64)]), scalar1=V(st8, 5, [(1, 1)]),
                                                   scalar2=V(st8, 9, [(1, 1)]), op0=ALU.subtract, op1=ALU.mult),
                  reads=[pk, "st8f", "st8j"], writes=["kn"])
            kb.op("dve", lambda g: g.tensor_tensor(out=kn[:], in0=kn[:], in1=V(ilg, 0, [(1, 64)]), op=ALU.mult),
                  reads=["kn", "ilg"], writes=["kn"])
            kb.op("dve", lambda g: g.tensor_tensor(out=kn[:], in0=kn[:], in1=V(ilg, 64, [(1, 64)]), op=ALU.add),
                  reads=["kn", "ilg"], writes=["kn"])
            cosb = V(self.ropeC, blk * 16, [(1, 16)])
            sinb = V(self.ropeS, blk * 16, [(1, 16)])
            x1, x2 = V(kn, 0, [(1, 16)]), V(kn, 16, [(1, 16)])
            kb.op("dve", lambda g: g.tensor_tensor(out=V(kt, 0, [(1, 16)]), in0=x1, in1=cosb, op=ALU.mult), reads=["kn", "ropeC"], writes=["kt0"])
            kb.op("dve", lambda g: g.tensor_tensor(out=V(kt, 16, [(1, 16)]), in0=x2, in1=sinb, op=ALU.mult), reads=["kn", "ropeS"], writes=["kt1"])
            kb.op("dve", lambda g: g.tensor_tensor(out=V(kt, 32, [(1, 16)]), in0=x1, in1=sinb, op=ALU.mult), reads=["kn", "ropeS"], writes=["kt2"])
            kb.op("dve", lambda g: g.tensor_tensor(out=V(kt, 48, [(1, 16)]), in0=x2, in1=cosb, op=ALU.mult), reads=["kn", "ropeC"], writes=["kt3"])
            krp, krk = kr[p], "kr%d" % p
            for half in range(2):
                kb.op("dve", lambda g: g.tensor_tensor(out=V(krp, half * 64, [(1, 16)]), in0=V(kt, 0, [(1, 16)]), in1=V(kt, 16, [(1, 16)]),
                                                       op=ALU.subtract), reads=["kt0", "kt1"], writes=[krk])
                kb.op("dve", lambda g: g.tensor_tensor(out=V(krp, half * 64 + 16, [(1, 16)]), in0=V(kt, 32, [(1, 16)]), in1=V(kt, 48, [(1, 16)]),
                                                       op=ALU.add), reads=["kt2", "kt3"], writes=[krk])
                kb.op("dve", lambda g: g.tensor_copy(out=V(krp, half * 64 + 32, [(1, 32)]), in_=V(kn, 32, [(1, 32)])),
                      reads=["kn"], writes=[krk])
            kb.mm(V(ps[7], 0, [(1, 128)]), krp[:], self.ident[:], True, True, reads=[krk, "ident"], writes=["ps7"])
            kip, kik = kit[p], "kit%d" % p
            self.evac(kip[:], V(ps[7], 0, [(1, 128)]), 1.0, ["ps7"], [kik])
            kb.dma("sp", self.d_kiT[:, blk * 128:(blk + 1) * 128], kip[:], kik, reads=[kik], writes=["kiT"])
            wp, wpk = wis[p], "wis%d" % p
            kb.op("dve", lambda g: g.tensor_scalar(out=wp[:], in0=V(ps[bank], 320, [(1, 16)]), scalar1=1.0 / 32.0, scalar2=None,
                                                   op0=ALU.mult), reads=[pk], writes=[wpk])
            kb.dma("sp", self.d_wi[blk], wp[:], wpk, reads=[wpk], writes=["wi"])

    def _p1_qi(self, l, t0, QT, hT, es, wcols):
        kb, ps, L = self.kb, self.ps, self.L
        qr = [self.sb(es, "qr%d" % i, 512, BF16) for i in range(2)]
        qt4 = [self.sb(es, "qt4%d" % i, 512, F32) for i in range(1)]
        qit = [self.sb(es, "qit%d" % i, 512, BF16) for i in range(2)]
        n = 0
        for grp in range(2):
            wv_, wk, s = self.load_w(wcols(C_QI + grp * 512, 512), [(512, 16), (1, 512)])
            for bi in range(QT // 128):
                blk = t0 // 128 + bi
                bank = 2 + (self.bank_i % 2)
                self.bank_i += 1
                pk = "ps%d" % bank
                for k in range(16):
                    kb.mm(ps[bank][:], V(hT, k * QT + bi * 128, [(1, 128)]), V(self.wring[s], k * 512, [(1, 512)]),
                          k == 0, k == 15, reads=[wk, "hT"], writes=[pk], last=(k == 15))
                p = n % 2
                n += 1
                q, qk = qr[p], "qr%d" % p
                t4 = qt4[0]
                cosb = V(self.ropeC, blk * 16, [(0, 8), (1, 16)])
                sinb = V(self.ropeS, blk * 16, [(0, 8), (1, 16)])
                x1 = V(ps[bank], 0, [(64, 8), (1, 16)])
                x2 = V(ps[bank], 16, [(64, 8), (1, 16)])
                kb.op("dve", lambda g: g.tensor_tensor(out=V(t4, 0, [(16, 8), (1, 16)]), in0=x1, in1=cosb, op=ALU.mult), reads=[pk, "ropeC"], writes=["qt0"])
                kb.op("dve", lambda g: g.tensor_tensor(out=V(t4, 128, [(16, 8), (1, 16)]), in0=x2, in1=sinb, op=ALU.mult), reads=[pk, "ropeS"], writes=["qt1"])
                kb.op("dve", lambda g: g.tensor_tensor(out=V(t4, 256, [(16, 8), (1, 16)]), in0=x1, in1=sinb, op=ALU.mult), reads=[pk, "ropeS"], writes=["qt2"])
                kb.op("dve", lambda g: g.tensor_tensor(out=V(t4, 384, [(16, 8), (1, 16)]), in0=x2, in1=cosb, op=ALU.mult), reads=[pk, "ropeC"], writes=["qt3"])
                kb.op("dve", lambda g: g.tensor_tensor(out=V(q, 0, [(64, 8), (1, 16)]), in0=V(t4, 0, [(16, 8), (1, 16)]),
                                                       in1=V(t4, 128, [(16, 8), (1, 16)]), op=ALU.subtract), reads=["qt0", "qt1"], writes=[qk])
                kb.op("dve", lambda g: g.tensor_tensor(out=V(q, 16, [(64, 8), (1, 16)]), in0=V(t4, 256, [(16, 8), (1, 16)]),
                                                       in1=V(t4, 384, [(16, 8), (1, 16)]), op=ALU.add), reads=["qt2", "qt3"], writes=[qk])
                kb.op("act", lambda g: g.activation(out=V(q, 32, [(64, 8), (1, 32)]), in_=V(ps[bank], 32, [(64, 8), (1, 32)]),
                                                    func=AF.Identity), reads=[pk], writes=[qk])
                for pr in range(4):
                    kb.mm(V(ps[6], pr * 128, [(1, 128)]), V(q, pr * 128, [(1, 128)]), self.ident[:], True, True,
                          reads=[qk, "ident"], writes=["ps6"])
                qi_, qik = qit[p], "qit%d" % p
                self.evac(qi_[:], ps[6][:], 1.0, ["ps6"], [qik])
                kb.dma("sp", self.d_qiT[:, grp * 4:(grp + 1) * 4, blk * 128:(blk + 1) * 128], V(qi_, 0, [(128, 4), (1, 128)]),
                       qik, reads=[qik], writes=["qiT"])

    def phase_att_a(self, l):
        kb, ps, L, NB, NR = self.kb, self.ps, self.L, self.NB, self.NR
        kAf, kCf = self.kA[l][1], self.kC[l][1]
        NTA = self.NTA
        with ExitStack() as es:
            ckvT = self.sb(es, "ckvTs", NR * 2 * L, BF16)
            ckvA = self.sb(es, "ckvAs", NR * NB * 256, BF16)
            kiT = self.sb(es, "kiTs", NR * L, BF16)
            bta = self.sb(es, "btas", NTA * 8 * 128, BF16)
            wuv = self.sb(es, "wuvs", 8 * 2 * 128, BF16)
            for rk in range(NR):
                kb.dma("sp", V(ckvT, rk * 2 * L, [(1, 2 * L)]), kAf[rk * 128:(rk + 1) * 128, 0:2 * L], "aa_k", reads=["kfull"], writes=["ckvTs"])
                kb.dma("sp", V(kiT, rk * L, [(1, L)]), kAf[rk * 128:(rk + 1) * 128, 2 * L:3 * L], "aa_k", reads=["kfull"], writes=["kiTs"])
                kb.dma("sp", V(ckvA, rk * NB * 256, [(1, NB * 256)]), kCf[rk * 128:(rk + 1) * 128, :], "aa_k",
                       reads=["kfull"], writes=["ckvAs"])
            kb.dma("pool", bta[:], self.d_bta, "aa_c", writes=["btas"])
            kb.dma("pool", V(wuv, 0, [(256, 8), (128, 2), (1, 128)]), self.d_wuv[l].rearrange("h (cc c) d -> c h cc d", c=128),
                   "aa_c", writes=["wuvs"])
            qiT = [self.sb(es, "aqi%d" % i, 8 * 128, BF16) for i in range(2)]
            wi = [self.sb(es, "awi%d" % i, 16, F32) for i in range(2)]
            qlat = [self.sb(es, "aql%d" % i, 16 * 128, BF16) for i in range(4)]
            dg = self.sb(es, "adg", 16 * 128, BF16)
            R = [self.sb(es, "aR%d" % i, 512, BF16) for i in range(4)]
            NK = NR * L
            scs = [self.sb(es, "asc%d" % i, NK, F32) for i in range(2)]
            bss = [self.sb(es, "abs%d" % i, 8, F32) for i in range(2)]
            junks = [self.sb(es, "ajk%d" % i, NK, BF16) for i in range(2)]
            assert NK <= 4096
            negm = [(self.wring[i // 2], (i % 2) * 4096) for i in range(4)]
            Pst = [self.sb(es, "aP%d" % i, 512, BF16) for i in range(3)]
            olat = self.sb(es, "aol", 2 * 512, BF16)
            densb = self.sb(es, "adn", 512, F32)
            ysb = self.sb(es, "ays", 512, F32)
            yst = [self.sb(es, "ayst%d" % i, 512, BF16) for i in range(2)]
            st = dict(rn=0, pn=0, yn=0, ln=0)
            LB = (4, 5, 7)

            def nkeys(s):
                return NR * (s + 1) * 128

            def stage1(s):
                b = s % 2
                t0 = s * 128
                scA, sck = scs[b], "asc%d" % b
                kb.dma("sp", V(qlat[s % 4], 0, [(128, 16), (1, 128)]), self.d_qlat[:, :, t0:t0 + 128], "aa_ql%d" % (s % 4),
                       reads=["qlat"], writes=["aql%d" % (s % 4)])
                if nkeys(s) <= 256:
                    return
                kb.dma("sp", V(qiT[b], 0, [(128, 8), (1, 128)]), self.d_qiT[:, :, t0:t0 + 128], "aa_q%d" % b,
                       reads=["qiT"], writes=["aqi%d" % b])
                kb.dma("sp", wi[b][:], self.d_wi[s], "aa_q%d" % b, reads=["wi"], writes=["awi%d" % b])
                kb.op("dve", lambda g: g.tensor_tensor(out=V(dg, 0, [(128, 16), (1, 128)]), in0=V(self.ident, 0, [(0, 16), (1, 128)]),
                                                       in1=V(wi[b], 0, [(1, 16), (0, 128)]), op=ALU.mult),
                      reads=["ident", "awi%d" % b], writes=["adg"])
                nb_r = s + 1
                for rk in range(NR):
                    cbase = rk * nb_r * 128
                    for kc in range((nb_r + 3) // 4):
                        w = min(512, nb_r * 128 - kc * 512)
                        LA = 2
                        pend = []
                        for h in range(16 + LA):
                            if h < 16:
                                pr, hf = h // 2, h % 2
                                bank = LB[st["ln"] % 3]
                                st["ln"] += 1
                                kb.mm(V(ps[bank], 0, [(1, w)]), V(qiT[b], pr * 128, [(1, 128)], p0=hf * 64, npart=64),
                                      V(kiT, rk * L + kc * 512, [(1, w)], p0=hf * 64, npart=64), True, True,
                                      reads=["aqi%d" % b, "kiTs"], writes=["ps%d" % bank])
                                r = R[st["rn"] % 4]
                                rkey = "aR%d" % (st["rn"] % 4)
                                st["rn"] += 1
                                kb.op("act", lambda g: g.activation(out=V(r, 0, [(1, w)]), in_=V(ps[bank], 0, [(1, w)]), func=AF.Relu),
                                      reads=["ps%d" % bank], writes=[rkey])
                                pend.append((r, rkey))
                            if h >= LA:
                                hd = h - LA
                                r2, rkey2 = pend[hd]
                                kb.mm(V(ps[6], 0, [(1, w)]), V(dg, hd * 128, [(1, 128)]), V(r2, 0, [(1, w)]), hd == 0, hd == 15,
                                      reads=["adg", rkey2], writes=["ps6"], last=(hd == 15))
                        kb.op("act", lambda g: g.activation(out=V(scA, cbase + kc * 512, [(1, w)]), in_=V(ps[6], 0, [(1, w)]),
                                                            func=AF.Identity), reads=["ps6"], writes=[sck])
                    kb.op("dve", lambda g: g.tensor_tensor(out=V(scA, cbase + s * 128, [(1, 128)]), in0=V(scA, cbase + s * 128, [(1, 128)]),
                                                           in1=V(self.cm, rk * 128, [(1, 128)]), op=ALU.add), reads=[sck, "cm"], writes=[sck])

            def stage2(s):
                n = nkeys(s)
                nm, nk = negm[s % 4], "anegm%d" % (s % 4)
                p = s % 2
                sc, sck = scs[p], "asc%d" % p
                bs_, bk = bss[p], "abs%d" % p
                jk_, jkk = junks[p], "ajk%d" % p
                if n <= 256:
                    kb.op("pool", lambda g: g.memset(V(nm[0], nm[1], [(1, n)]), 0.0), writes=[nk])
                    return
                lo, d0, mid, cnt, g2 = [V(bs_, i, [(1, 1)]) for i in range(5)]
                scv = V(sc, 0, [(1, n)])
                kb.op("dve", lambda g: g.reduce_max(out=d0, in_=scv, axis=mybir.AxisListType.X), reads=[sck], writes=[bk])
                yield
                kb.op("dve", lambda g: g.tensor_scalar(out=d0, in0=d0, scalar1=31.0, scalar2=None, op0=ALU.add), reads=[bk], writes=[bk])
                yield
                kb.op("dve", lambda g: g.memset(lo, -30.0), reads=[bk], writes=[bk])
                yield
                kb.op("dve", lambda g: g.scalar_tensor_tensor(out=mid, in0=d0, scalar=0.5, in1=lo, op0=ALU.mult, op1=ALU.add),
                      reads=[bk], writes=[bk])
                yield
                NIT = 26
                for k in range(NIT):
                    kb.op("dve", lambda g: g.tensor_scalar(out=V(jk_, 0, [(1, n)]), in0=scv, scalar1=mid, scalar2=0.0, op0=ALU.is_ge,
                                                           op1=ALU.add, accum_out=cnt), reads=[sck, bk], writes=[bk, jkk])
                    yield
                    kb.op("dve", lambda g: g.tensor_scalar(out=g2, in0=cnt, scalar1=255.5, scalar2=2.0 ** -(k + 1), op0=ALU.is_gt,
                                                           op1=ALU.mult), reads=[bk], writes=[bk])
                    yield
                    kb.op("dve", lambda g: g.scalar_tensor_tensor(out=lo, in0=d0, scalar=g2, in1=lo, op0=ALU.mult, op1=ALU.add),
                          reads=[bk], writes=[bk])
                    yield
                    if k < NIT - 1:
                        kb.op("dve", lambda g: g.scalar_tensor_tensor(out=mid, in0=d0, scalar=2.0 ** -(k + 2), in1=lo, op0=ALU.mult,
                                                                      op1=ALU.add), reads=[bk], writes=[bk])
                        yield
                kb.op("dve", lambda g: g.tensor_scalar(out=V(nm[0], nm[1], [(1, n)]), in0=scv, scalar1=lo, scalar2=NEG, op0=ALU.is_lt,
                                                       op1=ALU.mult), reads=[sck, bk], writes=[nk])
                yield

            def inter(*gens):
                gens = list(gens)
                while gens:
                    for g_ in list(gens):
                        try:
                            next(g_)
                        except StopIteration:
                            gens.remove(g_)

            def stage3(s):
                b = s % 4
                t0 = s * 128
                nm, nk = negm[b], "anegm%d" % b
                nb_r = s + 1
                keys = [(rk, sj) for rk in range(NR) for sj in range(nb_r)]
                for g4 in range(2):
                    for idx, (rk, sj) in enumerate(keys):
                        j = NR * sj + rk
                        dl = min(NR * s + NR - 1 - j, NTA - 1)
                        first, lastk = idx == 0, idx == len(keys) - 1
                        sb_ = LB[st["pn"] % 3]
                        sk = "ps%d" % sb_
                        kb.mm(ps[sb_][:], self.ident[:], V(bta, (dl * 8 + g4 * 4) * 128, [(1, 512)]), True, False,
                              reads=["ident", "btas"], writes=[sk], last=False)
                        kb.mm(ps[sb_][:], V(nm[0], nm[1] + (rk * nb_r + sj) * 128, [(1, 128)]), V(self.ident, 0, [(0, 4), (1, 128)]), False, False,
                              reads=[nk, "ident"], writes=[sk], last=False)
                        for cc in range(2):
                            kb.mm(ps[sb_][:], V(ckvT, (rk * 2 + cc) * L + sj * 128, [(1, 128)]),
                                  V(qlat[b], (cc * 8 + g4 * 4) * 128, [(1, 512)]), False, cc == 1,
                                  reads=["ckvTs", "aql%d" % b], writes=[sk], last=(cc == 1))
                        pt = Pst[st["pn"] % 3]
                        pk_ = "aP%d" % (st["pn"] % 3)
                        st["pn"] += 1
                        kb.op("act", lambda g: g.activation(out=pt[:], in_=ps[sb_][:], func=AF.Exp), reads=[sk], writes=[pk_])
                        for cc in range(2):
                            kb.mm(ps[cc][:], V(ckvA, (rk * NB + sj) * 256 + cc * 128, [(1, 128)]), pt[:], first, lastk,
                                  reads=["ckvAs", pk_], writes=["ps%d" % cc], last=lastk)
                        kb.mm(ps[2][:], self.ones[:], pt[:], first, lastk, reads=["ones", pk_], writes=["ps2"], last=lastk)
                    for cc in range(2):
                        self.evac(V(olat, cc * 512, [(1, 512)]), ps[cc][:], 1.0, ["ps%d" % cc], ["aol"], eng="act")
                    kb.op("act", lambda g: g.activation(out=densb[:], in_=ps[2][:], func=AF.Ln), reads=["ps2"], writes=["adn"])
                    kb.op("act", lambda g: g.activation(out=densb[:], in_=densb[:], func=AF.Exp, scale=-1.0), reads=["adn"], writes=["adn"])
                    for h in range(4):
                        hh = g4 * 4 + h
                        for cc in range(2):
                            kb.mm(V(ps[3], h * 128, [(1, 128)]), V(wuv, hh * 256 + cc * 128, [(1, 128)]),
                                  V(olat, cc * 512 + h * 128, [(1, 128)]), cc == 0, cc == 1, reads=["wuvs", "aol"], writes=["ps3"],
                                  last=(cc == 1))
                    self.evac(ysb[:], ps[3][:], 1.0, ["ps3"], ["ays"], eng="act")
                    y = yst[st["yn"] % 2]
                    yk = "ayst%d" % (st["yn"] % 2)
                    st["yn"] += 1
                    kb.op("pool", lambda g: g.tensor_tensor(out=y[:], in0=ysb[:], in1=densb[:], op=ALU.mult),
                          reads=["ays", "adn"], writes=[yk])
                    kb.dma("sp", self.d_ya[:, g4 * 4:(g4 + 1) * 4, t0:t0 + 128], V(y, 0, [(128, 4), (1, 128)]), yk,
                           reads=[yk], writes=["yaT"])

            stage1(0)
            stage1(1)
            inter(stage2(0), stage2(1))
            for s in range(0, NB, 2):
                if s + 2 < NB:
                    stage1(s + 2)
                    stage1(s + 3)
                stage3(s)
                stage3(s + 1)
                if s + 2 < NB:
                    inter(stage2(s + 2), stage2(s + 3))
            kb.barrier()

    def phase_att_b(self, l):
        kb, ps, L, NB, NR = self.kb, self.ps, self.L, self.NB, self.NR
        kKf = [self.kK[l][g][1] for g in range(3)]
        kVf = [self.kV[l][g][1] for g in range(3)]
        NRING, NTB, OFFB = self.NRING, self.NTB, self.OFFB
        with ExitStack() as es:
            btb = self.sb(es, "btbs", sum(NTB) * 4 * 128, BF16)
            kb.dma("pool", btb[:], self.d_btb, "ab_c", writes=["btbs"])
            kr = [self.sb(es, "bkr%d" % g, NRING[g] * 512, BF16) for g in range(3)]
            vr = [self.sb(es, "bvr%d" % g, NRING[g] * 512, BF16) for g in range(3)]
            qb = [self.sb(es, "bq%d" % i, 12 * 128, BF16) for i in range(2)]
            Pst = [self.sb(es, "bP%d" % i, 512, BF16) for i in range(3)]
            rden = self.sb(es, "brd", 512, F32)
            yst = [self.sb(es, "byst%d" % i, 512, BF16) for i in range(2)]
            pn = 0
            for s in range(NB):
                b = s % 2
                t0 = s * 128
                kb.dma("sp", V(qb[b], 0, [(128, 12), (1, 128)]), self.d_qb[:, :, t0:t0 + 128], "ab_q%d" % b, reads=["qb"],
                       writes=["bq%d" % b])
                for rk in range(NR):
                    j = NR * s + rk
                    for g in range(3):
                        sl = j % NRING[g]
                        kb.dma("sp", V(kr[g], sl * 512, [(128, 4), (1, 128)]),
                               kKf[g][rk * 128:(rk + 1) * 128, :].rearrange("p (h t) -> p h t", h=4)[:, :, t0:t0 + 128],
                               "ab_k%d_%d" % (g, s % 2), reads=["kfull"], writes=["bkr%d_%d" % (g, sl)])
                        kb.dma("sp", V(vr[g], sl * 512, [(1, 512)]), kVf[g][rk * 128:(rk + 1) * 128, s * 512:(s + 1) * 512],
                               "ab_v%d_%d" % (g, s % 2), reads=["kfull"], writes=["bvr%d_%d" % (g, sl)])
                jtop = NR * s + NR - 1
                pairs = [(g, dl) for g in range(3) for dl in range(NTB[g]) if jtop - dl >= 0]
                for idx, (g, dl) in enumerate(pairs):
                    j = jtop - dl
                    sl = j % NRING[g]
                    first, lastp = idx == 0, idx == len(pairs) - 1
                    sb_ = 5 + (pn % 2)
                    sk = "ps%d" % sb_
                    kb.mm(ps[sb_][:], self.ident[:], V(btb, (OFFB[g] + dl) * 512, [(1, 512)]), True, False,
                          reads=["ident", "btbs"], writes=[sk], last=False)
                    for hg in range(4):
                        kb.mm(V(ps[sb_], hg * 128, [(1, 128)]), V(kr[g], sl * 512 + hg * 128, [(1, 128)]),
                              V(qb[b], (g * 4 + hg) * 128, [(1, 128)]), False, hg == 3,
                              reads=["bkr%d_%d" % (g, sl), "bq%d" % b], writes=[sk], last=(hg == 3))
                    pt = Pst[pn % 3]
                    pk_ = "bP%d" % (pn % 3)
                    pn += 1
                    kb.op("act", lambda g_: g_.activation(out=pt[:], in_=ps[sb_][:], func=AF.Exp), reads=[sk], writes=[pk_])
                    for hg in range(4):
                        kb.mm(V(ps[0], hg * 128, [(1, 128)]), V(vr[g], sl * 512 + hg * 128, [(1, 128)]),
                              V(pt, hg * 128, [(1, 128)]), first and hg == 0, lastp, reads=["bvr%d_%d" % (g, sl), pk_], writes=["ps0"],
                              last=(lastp and hg == 3))
                    kb.mm(ps[1][:], self.ones[:], pt[:], first, lastp, reads=["ones", pk_], writes=["ps1"], last=lastp)
                kb.op("dve", lambda g_: g_.reciprocal(out=rden[:], in_=ps[1][:]), reads=["ps1"], writes=["brd"])
                y = yst[s % 2]
                yk = "byst%d" % (s % 2)
                kb.op("dve", lambda g_: g_.tensor_tensor(out=y[:], in0=ps[0][:], in1=rden[:], op=ALU.mult),
                      reads=["ps0", "brd"], writes=[yk])
                kb.dma("sp", self.d_yb[:, :, t0:t0 + 128], V(y, 0, [(128, 4), (1, 128)]), yk, reads=[yk], writes=["ybT"])
            kb.barrier()

    def phase_p4(self, l, xsrc, t0, QT, es):
        kb, ps, L = self.kb, self.ps, self.L
        ya = self.sb(es, "p4ya", 8 * QT, BF16)
        yb = self.sb(es, "p4yb", 4 * QT, BF16)
        mg = self.sb(es, "p4mg", 16 * QT, BF16)
        wa = self.sb(es, "p4wa", 8 * D, BF16)
        wb = self.sb(es, "p4wb", 4 * D, BF16)
        gt = [self.sb(es, "p4g%d" % i, 2 * QT, BF16) for i in range(2)]
        m1 = [self.sb(es, "p4m%d" % i, 512, F32) for i in range(2)]
        xo = [self.sb(es, "p4x%d" % i, 512, F32) for i in range(2)]
        kb.dma("sp", V(ya, 0, [(QT, 8), (1, QT)]), self.d_ya[:, :, t0:t0 + QT], "p4y", reads=["yaT"], writes=["p4ya"])
        kb.dma("sp", V(yb, 0, [(QT, 4), (1, QT)]), self.d_yb[:, :, t0:t0 + QT], "p4y", reads=["ybT"], writes=["p4yb"])
        kb.dma("pool", V(wa, 0, [(D, 8), (1, D)]), self.d_waup[l].rearrange("(h p) c -> p h c", p=128), "p4w", writes=["p4wa"])
        kb.dma("pool", V(wb, 0, [(D, 4), (1, D)]), self.d_wbup[l].rearrange("(h p) c -> p h c", p=128), "p4w", writes=["p4wb"])
        NT = QT // 512
        for oc in range(16):
            gb_ = oc % 2
            gk = "p4g%d" % gb_
            kb.dma("sp", V(gt[gb_], 0, [(1, QT)]), self.d_gates[:, oc, t0:t0 + QT], gk, reads=["gates"], writes=[gk])
            kb.dma("sp", V(gt[gb_], QT, [(1, QT)]), self.d_gates[:, 16 + oc, t0:t0 + QT], gk, reads=["gates"], writes=[gk])
            for ti in range(NT):
                for h in range(8):
                    kb.mm(ps[0][:], V(wa, h * D + oc * 128, [(1, 128)]), V(ya, h * QT + ti * 512, [(1, 512)]), h == 0, h == 7,
                          reads=["p4wa", "p4ya"], writes=["ps0"], last=(h == 7))
                for h in range(4):
                    kb.mm(ps[1][:], V(wb, h * D + oc * 128, [(1, 128)]), V(yb, h * QT + ti * 512, [(1, 512)]), h == 0, h == 3,
                          reads=["p4wb", "p4yb"], writes=["ps1"], last=(h == 3))
                kb.op("dve", lambda g: g.tensor_tensor(out=m1[0][:], in0=ps[0][:], in1=V(gt[gb_], ti * 512, [(1, 512)]), op=ALU.mult),
                      reads=["ps0", gk], writes=["p4m0"])
                kb.op("dve", lambda g: g.tensor_tensor(out=m1[1][:], in0=ps[1][:], in1=V(gt[gb_], QT + ti * 512, [(1, 512)]), op=ALU.mult),
                      reads=["ps1", gk], writes=["p4m1"])
                kb.op("pool", lambda g: g.tensor_tensor(out=V(mg, oc * QT + ti * 512, [(1, 512)]), in0=m1[0][:], in1=m1[1][:], op=ALU.add),
                      reads=["p4m0", "p4m1"], writes=["p4mg"])
        n = 0
        for grp in range(4):
            wv_, wk, s = self.load_w(self.d_wout[l][:, grp * 512:(grp + 1) * 512].rearrange("(k p) c -> p k c", p=128),
                                     [(512, 16), (1, 512)])
            for c in range(4):
                oc = grp * 4 + c
                for ti in range(NT):
                    tt = t0 + ti * 512
                    bank = 2 + n % 2
                    xb = n % 2
                    n += 1
                    xk = "p4x%d" % xb
                    kb.dma("sp", xo[xb][:], xsrc[oc, :, tt:tt + 512], xk, reads=["x_t%d" % (tt // 512)], writes=[xk])
                    for k in range(16):
                        kb.mm(ps[bank][:], V(self.wring[s], k * 512 + c * 128, [(1, 128)]), V(mg, k * QT + ti * 512, [(1, 512)]),
                              k == 0, k == 15, reads=[wk, "p4mg"], writes=["ps%d" % bank], last=(k == 15))
                    kb.op("dve", lambda g: g.scalar_tensor_tensor(out=xo[xb][:], in0=ps[bank][:], scalar=V(self.mod[l], 32 + oc, [(1, 1)]),
                                                                  in1=xo[xb][:], op0=ALU.mult, op1=ALU.add),
                          reads=["ps%d" % bank, xk], writes=[xk])
                    kb.dma("sp", self.d_xs[oc, :, tt:tt + 512], xo[xb][:], xk, reads=[xk], writes=["x_t%d" % (tt // 512)])

    def phase_ffn(self, l, t0, QT, hT, es):
        kb, ps, L = self.kb, self.ps, self.L
        act = self.sb(es, "fact", FC * QT, BF16)
        sg = [self.sb(es, "fsg%d" % i, 512, F32) for i in range(2)]
        xo = [self.sb(es, "fx%d" % i, 512, F32) for i in range(2)]
        NT = QT // 512
        n = 0
        for cg in range(DFF // 256):
            s = self.wr_i % 2
            self.wr_i += 1
            wk = "wring%d" % s
            kb.dma("pool", V(self.wring[s], 0, [(256, 16), (1, 256)]),
                   self.d_wfg[l][:, cg * 256:(cg + 1) * 256].rearrange("(k p) c -> p k c", p=128), wk, writes=[wk])
            kb.dma("pool", V(self.wring[s], 4096, [(256, 16), (1, 256)]),
                   self.d_wfu[l][:, cg * 256:(cg + 1) * 256].rearrange("(k p) c -> p k c", p=128), wk, writes=[wk])
            for c in range(2):
                f = cg * 2 + c
                for ti in range(NT):
                    bg, bu = 2 + 2 * (n % 2), 3 + 2 * (n % 2)
                    sgi = n % 2
                    n += 1
                    for k in range(16):
                        kb.mm(ps[bg][:], V(self.wring[s], k * 256 + c * 128, [(1, 128)]), V(hT, k * QT + ti * 512, [(1, 512)]),
                              k == 0, k == 15, reads=[wk, "hT"], writes=["ps%d" % bg], last=(k == 15))
                    for k in range(16):
                        kb.mm(ps[bu][:], V(self.wring[s], 4096 + k * 256 + c * 128, [(1, 128)]), V(hT, k * QT + ti * 512, [(1, 512)]),
                              k == 0, k == 15, reads=[wk, "hT"], writes=["ps%d" % bu], last=(k == 15))
                    kb.op("act", lambda g: g.activation(out=sg[sgi][:], in_=ps[bg][:], func=AF.Silu), reads=["ps%d" % bg],
                          writes=["fsg%d" % sgi])
                    kb.op("dve", lambda g: g.tensor_tensor(out=V(act, f * QT + ti * 512, [(1, 512)]), in0=ps[bu][:], in1=sg[sgi][:],
                                                           op=ALU.mult), reads=["ps%d" % bu, "fsg%d" % sgi], writes=["fact"])
        n = 0
        for oc in range(16):
            s = self.wr_i % 2
            self.wr_i += 1
            wk = "wring%d" % s
            kb.dma("pool", V(self.wring[s], 0, [(128, FC), (1, 128)]),
                   self.d_wfd[l][:, oc * 128:(oc + 1) * 128].rearrange("(f p) c -> p f c", p=128), wk, writes=[wk])
            for ti in range(NT):
                tt = t0 + ti * 512
                bank = n % 2
                xb = n % 2
                n += 1
                xk = "fx%d" % xb
                kb.dma("sp", xo[xb][:], self.d_xs[oc, :, tt:tt + 512], xk, reads=["x_t%d" % (tt // 512)], writes=[xk])
                for f in range(FC):
                    kb.mm(ps[bank][:], V(self.wring[s], f * 128, [(1, 128)]), V(act, f * QT + ti * 512, [(1, 512)]), f == 0,
                          f == FC - 1, reads=[wk, "fact"], writes=["ps%d" % bank], last=(f == FC - 1))
                kb.op("dve", lambda g: g.scalar_tensor_tensor(out=xo[xb][:], in0=ps[bank][:], scalar=V(self.mod[l], 80 + oc, [(1, 1)]),
                                                              in1=xo[xb][:], op0=ALU.mult, op1=ALU.add),
                      reads=["ps%d" % bank, xk], writes=[xk])
                kb.dma("sp", self.d_xs[oc, :, tt:tt + 512], xo[xb][:], xk, reads=[xk], writes=["x_t%d" % (tt // 512)])

    def build(self):
        kb, L, QT = self.kb, self.L, self.QT
        self.epsb = self.nc.alloc_sbuf_tensor("epsb", [128, 1], F32)
        kb.op("pool", lambda g: g.memset(self.epsb[:], EPS), writes=["epsb"])
        self.phase_const()
        self.phase_mod()
        ph = self.phases
        for l in range(self.NL):
            xsrc = self.d_xT if l == 0 else self.d_xs
            if ph is None or "p1" in ph:
                with ExitStack() as es:
                    QT1 = L
                    hT = self.sb(es, "hT", 16 * QT1, BF16)
                    with ExitStack() as es2:
                        self.norm_tiles(es2, xsrc, self.A1[l], (self.mod[l], 0), 0, QT1, hT)
                        kb.barrier()
                    with ExitStack() as es2:
                        self.phase_p1(l, 0, QT1, hT, es2, "k")
                        kb.barrier()
                    if self.NR > 1:
                        rg = [[c * self.NR + i for i in range(self.NR)] for c in range(8 // self.NR)]
                        for ci, (loc, full) in enumerate([self.kA[l], self.kC[l]] + self.kK[l] + self.kV[l]):
                            kb.cc(lambda g: g.collective_compute("AllGather", ALU.bypass, replica_groups=rg, ins=[loc], outs=[full]),
                                  "ag%d_%d" % (l, ci), reads=["kloc"], writes=["kfull"])
                    with ExitStack() as es2:
                        self.phase_p1(l, 0, QT1, hT, es2, "q")
                        kb.barrier()
            if ph is None or "atta" in ph:
                self.phase_att_a(l)
            if ph is None or "attb" in ph:
                self.phase_att_b(l)
            if ph is None or "p4" in ph:
                for q in range(L // QT):
                    with ExitStack() as es:
                        self.phase_p4(l, xsrc, q * QT, QT, es)
                        kb.barrier()
                    with ExitStack() as es:
                        hT = self.sb(es, "hT", 16 * QT, BF16)
                        with ExitStack() as es2:
                            self.norm_tiles(es2, self.d_xs, self.A2[l], (self.mod[l], 48), q * QT, QT, hT)
                            kb.barrier()
                        self.phase_ffn(l, q * QT, QT, hT, es)
                        kb.barrier()
        toks = []
        with ExitStack() as es:
            toks = self.norm_tiles(es, self.d_xs, self.fg, None, 0, L, None, final=True)
        for t in toks:
            kb._wait("sp", t)
        for nm in self.dbg:
            pass
        kb.barrier()
        return self.nc


def _rel_bucket(n):
    n = np.maximum(n, 0)
    nf = np.maximum(n, 1).astype(np.float32)
    large = 16 + (np.log(nf / np.float32(16)) / np.float32(math.log(2048 / 16)) * np.float32(16)).astype(np.int32)
    return np.where(n < 16, n, np.minimum(large, 31))


def _static_tables(LG, NR, r, rel_bias):
    L = LG // NR
    NB = L // 128
    k = np.arange(128)[:, None]
    q = np.arange(128)[None, :]
    negf = np.float32(NEG)
    sh = NR - 1 - r
    NTA = 17 + NR
    bta = np.full((128, NTA, 8, 128), negf, np.float32)
    for dl in range(NTA):
        d = dl - sh
        if d < 0:
            continue
        dist = min(d, 17) * 128 + q - k
        val = rel_bias[_rel_bucket(dist)][:, :, :8]
        val = np.where((dist >= 0)[:, :, None], val, negf)
        bta[:, dl] = np.transpose(val, (0, 2, 1))
    NTB = [n + NR - 1 for n in NBLK_G]
    btb = np.full((128, sum(NTB), 4, 128), negf, np.float32)
    off = 0
    for g, (win, dil) in enumerate(B_GROUPS):
        for dl in range(NTB[g]):
            d = dl - sh
            if 0 <= d < NBLK_G[g]:
                dist = d * 128 + q - k
                ok = (dist >= 0) & (dist <= win) & (dist % dil == 0)
                val = rel_bias[_rel_bucket(dist)][:, :, 8 + g * 4: 8 + g * 4 + 4]
                val = np.where(ok[:, :, None], val, negf)
                btb[:, off + dl] = np.transpose(val, (0, 2, 1))
        off += NTB[g]
    cm = np.where(np.arange(128)[None, :] <= np.arange(128)[:, None], np.float32(0), np.float32(-1e30)).astype(np.float32)
    cmr = np.zeros((128, NR, 128), np.float32)
    for rk in range(NR):
        if rk == r:
            cmr[:, rk] = cm
        elif rk > r:
            cmr[:, rk] = np.float32(-1e30)
    blocks = np.array([NR * s + r for s in range(NB)])
    pos = (blocks[:, None] * 128 + np.arange(128)[None, :]).astype(np.float32)
    freqs = (np.float32(10000.0) ** (-np.arange(16, dtype=np.float32) / np.float32(16))).astype(np.float32)
    ang = pos[:, :, None] * freqs[None, None, :]
    ropeC = np.cos(ang).astype(np.float32).transpose(1, 0, 2)
    ropeS = np.sin(ang).astype(np.float32).transpose(1, 0, 2)
    return dict(bta=np.ascontiguousarray(bta.reshape(128, -1)), btb=np.ascontiguousarray(btb.reshape(128, -1)),
                cmr=np.ascontiguousarray(cmr.reshape(128, -1)), ropeC=np.ascontiguousarray(ropeC),
                ropeS=np.ascontiguousarray(ropeS), ident=np.eye(128, dtype=np.float32))


def _pk(v):
    v = np.asarray(v, np.float32)
    return np.ascontiguousarray(np.swapaxes(v.reshape(v.shape[:-1] + (-1, 128)), -1, -2))


def make_in_maps(inp, LG, NR, cores):
    f = lambda a: np.ascontiguousarray(np.asarray(a, np.float32))
    shared = dict(
        w_ada=f(inp["w_ada"]), b_adaT=_pk(inp["b_ada"]), n1g=_pk(inp["norm1_g"]), n2g=_pk(inp["norm2_g"]), fg=_pk(inp["final_g"]),
        w_in=f(inp["w_in"]),
        kvg=np.ascontiguousarray(np.broadcast_to(f(inp["kv_norm_g"])[:, None, :], (len(inp["kv_norm_g"]), 128, 256))),
        ilg=np.ascontiguousarray(np.broadcast_to(f(inp["idx_ln_g"])[:, None, :], (len(inp["idx_ln_g"]), 128, 64))),
        ilb=np.ascontiguousarray(np.broadcast_to(f(inp["idx_ln_b"])[:, None, :], (len(inp["idx_ln_b"]), 128, 64))),
        w_uk=f(inp["w_uk"]), w_uv=f(inp["w_uv"]), w_a_up=f(inp["w_a_up"]), w_b_up=f(inp["w_b_up"]), w_out=f(inp["w_out"]),
        w_ffg=f(inp["w_ff_gate"]), w_ffu=f(inp["w_ff_up"]), w_ffd=f(inp["w_ff_down"]),
    )
    tabs = [_static_tables(LG, NR, r, f(inp["rel_bias"])) for r in range(NR)]
    x = f(inp["x"])
    c = f(inp["c"])
    L = LG // NR
    maps = []
    for b, r in cores:
        m = dict(shared)
        m.update(tabs[r])
        xb = x[b].reshape(LG // 128, 128, D)[r::NR].reshape(L, D)
        m["xT"] = np.ascontiguousarray(xb.T.reshape(KC, 128, L))
        m["c_pk"] = _pk(c[b])
        maps.append(m)
    return maps


_CACHE = {}
NR_FULL = 2


def kernel(**inputs):
    x = np.asarray(inputs["x"])
    B, LG, _ = x.shape
    nl = np.asarray(inputs["w_ada"]).shape[0]
    NR = NR_FULL
    key = (LG, nl, NR)
    if key not in _CACHE:
        _CACHE[key] = Prog(LG, nl, NR=NR).build()
    nc = _CACHE[key]
    cores = [((c // NR) % B, c % NR) for c in range(8)]
    in_maps = make_in_maps(inputs, LG, NR, cores)
    res = run_bass_kernel_spmd(nc, in_maps, core_ids=list(range(8)))
    L = LG // NR
    out = np.empty((B, LG // 128, 128, D), np.float32)
    for c, (b, r) in enumerate(cores):
        if c // NR >= B:
            continue
        o = np.asarray(res.results[c]["outT"]).reshape(D, L).T
        out[b, r::NR] = o.reshape(L // 128, 128, D)
    return out.reshape(B, LG, D)
```

```python
import math
from contextlib import ExitStack
import numpy as np
import concourse.bass as bass
import concourse.mybir as mybir
from concourse.bass_utils import run_bass_kernel_spmd

F32 = mybir.dt.float32
BF16 = mybir.dt.bfloat16
ALU = mybir.AluOpType
AF = mybir.ActivationFunctionType

D = 2048
KC = 16
NIN = 11088
DFF = 5632
FC = 44
NEG = -30000.0
EPS = 1e-6
C_QA, C_CKV, C_QI, C_KI, C_WI, C_QB, C_KB, C_VB, C_GATE = 0, 1024, 1280, 2304, 2368, 2384, 3920, 5456, 6992
B_GROUPS = ((128, 1), (512, 4), (2048, 16))
NBLK_G = (2, 5, 17)
OFF_G = (0, 2, 7)
NRING = (3, 6, 18)


class KB:
    def __init__(self, nc):
        self.nc = nc
        self.eng = {"pe": nc.tensor, "dve": nc.vector, "act": nc.scalar, "pool": nc.gpsimd, "sp": nc.sync}
        self.esem = {e: nc.alloc_semaphore("prog_" + e) for e in ("pe", "dve", "act", "pool")}
        self.cnt = {e: 0 for e in self.esem}
        self.dsem, self.dcnt = {}, {}
        self.waited, self.lastw, self.readers = {}, {}, {}
        self.ninst = 0

    def _sem(self, key):
        return self.esem[key[1]] if key[0] == "e" else self.dsem[key[1]]

    def _wait(self, e, tok):
        if tok is None:
            return
        key, val = tok
        if key == ("e", "pe") and e == "pe":
            return
        if key[0] == "d":
            val = self.dcnt[key[1]]
        k = (e, key)
        if self.waited.get(k, 0) >= val:
            return
        self.waited[k] = val
        self.eng[e].wait_ge(self._sem(key), val)
        self.ninst += 1

    def _deps(self, e, reads, writes):
        for r in reads:
            self._wait(e, self.lastw.get(r))
        for w in writes:
            self._wait(e, self.lastw.get(w))
            for t in self.readers.get(w, ()):
                self._wait(e, t)

    def _commit(self, tok, reads, writes):
        for r in reads:
            lst = self.readers.setdefault(r, [])
            lst.append(tok)
            if len(lst) > 12:
                best = {}
                for k, v in lst:
                    best[k] = max(best.get(k, 0), v)
                self.readers[r] = list(best.items())
        for w in writes:
            self.lastw[w] = tok
            self.readers[w] = []

    def op(self, e, fn, reads=(), writes=()):
        self._deps(e, reads, writes)
        ins = fn(self.eng[e])
        self.cnt[e] += 1
        ins.then_inc(self.esem[e], 1)
        tok = (("e", e), self.cnt[e])
        self._commit(tok, reads, writes)
        self.ninst += 1
        return tok

    def mm(self, out, lhsT, rhs, start, stop, reads=(), writes=(), last=True):
        e = "pe"
        self._deps(e, reads, writes)
        ins = self.nc.tensor.matmul(out, lhsT, rhs, start=start, stop=stop)
        self.ninst += 1
        if last:
            self.cnt[e] += 1
            ins.then_inc(self.esem[e], 1)
            tok = (("e", e), self.cnt[e])
        else:
            tok = (("e", e), self.cnt[e] + 1)
        self._commit(tok, reads, writes)
        return tok

    def dma(self, q, out, in_, slot, reads=(), writes=()):
        if slot not in self.dsem:
            self.dsem[slot] = self.nc.alloc_semaphore("d_" + slot)
            self.dcnt[slot] = 0
        self._deps(q, reads, writes)
        ins = self.eng[q].dma_start(out=out, in_=in_)
        self.dcnt[slot] += 16
        ins.then_inc(self.dsem[slot], 16)
        tok = (("d", slot), self.dcnt[slot])
        self._commit(tok, reads, writes)
        self.ninst += 1
        return tok

    def cc(self, fn, name, reads=(), writes=()):
        self.dsem[name] = self.nc.alloc_semaphore("cc_" + name)
        self._deps("pool", reads, writes)
        ins = fn(self.eng["pool"])
        ins.then_inc(self.dsem[name], 1)
        self.dcnt[name] = 1
        tok = (("d", name), 1)
        self._commit(tok, reads, writes)
        self.ninst += 1
        return tok

    def barrier(self):
        for e in self.eng:
            for e2 in self.esem:
                if e2 != e and self.cnt[e2] > 0:
                    self._wait(e, (("e", e2), self.cnt[e2]))
            for s, v in self.dcnt.items():
                if v > 0:
                    self._wait(e, (("d", s), v))
        self.lastw.clear()
        self.readers.clear()


def V(t, off, dims, p0=0, npart=128):
    base = t[:]
    ps = base.ap[0][0]
    return bass.AP(base.tensor, base.offset + p0 * ps + off, [[ps, npart]] + [[int(s), int(c)] for s, c in dims])


class Prog:
    def __init__(self, LG, nl=2, dbg=(), phases=None, NR=1):
        L = LG // NR
        self.LG, self.NR = LG, NR
        self.L, self.NB, self.NL = L, L // 128, nl
        self.NTA = 17 + NR
        self.NTB = [n + NR - 1 for n in NBLK_G]
        self.OFFB = [0, self.NTB[0], self.NTB[0] + self.NTB[1]]
        self.NRING = [n - 1 + 2 * NR for n in NBLK_G]
        self.O_KIT, self.O_KB, self.O_CKVA = 2 * L, 3 * L, 15 * L
        self.O_VB = 15 * L + (L // 128) * 256
        self.KCOLS = 15 * L + (L // 128) * 1792
        self.QT = min(1024, L)
        self.dbg = set(dbg)
        self.phases = phases
        nc = self.nc = bass.Bass("TRN2", target_bir_lowering=False)
        self.kb = KB(nc)
        self.ev_i = 0
        self.wr_i = 0
        NB = self.NB

        def din(name, shape):
            return nc.dram_tensor(name, list(shape), F32, kind="ExternalInput").ap()

        def dscr(name, shape, dt):
            kind = "ExternalOutput" if name in self.dbg else "Internal"
            return nc.dram_tensor(name, list(shape), dt, kind=kind).ap()

        self.d_xT = din("xT", [KC, 128, L])
        self.d_c = din("c_pk", [128, KC])
        self.d_wada = din("w_ada", [nl, D, 6 * D])
        self.d_bada = din("b_adaT", [nl, 128, 96])
        self.d_n1g = din("n1g", [nl, 128, KC])
        self.d_n2g = din("n2g", [nl, 128, KC])
        self.d_fg = din("fg", [128, KC])
        self.d_win = din("w_in", [nl, D, NIN])
        self.d_kvg = din("kvg", [nl, 128, 256])
        self.d_ilg = din("ilg", [nl, 128, 64])
        self.d_ilb = din("ilb", [nl, 128, 64])
        self.d_wuk = din("w_uk", [nl, 8, 128, 256])
        self.d_wuv = din("w_uv", [nl, 8, 256, 128])
        self.d_waup = din("w_a_up", [nl, 1024, D])
        self.d_wbup = din("w_b_up", [nl, 512, D])
        self.d_wout = din("w_out", [nl, D, D])
        self.d_wfg = din("w_ffg", [nl, D, DFF])
        self.d_wfu = din("w_ffu", [nl, D, DFF])
        self.d_wfd = din("w_ffd", [nl, DFF, D])
        self.d_ropeC = din("ropeC", [128, NB, 16])
        self.d_ropeS = din("ropeS", [128, NB, 16])
        self.d_bta = din("bta", [128, self.NTA * 8 * 128])
        self.d_btb = din("btb", [128, sum(self.NTB) * 4 * 128])
        self.d_cm = din("cmr", [128, NR * 128])
        self.d_ident = din("ident", [128, 128])
        self.d_out = nc.dram_tensor("outT", [KC, 128, L], F32, kind="ExternalOutput").ap()
        self.d_xs = dscr("xs", [KC, 128, L], F32)
        self.d_qlat = dscr("qlat", [128, 16, L], BF16)
        self.d_qiT = dscr("qiT", [128, 8, L], BF16)
        self.d_wi = dscr("wi", [NB, 128, 16], F32)
        self.d_qb = dscr("qb", [128, 12, L], BF16)
        self.d_gates = dscr("gates", [128, 32, L], BF16)
        def kt(name, cols):
            loc = dscr(name + "_l", [128, cols], BF16)
            full = loc if NR == 1 else dscr(name + "_f", [NR * 128, cols], BF16)
            return loc, full
        self.kA = [kt("kA%d" % l, 3 * L) for l in range(nl)]
        self.kC = [kt("kC%d" % l, NB * 256) for l in range(nl)]
        self.kK = [[kt("kK%d_%d" % (l, g), 4 * L) for g in range(3)] for l in range(nl)]
        self.kV = [[kt("kV%d_%d" % (l, g), NB * 512) for g in range(3)] for l in range(nl)]
        self.d_ya = dscr("yaT", [128, 8, L], BF16)
        self.d_yb = dscr("ybT", [128, 4, L], BF16)

        def sbp(name, f, dt):
            return nc.alloc_sbuf_tensor(name, [128, f], dt)

        self.ident = sbp("identb", 128, BF16)
        self.ones = sbp("onesb", 128, BF16)
        self.cm = sbp("cmf", NR * 128, F32)
        self.ropeC = sbp("ropeCs", NB * 16, F32)
        self.ropeS = sbp("ropeSs", NB * 16, F32)
        self.mod = [sbp("mod%d" % l, 96, F32) for l in range(nl)]
        self.A1 = [sbp("A1_%d" % l, 16, F32) for l in range(nl)]
        self.A2 = [sbp("A2_%d" % l, 16, F32) for l in range(nl)]
        self.fg = sbp("fgs", 16, F32)
        self.wring = [sbp("wring%d" % i, 8192, BF16) for i in range(2)]
        self.ps = [nc.alloc_psum_tensor("psb%d" % i, [128, 512], F32) for i in range(8)]

    def sb(self, es, name, f, dt):
        self.sb_n = getattr(self, "sb_n", 0) + 1
        return es.enter_context(self.nc.sbuf_tensor("s%d_%s" % (self.sb_n, name), [128, f], dt))

    def evq(self):
        self.ev_i += 1
        return "act" if self.ev_i % 2 else "dve"

    def evac(self, out, in_, scale, reads, writes, eng=None):
        e = eng or self.evq()
        if e == "act":
            return self.kb.op("act", lambda g: g.activation(out=out, in_=in_, func=AF.Identity, scale=float(scale)),
                              reads=reads, writes=writes)
        if scale == 1.0:
            return self.kb.op("dve", lambda g: g.tensor_copy(out=out, in_=in_), reads=reads, writes=writes)
        return self.kb.op("dve", lambda g: g.tensor_scalar(out=out, in0=in_, scalar1=float(scale), scalar2=None,
                                                            op0=ALU.mult), reads=reads, writes=writes)

    def load_w(self, dram_ap, dims):
        s = self.wr_i % 2
        self.wr_i += 1
        key = "wring%d" % s
        view = V(self.wring[s], 0, dims)
        self.kb.dma("pool", view, dram_ap, key, writes=[key])
        return view, key, s

    def wslice(self, s, off, dims):
        return V(self.wring[s], off, dims)

    def phase_const(self):
        kb = self.kb
        kb.dma("pool", self.ident[:], self.d_ident, "c_ident", writes=["ident"])
        kb.op("pool", lambda g: g.memset(self.ones[:], 1.0), writes=["ones"])
        kb.dma("sp", self.cm[:], self.d_cm, "c_misc", writes=["cm"])
        kb.dma("sp", self.ropeC[:], self.d_ropeC.rearrange("p b f -> p (b f)"), "c_misc", writes=["ropeC"])
        kb.dma("sp", self.ropeS[:], self.d_ropeS.rearrange("p b f -> p (b f)"), "c_misc", writes=["ropeS"])
        kb.dma("sp", self.fg[:], self.d_fg, "c_misc", writes=["fg"])

    def phase_mod(self):
        kb, ps = self.kb, self.ps
        with ExitStack() as es:
            sc = self.sb(es, "sc", 16, F32)
            scb = self.sb(es, "scb", 16, BF16)
            bada = self.sb(es, "bada", 96, F32)
            ng = self.sb(es, "ng", 32, F32)
            W = [self.sb(es, "wada%d" % i, 16 * 1536, BF16) for i in range(2)]
            kb.dma("sp", sc[:], self.d_c, "m_c", writes=["sc"])
            kb.op("act", lambda g: g.activation(out=scb[:], in_=sc[:], func=AF.Silu), reads=["sc"], writes=["scb"])
            n = 0
            for l in range(self.NL):
                for cb in range(8):
                    s = n % 2
                    n += 1
                    wk = "wada%d" % s
                    kb.dma("pool", V(W[s], 0, [(1536, 16), (1, 1536)]),
                           self.d_wada[l, :, cb * 1536:(cb + 1) * 1536].rearrange("(k p) c -> p k c", p=128),
                           wk, writes=[wk])
                    for jj in range(12):
                        j = cb * 12 + jj
                        for k in range(16):
                            kb.mm(V(ps[0], j, [(1, 1)]), V(W[s], k * 1536 + jj * 128, [(1, 128)]),
                                  V(scb, k, [(1, 1)]), k == 0, k == 15, reads=[wk, "scb"], writes=["ps0"],
                                  last=(k == 15))
                kb.dma("sp", bada[:], self.d_bada[l], "m_b", writes=["bada"])
                kb.dma("sp", V(ng, 0, [(1, 16)]), self.d_n1g[l], "m_b", writes=["ng"])
                kb.dma("sp", V(ng, 16, [(1, 16)]), self.d_n2g[l], "m_b", writes=["ng"])
                mod = self.mod[l]
                kb.op("dve", lambda g: g.tensor_tensor(out=mod[:], in0=V(ps[0], 0, [(1, 96)]), in1=bada[:], op=ALU.add),
                      reads=["ps0", "bada"], writes=["mod%d" % l])
                kb.op("dve", lambda g: g.scalar_tensor_tensor(out=self.A1[l][:], in0=V(mod, 16, [(1, 16)]), scalar=1.0,
                                                              in1=V(ng, 0, [(1, 16)]), op0=ALU.add, op1=ALU.mult),
                      reads=["mod%d" % l, "ng"], writes=["A1_%d" % l])
                kb.op("dve", lambda g: g.scalar_tensor_tensor(out=self.A2[l][:], in0=V(mod, 64, [(1, 16)]), scalar=1.0,
                                                              in1=V(ng, 16, [(1, 16)]), op0=ALU.add, op1=ALU.mult),
                      reads=["mod%d" % l, "ng"], writes=["A2_%d" % l])
            kb.barrier()

    def norm_tiles(self, es, xsrc, A, shift, t0, QT, hT, final=False):
        kb, ps = self.kb, self.ps
        xt = [self.sb(es, "nx%d" % i, 16 * 512, F32) for i in range(2)]
        sq = self.sb(es, "nsq", 16 * 512, BF16)
        rs = self.sb(es, "nrs", 512, F32)
        toks = []
        for ti in range(QT // 512):
            b = ti % 2
            tt = t0 + ti * 512
            xk = "nx%d" % b
            kb.dma("sp", V(xt[b], 0, [(512, 16), (1, 512)]), xsrc[:, :, tt:tt + 512].rearrange("k p t -> p k t"),
                   xk, reads=["x_t%d" % (tt // 512)], writes=[xk])
            kb.op("act", lambda g: g.activation(out=sq[:], in_=xt[b][:], func=AF.Square), reads=[xk], writes=["nsq"])
            for k in range(16):
                kb.mm(ps[1][:], self.ones[:], V(sq, k * 512, [(1, 512)]), k == 0, k == 15, reads=["nsq", "ones"],
                      writes=["ps1"], last=(k == 15))
            kb.op("act", lambda g: g.activation(out=rs[:], in_=ps[1][:], func=AF.Sqrt, scale=1.0 / D, bias=self.epsb[:]),
                  reads=["ps1"], writes=["nrs"])
            kb.op("dve", lambda g: g.reciprocal(out=rs[:], in_=rs[:]), reads=["nrs"], writes=["nrs"])
            kb.op("dve", lambda g: g.tensor_tensor(out=V(xt[b], 0, [(512, 16), (1, 512)]),
                                                   in0=V(xt[b], 0, [(512, 16), (1, 512)]),
                                                   in1=V(rs, 0, [(0, 16), (1, 512)]), op=ALU.mult),
                  reads=[xk, "nrs"], writes=[xk])
            if final:
                for k in range(16):
                    kb.op("dve" if k % 2 else "act",
                          (lambda g: g.tensor_scalar(out=V(xt[b], k * 512, [(1, 512)]), in0=V(xt[b], k * 512, [(1, 512)]),
                                                     scalar1=V(A, k, [(1, 1)]), scalar2=None, op0=ALU.mult)) if k % 2 else
                          (lambda g: g.activation(out=V(xt[b], k * 512, [(1, 512)]), in_=V(xt[b], k * 512, [(1, 512)]),
                                                  func=AF.Identity, scale=V(A, k, [(1, 1)]))),
                          reads=[xk, "fg"], writes=[xk])
                toks.append(kb.dma("sp", self.d_out[:, :, tt:tt + 512].rearrange("k p t -> p k t"),
                                   V(xt[b], 0, [(512, 16), (1, 512)]), "outst", reads=[xk]))
                continue
            for k in range(16):
                o = V(hT, k * QT + ti * 512, [(1, 512)])
                i_ = V(xt[b], k * 512, [(1, 512)])
                if k % 2:
                    kb.op("dve", lambda g: g.tensor_scalar(out=o, in0=i_, scalar1=V(A, k, [(1, 1)]),
                                                           scalar2=V(shift[0], shift[1] + k, [(1, 1)]),
                                                           op0=ALU.mult, op1=ALU.add), reads=[xk], writes=["hT"])
                else:
                    kb.op("act", lambda g: g.activation(out=o, in_=i_, func=AF.Identity, scale=V(A, k, [(1, 1)]),
                                                        bias=V(shift[0], shift[1] + k, [(1, 1)])), reads=[xk], writes=["hT"])
        return toks

    def fm_proj(self, hT, QT, t0, wv, wk, c0, ncol, out_cb):
        kb, ps = self.kb, self.ps
        for c in range(ncol // 128):
            for ti in range(QT // 512):
                bank = 2 + (self.bank_i % 2)
                self.bank_i += 1
                pk = "ps%d" % bank
                for k in range(16):
                    kb.mm(ps[bank][:], V(wv[0], wv[1] + k * wv[2] + c * 128, [(1, 128)]),
                          V(hT, k * QT + ti * 512, [(1, 512)]), k == 0, k == 15, reads=[wk, "hT"], writes=[pk],
                          last=(k == 15))
                out_cb(c0 + c, ti, bank)

    def phase_p1(self, l, t0, QT, hT, es, part):
        kb, ps, L = self.kb, self.ps, self.L
        self.bank_i = 0
        win = self.d_win[l]
        self.d_ckvT = self.kA[l][0][:, 0:2 * L].rearrange("p (c t) -> p c t", c=2)
        self.d_kiT = self.kA[l][0][:, 2 * L:3 * L]
        self.d_ckvA = self.kC[l][0].rearrange("p (b c) -> b p c", c=256)
        kKl = [self.kK[l][g][0].rearrange("p (h t) -> p h t", h=4) for g in range(3)]
        kVl = [self.kV[l][g][0] for g in range(3)]
        stg = [self.sb(es, "p1stg%d" % i, 512, BF16) for i in range(4)]
        self.stg_i = 0

        def nstg():
            i = self.stg_i % 4
            self.stg_i += 1
            return stg[i], "p1stg%d" % i

        def wcols(c0, n):
            return win[:, c0:c0 + n].rearrange("(k p) c -> p k c", p=128)

        if part == "q":
            self._p1_q(l, t0, QT, hT, es, wcols, nstg)
        else:
            self._p1_k(l, t0, QT, hT, es, wcols, nstg, kKl, kVl)

    def _p1_q(self, l, t0, QT, hT, es, wcols, nstg):
        kb, ps, L = self.kb, self.ps, self.L
        wuk = self.sb(es, "wuk", 8 * 256, BF16)
        kb.dma("pool", V(wuk, 0, [(256, 8), (1, 256)]), self.d_wuk[l].rearrange("h d c -> d h c"), "p1wuk", writes=["wuk"])
        qa = [self.sb(es, "qa%d" % i, 512, BF16) for i in range(2)]
        self.qa_i = 0
        for grp in range(2):
            wv_, wk, s = self.load_w(wcols(C_QA + grp * 512, 512), [(512, 16), (1, 512)])

            def cb_qa(h, ti, bank):
                qi = self.qa_i % 2
                self.qa_i += 1
                qk = "qa%d" % qi
                self.evac(qa[qi][:], ps[bank][:], 1.0, ["ps%d" % bank], [qk])
                for cc in range(2):
                    b2 = 4 + cc
                    kb.mm(ps[b2][:], V(wuk, h * 256 + cc * 128, [(1, 128)]), qa[qi][:], True, True,
                          reads=["wuk", qk], writes=["ps%d" % b2])
                    st, sk = nstg()
                    self.evac(st[:], ps[b2][:], 128 ** -0.5, ["ps%d" % b2], [sk])
                    tt = t0 + ti * 512
                    kb.dma("sp", self.d_qlat[:, cc * 8 + h, tt:tt + 512], st[:], sk, reads=[sk], writes=["qlat"])

            self.fm_proj(hT, QT, t0, (self.wring[s], 0, 512), wk, grp * 4, 512, cb_qa)

        self._p1_fm(t0, QT, hT, wcols, nstg, C_QB, self.d_qb, 128 ** -0.5, "qb", None)
        self._p1_gates_qi(l, t0, QT, hT, es, wcols, nstg)

    def _p1_fm(self, t0, QT, hT, wcols, nstg, cbase, dst, scl, dk, kKl):
        kb, ps = self.kb, self.ps
        if True:
            for grp in range(3):
                wv_, wk, s = self.load_w(wcols(cbase + grp * 512, 512), [(512, 16), (1, 512)])

                def cb_b(h, ti, bank, dst=dst, scl=scl, dk=dk):
                    st, sk = nstg()
                    self.evac(st[:], ps[bank][:], scl, ["ps%d" % bank], [sk])
                    tt = t0 + ti * 512
                    dap = dst[:, h, tt:tt + 512] if dst is not None else kKl[h // 4][:, h % 4, tt:tt + 512]
                    kb.dma("sp", dap, st[:], sk, reads=[sk], writes=[dk])

                self.fm_proj(hT, QT, t0, (self.wring[s], 0, 512), wk, grp * 4, 512, cb_b)

    def _p1_gates_qi(self, l, t0, QT, hT, es, wcols, nstg):
        kb, ps, L = self.kb, self.ps, self.L
        for grp in range(8):
            wv_, wk, s = self.load_w(wcols(C_GATE + grp * 512, 512), [(512, 16), (1, 512)])

            def cb_g(c, ti, bank):
                st, sk = nstg()
                kb.op("act", lambda g: g.activation(out=st[:], in_=ps[bank][:], func=AF.Sigmoid),
                      reads=["ps%d" % bank], writes=[sk])
                tt = t0 + ti * 512
                kb.dma("sp", self.d_gates[:, c, tt:tt + 512], st[:], sk, reads=[sk], writes=["gates"])

            self.fm_proj(hT, QT, t0, (self.wring[s], 0, 512), wk, grp * 4, 512, cb_g)

        self._p1_qi(l, t0, QT, hT, es, wcols)

    def _p1_k(self, l, t0, QT, hT, es, wcols, nstg, kKl, kVl):
        kb, ps, L = self.kb, self.ps, self.L
        self._p1_fm(t0, QT, hT, wcols, nstg, C_KB, None, 1.0, "kbT", kKl)
        vst = [self.sb(es, "vst%d" % i, 512, BF16) for i in range(2)]
        n = 0
        for grp in range(3):
            wv_, wk, s = self.load_w(wcols(C_VB + grp * 512, 512), [(512, 16), (1, 512)])
            for bi in range(QT // 128):
                blk = t0 // 128 + bi
                bank = 2 + (self.bank_i % 2)
                self.bank_i += 1
                for k in range(16):
                    kb.mm(ps[bank][:], V(hT, k * QT + bi * 128, [(1, 128)]), V(self.wring[s], k * 512, [(1, 512)]),
                          k == 0, k == 15, reads=[wk, "hT"], writes=["ps%d" % bank], last=(k == 15))
                v = vst[n % 2]
                vk = "vst%d" % (n % 2)
                n += 1
                self.evac(v[:], ps[bank][:], 1.0, ["ps%d" % bank], [vk])
                kb.dma("sp", kVl[grp][:, blk * 512:(blk + 1) * 512], v[:], vk, reads=[vk], writes=["vb"])

        kvg = self.sb(es, "kvg", 256, F32)
        ilg = self.sb(es, "ilg", 128, F32)
        kb.dma("sp", kvg[:], self.d_kvg[l], "p1c", writes=["kvg"])
        kb.dma("sp", V(ilg, 0, [(1, 64)]), self.d_ilg[l], "p1c", writes=["ilg"])
        kb.dma("sp", V(ilg, 64, [(1, 64)]), self.d_ilb[l], "p1c", writes=["ilg"])
        s = self.wr_i % 2
        self.wr_i += 1
        wk = "wring%d" % s
        kb.dma("pool", V(self.wring[s], 0, [(336, 16), (1, 256)]), wcols(C_CKV, 256), wk, writes=[wk])
        kb.dma("pool", V(self.wring[s], 256, [(336, 16), (1, 80)]), wcols(C_KI, 80), wk, writes=[wk])
        junk = self.sb(es, "junk", 256, F32)
        st8 = self.sb(es, "st8", 16, F32)
        ckvb = [self.sb(es, "ckvb%d" % i, 256, BF16) for i in range(2)]
        ckt = [self.sb(es, "ckt%d" % i, 256, BF16) for i in range(2)]
        kn = self.sb(es, "kn", 64, F32)
        kt = self.sb(es, "ktmp", 64, F32)
        kr = [self.sb(es, "kr%d" % i, 128, BF16) for i in range(2)]
        kit = [self.sb(es, "kit%d" % i, 128, BF16) for i in range(2)]
        wis = [self.sb(es, "wis%d" % i, 16, F32) for i in range(2)]
        for bi in range(QT // 128):
            blk = t0 // 128 + bi
            p = bi % 2
            bank = 2 + (self.bank_i % 2)
            self.bank_i += 1
            pk = "ps%d" % bank
            for k in range(16):
                kb.mm(V(ps[bank], 0, [(1, 336)]), V(hT, k * QT + bi * 128, [(1, 128)]), V(self.wring[s], k * 336, [(1, 336)]),
                      k == 0, k == 15, reads=[wk, "hT"], writes=[pk], last=(k == 15))
            kb.op("act", lambda g: g.activation(out=junk[:], in_=V(ps[bank], 0, [(1, 256)]), func=AF.Square),
                  reads=[pk], writes=["junk"])
            kb.op("dve", lambda g: g.reduce_sum(out=V(st8, 0, [(1, 1)]), in_=junk[:], axis=mybir.AxisListType.X),
                  reads=["junk"], writes=["st8a"])
            kb.op("act", lambda g: g.activation(out=V(st8, 1, [(1, 1)]), in_=V(st8, 0, [(1, 1)]), func=AF.Sqrt,
                                                scale=1.0 / 256, bias=self.epsb[:]), reads=["st8a"], writes=["st8b"])
            kb.op("dve", lambda g: g.reciprocal(out=V(st8, 2, [(1, 1)]), in_=V(st8, 1, [(1, 1)])), reads=["st8b"], writes=["st8c"])
            cb_, ck = ckvb[p], "ckvb%d" % p
            kb.op("dve", lambda g: g.scalar_tensor_tensor(out=cb_[:], in0=V(ps[bank], 0, [(1, 256)]), scalar=V(st8, 2, [(1, 1)]),
                                                          in1=kvg[:], op0=ALU.mult, op1=ALU.mult),
                  reads=[pk, "st8c", "kvg"], writes=[ck])
            kb.dma("sp", self.d_ckvA[blk], cb_[:], ck, reads=[ck], writes=["ckvA"])
            for cc in range(2):
                kb.mm(V(ps[6], cc * 128, [(1, 128)]), V(cb_, cc * 128, [(1, 128)]), self.ident[:], True, True,
                      reads=[ck, "ident"], writes=["ps6"])
            ct, ctk = ckt[p], "ckt%d" % p
            self.evac(ct[:], V(ps[6], 0, [(1, 256)]), 1.0, ["ps6"], [ctk])
            kb.dma("sp", self.d_ckvT[:, :, blk * 128:(blk + 1) * 128], V(ct, 0, [(128, 2), (1, 128)]), ctk,
                   reads=[ctk], writes=["ckvT"])
            kb.op("dve", lambda g: g.reduce_sum(out=V(st8, 3, [(1, 1)]), in_=V(ps[bank], 256, [(1, 64)]), axis=mybir.AxisListType.X),
                  reads=[pk], writes=["st8d"])
            kb.op("act", lambda g: g.activation(out=V(junk, 0, [(1, 64)]), in_=V(ps[bank], 256, [(1, 64)]), func=AF.Square),
                  reads=[pk], writes=["junk"])
            kb.op("dve", lambda g: g.reduce_sum(out=V(st8, 4, [(1, 1)]), in_=V(junk, 0, [(1, 64)]), axis=mybir.AxisListType.X),
                  reads=["junk"], writes=["st8e"])
            kb.op("dve", lambda g: g.tensor_scalar(out=V(st8, 5, [(1, 1)]), in0=V(st8, 3, [(1, 1)]), scalar1=1.0 / 64, scalar2=None,
                                                   op0=ALU.mult), reads=["st8d"], writes=["st8f"])
            kb.op("dve", lambda g: g.tensor_tensor(out=V(st8, 6, [(1, 1)]), in0=V(st8, 5, [(1, 1)]), in1=V(st8, 5, [(1, 1)]),
                                                   op=ALU.mult), reads=["st8f"], writes=["st8g"])
            kb.op("dve", lambda g: g.scalar_tensor_tensor(out=V(st8, 7, [(1, 1)]), in0=V(st8, 4, [(1, 1)]), scalar=1.0 / 64,
                                                          in1=V(st8, 6, [(1, 1)]), op0=ALU.mult, op1=ALU.subtract),
                  reads=["st8e", "st8g"], writes=["st8h"])
            kb.op("act", lambda g: g.activation(out=V(st8, 8, [(1, 1)]), in_=V(st8, 7, [(1, 1)]), func=AF.Sqrt,
                                                bias=self.epsb[:]), reads=["st8h"], writes=["st8i"])
            kb.op("dve", lambda g: g.reciprocal(out=V(st8, 9, [(1, 1)]), in_=V(st8, 8, [(1, 1)])), reads=["st8i"], writes=["st8j"])
            kb.op("dve", lambda g: g.tensor_scalar(out=kn[:], in0=V(ps[bank], 256, [(1, 64)]), scalar1=V(st8, 5, [(1, 1)]),
                                                   scalar2=V(st8, 9, [(1, 1)]), op0=ALU.subtract, op1=ALU.mult),
                  reads=[pk, "st8f", "st8j"], writes=["kn"])
            kb.op("dve", lambda g: g.tensor_tensor(out=kn[:], in0=kn[:], in1=V(ilg, 0, [(1, 64)]), op=ALU.mult),
                  reads=["kn", "ilg"], writes=["kn"])
            kb.op("dve", lambda g: g.tensor_tensor(out=kn[:], in0=kn[:], in1=V(ilg, 64, [(1, 64)]), op=ALU.add),
                  reads=["kn", "ilg"], writes=["kn"])
            cosb = V(self.ropeC, blk * 16, [(1, 16)])
            sinb = V(self.ropeS, blk * 16, [(1, 16)])
            x1, x2 = V(kn, 0, [(1, 16)]), V(kn, 16, [(1, 16)])
            kb.op("dve", lambda g: g.tensor_tensor(out=V(kt, 0, [(1, 16)]), in0=x1, in1=cosb, op=ALU.mult), reads=["kn", "ropeC"], writes=["kt0"])
            kb.op("dve", lambda g: g.tensor_tensor(out=V(kt, 16, [(1, 16)]), in0=x2, in1=sinb, op=ALU.mult), reads=["kn", "ropeS"], writes=["kt1"])
            kb.op("dve", lambda g: g.tensor_tensor(out=V(kt, 32, [(1, 16)]), in0=x1, in1=sinb, op=ALU.mult), reads=["kn", "ropeS"], writes=["kt2"])
            kb.op("dve", lambda g: g.tensor_tensor(out=V(kt, 48, [(1, 16)]), in0=x2, in1=cosb, op=ALU.mult), reads=["kn", "ropeC"], writes=["kt3"])
            krp, krk = kr[p], "kr%d" % p
            for half in range(2):
                kb.op("dve", lambda g: g.tensor_tensor(out=V(krp, half * 64, [(1, 16)]), in0=V(kt, 0, [(1, 16)]), in1=V(kt, 16, [(1, 16)]),
                                                       op=ALU.subtract), reads=["kt0", "kt1"], writes=[krk])
                kb.op("dve", lambda g: g.tensor_tensor(out=V(krp, half * 64 + 16, [(1, 16)]), in0=V(kt, 32, [(1, 16)]), in1=V(kt, 48, [(1, 16)]),
                                                       op=ALU.add), reads=["kt2", "kt3"], writes=[krk])
                kb.op("dve", lambda g: g.tensor_copy(out=V(krp, half * 64 + 32, [(1, 32)]), in_=V(kn, 32, [(1, 32)])),
                      reads=["kn"], writes=[krk])
            kb.mm(V(ps[7], 0, [(1, 128)]), krp[:], self.ident[:], True, True, reads=[krk, "ident"], writes=["ps7"])
            kip, kik = kit[p], "kit%d" % p
            self.evac(kip[:], V(ps[7], 0, [(1, 128)]), 1.0, ["ps7"], [kik])
            kb.dma("sp", self.d_kiT[:, blk * 128:(blk + 1) * 128], kip[:], kik, reads=[kik], writes=["kiT"])
            wp, wpk = wis[p], "wis%d" % p
            kb.op("dve", lambda g: g.tensor_scalar(out=wp[:], in0=V(ps[bank], 320, [(1, 16)]), scalar1=1.0 / 32.0, scalar2=None,
                                                   op0=ALU.mult), reads=[pk], writes=[wpk])
            kb.dma("sp", self.d_wi[blk], wp[:], wpk, reads=[wpk], writes=["wi"])

    def _p1_qi(self, l, t0, QT, hT, es, wcols):
        kb, ps, L = self.kb, self.ps, self.L
        qr = [self.sb(es, "qr%d" % i, 512, BF16) for i in range(2)]
        qt4 = [self.sb(es, "qt4%d" % i, 512, F32) for i in range(1)]
        qit = [self.sb(es, "qit%d" % i, 512, BF16) for i in range(2)]
        n = 0
        for grp in range(2):
            wv_, wk, s = self.load_w(wcols(C_QI + grp * 512, 512), [(512, 16), (1, 512)])
            for bi in range(QT // 128):
                blk = t0 // 128 + bi
                bank = 2 + (self.bank_i % 2)
                self.bank_i += 1
                pk = "ps%d" % bank
                for k in range(16):
                    kb.mm(ps[bank][:], V(hT, k * QT + bi * 128, [(1, 128)]), V(self.wring[s], k * 512, [(1, 512)]),
                          k == 0, k == 15, reads=[wk, "hT"], writes=[pk], last=(k == 15))
                p = n % 2
                n += 1
                q, qk = qr[p], "qr%d" % p
                t4 = qt4[0]
                cosb = V(self.ropeC, blk * 16, [(0, 8), (1, 16)])
                sinb = V(self.ropeS, blk * 16, [(0, 8), (1, 16)])
                x1 = V(ps[bank], 0, [(64, 8), (1, 16)])
                x2 = V(ps[bank], 16, [(64, 8), (1, 16)])
                kb.op("dve", lambda g: g.tensor_tensor(out=V(t4, 0, [(16, 8), (1, 16)]), in0=x1, in1=cosb, op=ALU.mult), reads=[pk, "ropeC"], writes=["qt0"])
                kb.op("dve", lambda g: g.tensor_tensor(out=V(t4, 128, [(16, 8), (1, 16)]), in0=x2, in1=sinb, op=ALU.mult), reads=[pk, "ropeS"], writes=["qt1"])
                kb.op("dve", lambda g: g.tensor_tensor(out=V(t4, 256, [(16, 8), (1, 16)]), in0=x1, in1=sinb, op=ALU.mult), reads=[pk, "ropeS"], writes=["qt2"])
                kb.op("dve", lambda g: g.tensor_tensor(out=V(t4, 384, [(16, 8), (1, 16)]), in0=x2, in1=cosb, op=ALU.mult), reads=[pk, "ropeC"], writes=["qt3"])
                kb.op("dve", lambda g: g.tensor_tensor(out=V(q, 0, [(64, 8), (1, 16)]), in0=V(t4, 0, [(16, 8), (1, 16)]),
                                                       in1=V(t4, 128, [(16, 8), (1, 16)]), op=ALU.subtract), reads=["qt0", "qt1"], writes=[qk])
                kb.op("dve", lambda g: g.tensor_tensor(out=V(q, 16, [(64, 8), (1, 16)]), in0=V(t4, 256, [(16, 8), (1, 16)]),
                                                       in1=V(t4, 384, [(16, 8), (1, 16)]), op=ALU.add), reads=["qt2", "qt3"], writes=[qk])
                kb.op("act", lambda g: g.activation(out=V(q, 32, [(64, 8), (1, 32)]), in_=V(ps[bank], 32, [(64, 8), (1, 32)]),
                                                    func=AF.Identity), reads=[pk], writes=[qk])
                for pr in range(4):
                    kb.mm(V(ps[6], pr * 128, [(1, 128)]), V(q, pr * 128, [(1, 128)]), self.ident[:], True, True,
                          reads=[qk, "ident"], writes=["ps6"])
                qi_, qik = qit[p], "qit%d" % p
                self.evac(qi_[:], ps[6][:], 1.0, ["ps6"], [qik])
                kb.dma("sp", self.d_qiT[:, grp * 4:(grp + 1) * 4, blk * 128:(blk + 1) * 128], V(qi_, 0, [(128, 4), (1, 128)]),
                       qik, reads=[qik], writes=["qiT"])

    def phase_att_a(self, l):
        kb, ps, L, NB, NR = self.kb, self.ps, self.L, self.NB, self.NR
        kAf, kCf = self.kA[l][1], self.kC[l][1]
        NTA = self.NTA
        with ExitStack() as es:
            ckvT = self.sb(es, "ckvTs", NR * 2 * L, BF16)
            ckvA = self.sb(es, "ckvAs", NR * NB * 256, BF16)
            kiT = self.sb(es, "kiTs", NR * L, BF16)
            bta = self.sb(es, "btas", NTA * 8 * 128, BF16)
            wuv = self.sb(es, "wuvs", 8 * 2 * 128, BF16)
            for rk in range(NR):
                kb.dma("sp", V(ckvT, rk * 2 * L, [(1, 2 * L)]), kAf[rk * 128:(rk + 1) * 128, 0:2 * L], "aa_k", reads=["kfull"], writes=["ckvTs"])
                kb.dma("sp", V(kiT, rk * L, [(1, L)]), kAf[rk * 128:(rk + 1) * 128, 2 * L:3 * L], "aa_k", reads=["kfull"], writes=["kiTs"])
                kb.dma("sp", V(ckvA, rk * NB * 256, [(1, NB * 256)]), kCf[rk * 128:(rk + 1) * 128, :], "aa_k",
                       reads=["kfull"], writes=["ckvAs"])
            kb.dma("pool", bta[:], self.d_bta, "aa_c", writes=["btas"])
            kb.dma("pool", V(wuv, 0, [(256, 8), (128, 2), (1, 128)]), self.d_wuv[l].rearrange("h (cc c) d -> c h cc d", c=128),
                   "aa_c", writes=["wuvs"])
            qiT = [self.sb(es, "aqi%d" % i, 8 * 128, BF16) for i in range(2)]
            wi = [self.sb(es, "awi%d" % i, 16, F32) for i in range(2)]
            qlat = [self.sb(es, "aql%d" % i, 16 * 128, BF16) for i in range(4)]
            dg = self.sb(es, "adg", 16 * 128, BF16)
            R = [self.sb(es, "aR%d" % i, 512, BF16) for i in range(4)]
            NK = NR * L
            scs = [self.sb(es, "asc%d" % i, NK, F32) for i in range(2)]
            bss = [self.sb(es, "abs%d" % i, 8, F32) for i in range(2)]
            junks = [self.sb(es, "ajk%d" % i, NK, BF16) for i in range(2)]
            assert NK <= 4096
            negm = [(self.wring[i // 2], (i % 2) * 4096) for i in range(4)]
            Pst = [self.sb(es, "aP%d" % i, 512, BF16) for i in range(3)]
            olat = self.sb(es, "aol", 2 * 512, BF16)
            densb = self.sb(es, "adn", 512, F32)
            ysb = self.sb(es, "ays", 512, F32)
            yst = [self.sb(es, "ayst%d" % i, 512, BF16) for i in range(2)]
            st = dict(rn=0, pn=0, yn=0, ln=0)
            LB = (4, 5, 7)

            def nkeys(s):
                return NR * (s + 1) * 128

            def stage1(s):
                b = s % 2
                t0 = s * 128
                scA, sck = scs[b], "asc%d" % b
                kb.dma("sp", V(qlat[s % 4], 0, [(128, 16), (1, 128)]), self.d_qlat[:, :, t0:t0 + 128], "aa_ql%d" % (s % 4),
                       reads=["qlat"], writes=["aql%d" % (s % 4)])
                if nkeys(s) <= 256:
                    return
                kb.dma("sp", V(qiT[b], 0, [(128, 8), (1, 128)]), self.d_qiT[:, :, t0:t0 + 128], "aa_q%d" % b,
                       reads=["qiT"], writes=["aqi%d" % b])
                kb.dma("sp", wi[b][:], self.d_wi[s], "aa_q%d" % b, reads=["wi"], writes=["awi%d" % b])
                kb.op("dve", lambda g: g.tensor_tensor(out=V(dg, 0, [(128, 16), (1, 128)]), in0=V(self.ident, 0, [(0, 16), (1, 128)]),
                                                       in1=V(wi[b], 0, [(1, 16), (0, 128)]), op=ALU.mult),
                      reads=["ident", "awi%d" % b], writes=["adg"])
                nb_r = s + 1
                for rk in range(NR):
                    cbase = rk * nb_r * 128
                    for kc in range((nb_r + 3) // 4):
                        w = min(512, nb_r * 128 - kc * 512)
                        LA = 2
                        pend = []
                        for h in range(16 + LA):
                            if h < 16:
                                pr, hf = h // 2, h % 2
                                bank = LB[st["ln"] % 3]
                                st["ln"] += 1
                                kb.mm(V(ps[bank], 0, [(1, w)]), V(qiT[b], pr * 128, [(1, 128)], p0=hf * 64, npart=64),
                                      V(kiT, rk * L + kc * 512, [(1, w)], p0=hf * 64, npart=64), True, True,
                                      reads=["aqi%d" % b, "kiTs"], writes=["ps%d" % bank])
                                r = R[st["rn"] % 4]
                                rkey = "aR%d" % (st["rn"] % 4)
                                st["rn"] += 1
                                kb.op("act", lambda g: g.activation(out=V(r, 0, [(1, w)]), in_=V(ps[bank], 0, [(1, w)]), func=AF.Relu),
                                      reads=["ps%d" % bank], writes=[rkey])
                                pend.append((r, rkey))
                            if h >= LA:
                                hd = h - LA
                                r2, rkey2 = pend[hd]
                                kb.mm(V(ps[6], 0, [(1, w)]), V(dg, hd * 128, [(1, 128)]), V(r2, 0, [(1, w)]), hd == 0, hd == 15,
                                      reads=["adg", rkey2], writes=["ps6"], last=(hd == 15))
                        kb.op("act", lambda g: g.activation(out=V(scA, cbase + kc * 512, [(1, w)]), in_=V(ps[6], 0, [(1, w)]),
                                                            func=AF.Identity), reads=["ps6"], writes=[sck])
                    kb.op("dve", lambda g: g.tensor_tensor(out=V(scA, cbase + s * 128, [(1, 128)]), in0=V(scA, cbase + s * 128, [(1, 128)]),
                                                           in1=V(self.cm, rk * 128, [(1, 128)]), op=ALU.add), reads=[sck, "cm"], writes=[sck])

            def stage2(s):
                n = nkeys(s)
                nm, nk = negm[s % 4], "anegm%d" % (s % 4)
                p = s % 2
                sc, sck = scs[p], "asc%d" % p
                bs_, bk = bss[p], "abs%d" % p
                jk_, jkk = junks[p], "ajk%d" % p
                if n <= 256:
                    kb.op("pool", lambda g: g.memset(V(nm[0], nm[1], [(1, n)]), 0.0), writes=[nk])
                    return
                lo, d0, mid, cnt, g2 = [V(bs_, i, [(1, 1)]) for i in range(5)]
                scv = V(sc, 0, [(1, n)])
                kb.op("dve", lambda g: g.reduce_max(out=d0, in_=scv, axis=mybir.AxisListType.X), reads=[sck], writes=[bk])
                yield
                kb.op("dve", lambda g: g.tensor_scalar(out=d0, in0=d0, scalar1=31.0, scalar2=None, op0=ALU.add), reads=[bk], writes=[bk])
                yield
                kb.op("dve", lambda g: g.memset(lo, -30.0), reads=[bk], writes=[bk])
                yield
                kb.op("dve", lambda g: g.scalar_tensor_tensor(out=mid, in0=d0, scalar=0.5, in1=lo, op0=ALU.mult, op1=ALU.add),
                      reads=[bk], writes=[bk])
                yield
                NIT = 26
                for k in range(NIT):
                    kb.op("dve", lambda g: g.tensor_scalar(out=V(jk_, 0, [(1, n)]), in0=scv, scalar1=mid, scalar2=0.0, op0=ALU.is_ge,
                                                           op1=ALU.add, accum_out=cnt), reads=[sck, bk], writes=[bk, jkk])
                    yield
                    kb.op("dve", lambda g: g.tensor_scalar(out=g2, in0=cnt, scalar1=255.5, scalar2=2.0 ** -(k + 1), op0=ALU.is_gt,
                                                           op1=ALU.mult), reads=[bk], writes=[bk])
                    yield
                    kb.op("dve", lambda g: g.scalar_tensor_tensor(out=lo, in0=d0, scalar=g2, in1=lo, op0=ALU.mult, op1=ALU.add),
                          reads=[bk], writes=[bk])
                    yield
                    if k < NIT - 1:
                        kb.op("dve", lambda g: g.scalar_tensor_tensor(out=mid, in0=d0, scalar=2.0 ** -(k + 2), in1=lo, op0=ALU.mult,
                                                                      op1=ALU.add), reads=[bk], writes=[bk])
                        yield
                kb.op("dve", lambda g: g.tensor_scalar(out=V(nm[0], nm[1], [(1, n)]), in0=scv, scalar1=lo, scalar2=NEG, op0=ALU.is_lt,
                                                       op1=ALU.mult), reads=[sck, bk], writes=[nk])
                yield

            def inter(*gens):
                gens = list(gens)
                while gens:
                    for g_ in list(gens):
                        try:
                            next(g_)
                        except StopIteration:
                            gens.remove(g_)

            def stage3(s):
                b = s % 4
                t0 = s * 128
                nm, nk = negm[b], "anegm%d" % b
                nb_r = s + 1
                keys = [(rk, sj) for rk in range(NR) for sj in range(nb_r)]
                def emitS(g4, idx):
                    rk, sj = keys[idx]
                    j = NR * sj + rk
                    dl = min(NR * s + NR - 1 - j, NTA - 1)
                    sb_ = LB[st["pn"] % 3]
                    sk = "ps%d" % sb_
                    kb.mm(ps[sb_][:], self.ident[:], V(bta, (dl * 8 + g4 * 4) * 128, [(1, 512)]), True, False,
                          reads=["ident", "btas"], writes=[sk], last=False)
                    kb.mm(ps[sb_][:], V(nm[0], nm[1] + (rk * nb_r + sj) * 128, [(1, 128)]), V(self.ident, 0, [(0, 4), (1, 128)]), False, False,
                          reads=[nk, "ident"], writes=[sk], last=False)
                    for cc in range(2):
                        kb.mm(ps[sb_][:], V(ckvT, (rk * 2 + cc) * L + sj * 128, [(1, 128)]),
                              V(qlat[b], (cc * 8 + g4 * 4) * 128, [(1, 512)]), False, cc == 1,
                              reads=["ckvTs", "aql%d" % b], writes=[sk], last=(cc == 1))
                    pt = Pst[st["pn"] % 3]
                    pk_ = "aP%d" % (st["pn"] % 3)
                    st["pn"] += 1
                    kb.op("act", lambda g: g.activation(out=pt[:], in_=ps[sb_][:], func=AF.Exp), reads=[sk], writes=[pk_])
                    return pt, pk_

                def emitPV(g4, idx, pt, pk_):
                    rk, sj = keys[idx]
                    first, lastk = idx == 0, idx == len(keys) - 1
                    for cc in range(2):
                        kb.mm(ps[cc][:], V(ckvA, (rk * NB + sj) * 256 + cc * 128, [(1, 128)]), pt[:], first, lastk,
                              reads=["ckvAs", pk_], writes=["ps%d" % cc], last=lastk)
                    kb.mm(ps[2][:], self.ones[:], pt[:], first, lastk, reads=["ones", pk_], writes=["ps2"], last=lastk)
                    if not lastk:
                        return
                    for cc in range(2):
                        self.evac(V(olat, cc * 512, [(1, 512)]), ps[cc][:], 1.0, ["ps%d" % cc], ["aol"], eng="act")
                    kb.op("act", lambda g: g.activation(out=densb[:], in_=ps[2][:], func=AF.Ln), reads=["ps2"], writes=["adn"])
                    kb.op("act", lambda g: g.activation(out=densb[:], in_=densb[:], func=AF.Exp, scale=-1.0), reads=["adn"], writes=["adn"])
                    for h in range(4):
                        hh = g4 * 4 + h
                        for cc in range(2):
                            kb.mm(V(ps[3], h * 128, [(1, 128)]), V(wuv, hh * 256 + cc * 128, [(1, 128)]),
                                  V(olat, cc * 512 + h * 128, [(1, 128)]), cc == 0, cc == 1, reads=["wuvs", "aol"], writes=["ps3"],
                                  last=(cc == 1))
                    self.evac(ysb[:], ps[3][:], 1.0, ["ps3"], ["ays"], eng="act")
                    y = yst[st["yn"] % 2]
                    yk = "ayst%d" % (st["yn"] % 2)
                    st["yn"] += 1
                    kb.op("pool", lambda g: g.tensor_tensor(out=y[:], in0=ysb[:], in1=densb[:], op=ALU.mult),
                          reads=["ays", "adn"], writes=[yk])
                    kb.dma("sp", self.d_ya[:, g4 * 4:(g4 + 1) * 4, t0:t0 + 128], V(y, 0, [(128, 4), (1, 128)]), yk,
                           reads=[yk], writes=["yaT"])

                prev = None
                for g4 in range(2):
                    for idx in range(len(keys)):
                        cur = (g4, idx) + emitS(g4, idx)
                        if prev is not None:
                            emitPV(*prev)
                        prev = cur
                emitPV(*prev)

            stage1(0)
            stage1(1)
            inter(stage2(0), stage2(1))
            for s in range(0, NB, 2):
                if s + 2 < NB:
                    stage1(s + 2)
                    stage1(s + 3)
                stage3(s)
                stage3(s + 1)
                if s + 2 < NB:
                    inter(stage2(s + 2), stage2(s + 3))
            kb.barrier()

    def phase_att_b(self, l):
        kb, ps, L, NB, NR = self.kb, self.ps, self.L, self.NB, self.NR
        kKf = [self.kK[l][g][1] for g in range(3)]
        kVf = [self.kV[l][g][1] for g in range(3)]
        NRING, NTB, OFFB = self.NRING, self.NTB, self.OFFB
        with ExitStack() as es:
            btb = self.sb(es, "btbs", sum(NTB) * 4 * 128, BF16)
            kb.dma("pool", btb[:], self.d_btb, "ab_c", writes=["btbs"])
            kr = [self.sb(es, "bkr%d" % g, NRING[g] * 512, BF16) for g in range(3)]
            vr = [self.sb(es, "bvr%d" % g, NRING[g] * 512, BF16) for g in range(3)]
            qb = [self.sb(es, "bq%d" % i, 12 * 128, BF16) for i in range(2)]
            Pst = [self.sb(es, "bP%d" % i, 512, BF16) for i in range(3)]
            rden = self.sb(es, "brd", 512, F32)
            yst = [self.sb(es, "byst%d" % i, 512, BF16) for i in range(2)]
            pn = 0
            for s in range(NB):
                b = s % 2
                t0 = s * 128
                kb.dma("sp", V(qb[b], 0, [(128, 12), (1, 128)]), self.d_qb[:, :, t0:t0 + 128], "ab_q%d" % b, reads=["qb"],
                       writes=["bq%d" % b])
                for rk in range(NR):
                    j = NR * s + rk
                    for g in range(3):
                        sl = j % NRING[g]
                        kb.dma("sp", V(kr[g], sl * 512, [(128, 4), (1, 128)]),
                               kKf[g][rk * 128:(rk + 1) * 128, :].rearrange("p (h t) -> p h t", h=4)[:, :, t0:t0 + 128],
                               "ab_k%d_%d" % (g, s % 2), reads=["kfull"], writes=["bkr%d_%d" % (g, sl)])
                        kb.dma("sp", V(vr[g], sl * 512, [(1, 512)]), kVf[g][rk * 128:(rk + 1) * 128, s * 512:(s + 1) * 512],
                               "ab_v%d_%d" % (g, s % 2), reads=["kfull"], writes=["bvr%d_%d" % (g, sl)])
                jtop = NR * s + NR - 1
                pairs = [(g, dl) for g in range(3) for dl in range(NTB[g]) if jtop - dl >= 0]
                def emitS(idx):
                    nonlocal pn
                    g, dl = pairs[idx]
                    j = jtop - dl
                    sl = j % NRING[g]
                    sb_ = 5 + (pn % 2)
                    sk = "ps%d" % sb_
                    kb.mm(ps[sb_][:], self.ident[:], V(btb, (OFFB[g] + dl) * 512, [(1, 512)]), True, False,
                          reads=["ident", "btbs"], writes=[sk], last=False)
                    for hg in range(4):
                        kb.mm(V(ps[sb_], hg * 128, [(1, 128)]), V(kr[g], sl * 512 + hg * 128, [(1, 128)]),
                              V(qb[b], (g * 4 + hg) * 128, [(1, 128)]), False, hg == 3,
                              reads=["bkr%d_%d" % (g, sl), "bq%d" % b], writes=[sk], last=(hg == 3))
                    pt = Pst[pn % 3]
                    pk_ = "bP%d" % (pn % 3)
                    pn += 1
                    kb.op("act", lambda g_: g_.activation(out=pt[:], in_=ps[sb_][:], func=AF.Exp), reads=[sk], writes=[pk_])
                    return pt, pk_

                def emitPV(idx, pt, pk_):
                    g, dl = pairs[idx]
                    sl = (jtop - dl) % NRING[g]
                    first, lastp = idx == 0, idx == len(pairs) - 1
                    for hg in range(4):
                        kb.mm(V(ps[0], hg * 128, [(1, 128)]), V(vr[g], sl * 512 + hg * 128, [(1, 128)]),
                              V(pt, hg * 128, [(1, 128)]), first and hg == 0, lastp, reads=["bvr%d_%d" % (g, sl), pk_], writes=["ps0"],
                              last=(lastp and hg == 3))
                    kb.mm(ps[1][:], self.ones[:], pt[:], first, lastp, reads=["ones", pk_], writes=["ps1"], last=lastp)

                prev = None
                for idx in range(len(pairs)):
                    cur = (idx,) + emitS(idx)
                    if prev is not None:
                        emitPV(*prev)
                    prev = cur
                emitPV(*prev)
                kb.op("dve", lambda g_: g_.reciprocal(out=rden[:], in_=ps[1][:]), reads=["ps1"], writes=["brd"])
                y = yst[s % 2]
                yk = "byst%d" % (s % 2)
                kb.op("dve", lambda g_: g_.tensor_tensor(out=y[:], in0=ps[0][:], in1=rden[:], op=ALU.mult),
                      reads=["ps0", "brd"], writes=[yk])
                kb.dma("sp", self.d_yb[:, :, t0:t0 + 128], V(y, 0, [(128, 4), (1, 128)]), yk, reads=[yk], writes=["ybT"])
            kb.barrier()

    def phase_p4(self, l, xsrc, t0, QT, es):
        kb, ps, L = self.kb, self.ps, self.L
        ya = self.sb(es, "p4ya", 8 * QT, BF16)
        yb = self.sb(es, "p4yb", 4 * QT, BF16)
        mg = self.sb(es, "p4mg", 16 * QT, BF16)
        wa = self.sb(es, "p4wa", 8 * D, BF16)
        wb = self.sb(es, "p4wb", 4 * D, BF16)
        gt = [self.sb(es, "p4g%d" % i, 2 * QT, BF16) for i in range(2)]
        m1 = [self.sb(es, "p4m%d" % i, 512, F32) for i in range(2)]
        xo = [self.sb(es, "p4x%d" % i, 512, F32) for i in range(2)]
        kb.dma("sp", V(ya, 0, [(QT, 8), (1, QT)]), self.d_ya[:, :, t0:t0 + QT], "p4y", reads=["yaT"], writes=["p4ya"])
        kb.dma("sp", V(yb, 0, [(QT, 4), (1, QT)]), self.d_yb[:, :, t0:t0 + QT], "p4y", reads=["ybT"], writes=["p4yb"])
        kb.dma("pool", V(wa, 0, [(D, 8), (1, D)]), self.d_waup[l].rearrange("(h p) c -> p h c", p=128), "p4w", writes=["p4wa"])
        kb.dma("pool", V(wb, 0, [(D, 4), (1, D)]), self.d_wbup[l].rearrange("(h p) c -> p h c", p=128), "p4w", writes=["p4wb"])
        NT = QT // 512
        for oc in range(16):
            gb_ = oc % 2
            gk = "p4g%d" % gb_
            kb.dma("sp", V(gt[gb_], 0, [(1, QT)]), self.d_gates[:, oc, t0:t0 + QT], gk, reads=["gates"], writes=[gk])
            kb.dma("sp", V(gt[gb_], QT, [(1, QT)]), self.d_gates[:, 16 + oc, t0:t0 + QT], gk, reads=["gates"], writes=[gk])
            for ti in range(NT):
                for h in range(8):
                    kb.mm(ps[0][:], V(wa, h * D + oc * 128, [(1, 128)]), V(ya, h * QT + ti * 512, [(1, 512)]), h == 0, h == 7,
                          reads=["p4wa", "p4ya"], writes=["ps0"], last=(h == 7))
                for h in range(4):
                    kb.mm(ps[1][:], V(wb, h * D + oc * 128, [(1, 128)]), V(yb, h * QT + ti * 512, [(1, 512)]), h == 0, h == 3,
                          reads=["p4wb", "p4yb"], writes=["ps1"], last=(h == 3))
                kb.op("dve", lambda g: g.tensor_tensor(out=m1[0][:], in0=ps[0][:], in1=V(gt[gb_], ti * 512, [(1, 512)]), op=ALU.mult),
                      reads=["ps0", gk], writes=["p4m0"])
                kb.op("dve", lambda g: g.tensor_tensor(out=m1[1][:], in0=ps[1][:], in1=V(gt[gb_], QT + ti * 512, [(1, 512)]), op=ALU.mult),
                      reads=["ps1", gk], writes=["p4m1"])
                kb.op("pool", lambda g: g.tensor_tensor(out=V(mg, oc * QT + ti * 512, [(1, 512)]), in0=m1[0][:], in1=m1[1][:], op=ALU.add),
                      reads=["p4m0", "p4m1"], writes=["p4mg"])
        n = 0
        for grp in range(4):
            wv_, wk, s = self.load_w(self.d_wout[l][:, grp * 512:(grp + 1) * 512].rearrange("(k p) c -> p k c", p=128),
                                     [(512, 16), (1, 512)])
            for c in range(4):
                oc = grp * 4 + c
                for ti in range(NT):
                    tt = t0 + ti * 512
                    bank = 2 + n % 2
                    xb = n % 2
                    n += 1
                    xk = "p4x%d" % xb
                    kb.dma("sp", xo[xb][:], xsrc[oc, :, tt:tt + 512], xk, reads=["x_t%d" % (tt // 512)], writes=[xk])
                    for k in range(16):
                        kb.mm(ps[bank][:], V(self.wring[s], k * 512 + c * 128, [(1, 128)]), V(mg, k * QT + ti * 512, [(1, 512)]),
                              k == 0, k == 15, reads=[wk, "p4mg"], writes=["ps%d" % bank], last=(k == 15))
                    kb.op("dve", lambda g: g.scalar_tensor_tensor(out=xo[xb][:], in0=ps[bank][:], scalar=V(self.mod[l], 32 + oc, [(1, 1)]),
                                                                  in1=xo[xb][:], op0=ALU.mult, op1=ALU.add),
                          reads=["ps%d" % bank, xk], writes=[xk])
                    kb.dma("sp", self.d_xs[oc, :, tt:tt + 512], xo[xb][:], xk, reads=[xk], writes=["x_t%d" % (tt // 512)])

    def phase_ffn(self, l, t0, QT, hT, es):
        kb, ps, L = self.kb, self.ps, self.L
        act = self.sb(es, "fact", FC * QT, BF16)
        sg = [self.sb(es, "fsg%d" % i, 512, F32) for i in range(2)]
        xo = [self.sb(es, "fx%d" % i, 512, F32) for i in range(2)]
        NT = QT // 512
        n = 0
        for cg in range(DFF // 256):
            s = self.wr_i % 2
            self.wr_i += 1
            wk = "wring%d" % s
            kb.dma("pool", V(self.wring[s], 0, [(256, 16), (1, 256)]),
                   self.d_wfg[l][:, cg * 256:(cg + 1) * 256].rearrange("(k p) c -> p k c", p=128), wk, writes=[wk])
            kb.dma("pool", V(self.wring[s], 4096, [(256, 16), (1, 256)]),
                   self.d_wfu[l][:, cg * 256:(cg + 1) * 256].rearrange("(k p) c -> p k c", p=128), wk, writes=[wk])
            for c in range(2):
                f = cg * 2 + c
                for ti in range(NT):
                    bg, bu = 2 + 2 * (n % 2), 3 + 2 * (n % 2)
                    sgi = n % 2
                    n += 1
                    for k in range(16):
                        kb.mm(ps[bg][:], V(self.wring[s], k * 256 + c * 128, [(1, 128)]), V(hT, k * QT + ti * 512, [(1, 512)]),
                              k == 0, k == 15, reads=[wk, "hT"], writes=["ps%d" % bg], last=(k == 15))
                    for k in range(16):
                        kb.mm(ps[bu][:], V(self.wring[s], 4096 + k * 256 + c * 128, [(1, 128)]), V(hT, k * QT + ti * 512, [(1, 512)]),
                              k == 0, k == 15, reads=[wk, "hT"], writes=["ps%d" % bu], last=(k == 15))
                    kb.op("act", lambda g: g.activation(out=sg[sgi][:], in_=ps[bg][:], func=AF.Silu), reads=["ps%d" % bg],
                          writes=["fsg%d" % sgi])
                    kb.op("dve", lambda g: g.tensor_tensor(out=V(act, f * QT + ti * 512, [(1, 512)]), in0=ps[bu][:], in1=sg[sgi][:],
                                                           op=ALU.mult), reads=["ps%d" % bu, "fsg%d" % sgi], writes=["fact"])
        n = 0
        for oc in range(16):
            s = self.wr_i % 2
            self.wr_i += 1
            wk = "wring%d" % s
            kb.dma("pool", V(self.wring[s], 0, [(128, FC), (1, 128)]),
                   self.d_wfd[l][:, oc * 128:(oc + 1) * 128].rearrange("(f p) c -> p f c", p=128), wk, writes=[wk])
            for ti in range(NT):
                tt = t0 + ti * 512
                bank = n % 2
                xb = n % 2
                n += 1
                xk = "fx%d" % xb
                kb.dma("sp", xo[xb][:], self.d_xs[oc, :, tt:tt + 512], xk, reads=["x_t%d" % (tt // 512)], writes=[xk])
                for f in range(FC):
                    kb.mm(ps[bank][:], V(self.wring[s], f * 128, [(1, 128)]), V(act, f * QT + ti * 512, [(1, 512)]), f == 0,
                          f == FC - 1, reads=[wk, "fact"], writes=["ps%d" % bank], last=(f == FC - 1))
                kb.op("dve", lambda g: g.scalar_tensor_tensor(out=xo[xb][:], in0=ps[bank][:], scalar=V(self.mod[l], 80 + oc, [(1, 1)]),
                                                              in1=xo[xb][:], op0=ALU.mult, op1=ALU.add),
                      reads=["ps%d" % bank, xk], writes=[xk])
                kb.dma("sp", self.d_xs[oc, :, tt:tt + 512], xo[xb][:], xk, reads=[xk], writes=["x_t%d" % (tt // 512)])

    def build(self):
        kb, L, QT = self.kb, self.L, self.QT
        self.epsb = self.nc.alloc_sbuf_tensor("epsb", [128, 1], F32)
        kb.op("pool", lambda g: g.memset(self.epsb[:], EPS), writes=["epsb"])
        self.phase_const()
        self.phase_mod()
        ph = self.phases
        for l in range(self.NL):
            xsrc = self.d_xT if l == 0 else self.d_xs
            if ph is None or "p1" in ph:
                with ExitStack() as es:
                    QT1 = L
                    hT = self.sb(es, "hT", 16 * QT1, BF16)
                    with ExitStack() as es2:
                        self.norm_tiles(es2, xsrc, self.A1[l], (self.mod[l], 0), 0, QT1, hT)
                        kb.barrier()
                    with ExitStack() as es2:
                        self.phase_p1(l, 0, QT1, hT, es2, "k")
                        kb.barrier()
                    if self.NR > 1:
                        rg = [[c * self.NR + i for i in range(self.NR)] for c in range(8 // self.NR)]
                        for ci, (loc, full) in enumerate([self.kA[l], self.kC[l]] + self.kK[l] + self.kV[l]):
                            kb.cc(lambda g: g.collective_compute("AllGather", ALU.bypass, replica_groups=rg, ins=[loc], outs=[full]),
                                  "ag%d_%d" % (l, ci), reads=["kloc"], writes=["kfull"])
                    with ExitStack() as es2:
                        self.phase_p1(l, 0, QT1, hT, es2, "q")
                        kb.barrier()
            if ph is None or "atta" in ph:
                self.phase_att_a(l)
            if ph is None or "attb" in ph:
                self.phase_att_b(l)
            if ph is None or "p4" in ph:
                for q in range(L // QT):
                    with ExitStack() as es:
                        self.phase_p4(l, xsrc, q * QT, QT, es)
                        kb.barrier()
                    with ExitStack() as es:
                        hT = self.sb(es, "hT", 16 * QT, BF16)
                        with ExitStack() as es2:
                            self.norm_tiles(es2, self.d_xs, self.A2[l], (self.mod[l], 48), q * QT, QT, hT)
                            kb.barrier()
                        self.phase_ffn(l, q * QT, QT, hT, es)
                        kb.barrier()
        toks = []
        with ExitStack() as es:
            toks = self.norm_tiles(es, self.d_xs, self.fg, None, 0, L, None, final=True)
        for t in toks:
            kb._wait("sp", t)
        for nm in self.dbg:
            pass
        kb.barrier()
        return self.nc


def _rel_bucket(n):
    n = np.maximum(n, 0)
    nf = np.maximum(n, 1).astype(np.float32)
    large = 16 + (np.log(nf / np.float32(16)) / np.float32(math.log(2048 / 16)) * np.float32(16)).astype(np.int32)
    return np.where(n < 16, n, np.minimum(large, 31))


def _static_tables(LG, NR, r, rel_bias):
    L = LG // NR
    NB = L // 128
    k = np.arange(128)[:, None]
    q = np.arange(128)[None, :]
    negf = np.float32(NEG)
    sh = NR - 1 - r
    NTA = 17 + NR
    bta = np.full((128, NTA, 8, 128), negf, np.float32)
    for dl in range(NTA):
        d = dl - sh
        if d < 0:
            continue
        dist = min(d, 17) * 128 + q - k
        val = rel_bias[_rel_bucket(dist)][:, :, :8]
        val = np.where((dist >= 0)[:, :, None], val, negf)
        bta[:, dl] = np.transpose(val, (0, 2, 1))
    NTB = [n + NR - 1 for n in NBLK_G]
    btb = np.full((128, sum(NTB), 4, 128), negf, np.float32)
    off = 0
    for g, (win, dil) in enumerate(B_GROUPS):
        for dl in range(NTB[g]):
            d = dl - sh
            if 0 <= d < NBLK_G[g]:
                dist = d * 128 + q - k
                ok = (dist >= 0) & (dist <= win) & (dist % dil == 0)
                val = rel_bias[_rel_bucket(dist)][:, :, 8 + g * 4: 8 + g * 4 + 4]
                val = np.where(ok[:, :, None], val, negf)
                btb[:, off + dl] = np.transpose(val, (0, 2, 1))
        off += NTB[g]
    cm = np.where(np.arange(128)[None, :] <= np.arange(128)[:, None], np.float32(0), np.float32(-1e30)).astype(np.float32)
    cmr = np.zeros((128, NR, 128), np.float32)
    for rk in range(NR):
        if rk == r:
            cmr[:, rk] = cm
        elif rk > r:
            cmr[:, rk] = np.float32(-1e30)
    blocks = np.array([NR * s + r for s in range(NB)])
    pos = (blocks[:, None] * 128 + np.arange(128)[None, :]).astype(np.float32)
    freqs = (np.float32(10000.0) ** (-np.arange(16, dtype=np.float32) / np.float32(16))).astype(np.float32)
    ang = pos[:, :, None] * freqs[None, None, :]
    ropeC = np.cos(ang).astype(np.float32).transpose(1, 0, 2)
    ropeS = np.sin(ang).astype(np.float32).transpose(1, 0, 2)
    return dict(bta=np.ascontiguousarray(bta.reshape(128, -1)), btb=np.ascontiguousarray(btb.reshape(128, -1)),
                cmr=np.ascontiguousarray(cmr.reshape(128, -1)), ropeC=np.ascontiguousarray(ropeC),
                ropeS=np.ascontiguousarray(ropeS), ident=np.eye(128, dtype=np.float32))


def _pk(v):
    v = np.asarray(v, np.float32)
    return np.ascontiguousarray(np.swapaxes(v.reshape(v.shape[:-1] + (-1, 128)), -1, -2))


def make_in_maps(inp, LG, NR, cores):
    f = lambda a: np.ascontiguousarray(np.asarray(a, np.float32))
    shared = dict(
        w_ada=f(inp["w_ada"]), b_adaT=_pk(inp["b_ada"]), n1g=_pk(inp["norm1_g"]), n2g=_pk(inp["norm2_g"]), fg=_pk(inp["final_g"]),
        w_in=f(inp["w_in"]),
        kvg=np.ascontiguousarray(np.broadcast_to(f(inp["kv_norm_g"])[:, None, :], (len(inp["kv_norm_g"]), 128, 256))),
        ilg=np.ascontiguousarray(np.broadcast_to(f(inp["idx_ln_g"])[:, None, :], (len(inp["idx_ln_g"]), 128, 64))),
        ilb=np.ascontiguousarray(np.broadcast_to(f(inp["idx_ln_b"])[:, None, :], (len(inp["idx_ln_b"]), 128, 64))),
        w_uk=f(inp["w_uk"]), w_uv=f(inp["w_uv"]), w_a_up=f(inp["w_a_up"]), w_b_up=f(inp["w_b_up"]), w_out=f(inp["w_out"]),
        w_ffg=f(inp["w_ff_gate"]), w_ffu=f(inp["w_ff_up"]), w_ffd=f(inp["w_ff_down"]),
    )
    tabs = [_static_tables(LG, NR, r, f(inp["rel_bias"])) for r in range(NR)]
    x = f(inp["x"])
    c = f(inp["c"])
    L = LG // NR
    maps = []
    for b, r in cores:
        m = dict(shared)
        m.update(tabs[r])
        xb = x[b].reshape(LG // 128, 128, D)[r::NR].reshape(L, D)
        m["xT"] = np.ascontiguousarray(xb.T.reshape(KC, 128, L))
        m["c_pk"] = _pk(c[b])
        maps.append(m)
    return maps


_CACHE = {}
NR_FULL = 2


def kernel(**inputs):
    x = np.asarray(inputs["x"])
    B, LG, _ = x.shape
    nl = np.asarray(inputs["w_ada"]).shape[0]
    NR = NR_FULL
    key = (LG, nl, NR)
    if key not in _CACHE:
        _CACHE[key] = Prog(LG, nl, NR=NR).build()
    nc = _CACHE[key]
    cores = [((c // NR) % B, c % NR) for c in range(8)]
    in_maps = make_in_maps(inputs, LG, NR, cores)
    res = run_bass_kernel_spmd(nc, in_maps, core_ids=list(range(8)))
    L = LG // NR
    out = np.empty((B, LG // 128, 128, D), np.float32)
    for c, (b, r) in enumerate(cores):
        if c // NR >= B:
            continue
        o = np.asarray(res.results[c]["outT"]).reshape(D, L).T
        out[b, r::NR] = o.reshape(L // 128, 128, D)
    return out.reshape(B, LG, D)
```

```python
import math
from contextlib import ExitStack
import numpy as np
import concourse.bass as bass
import concourse.mybir as mybir
from concourse.bass_utils import run_bass_kernel_spmd

F32 = mybir.dt.float32
BF16 = mybir.dt.bfloat16
ALU = mybir.AluOpType
AF = mybir.ActivationFunctionType

D = 2048
KC = 16
NIN = 11088
DFF = 5632
FC = 44
NEG = -30000.0
EPS = 1e-6
C_QA, C_CKV, C_QI, C_KI, C_WI, C_QB, C_KB, C_VB, C_GATE = 0, 1024, 1280, 2304, 2368, 2384, 3920, 5456, 6992
B_GROUPS = ((128, 1), (512, 4), (2048, 16))
NBLK_G = (2, 5, 17)
OFF_G = (0, 2, 7)
NRING = (3, 6, 18)


class KB:
    def __init__(self, nc):
        self.nc = nc
        self.eng = {"pe": nc.tensor, "dve": nc.vector, "act": nc.scalar, "pool": nc.gpsimd, "sp": nc.sync}
        self.esem = {e: nc.alloc_semaphore("prog_" + e) for e in ("pe", "dve", "act", "pool")}
        self.cnt = {e: 0 for e in self.esem}
        self.dsem, self.dcnt = {}, {}
        self.waited, self.lastw, self.readers = {}, {}, {}
        self.ninst = 0

    def _sem(self, key):
        return self.esem[key[1]] if key[0] == "e" else self.dsem[key[1]]

    def _wait(self, e, tok):
        if tok is None:
            return
        key, val = tok
        if key == ("e", "pe") and e == "pe":
            return
        if key[0] == "d":
            val = self.dcnt[key[1]]
        k = (e, key)
        if self.waited.get(k, 0) >= val:
            return
        self.waited[k] = val
        self.eng[e].wait_ge(self._sem(key), val)
        self.ninst += 1

    def _deps(self, e, reads, writes):
        for r in reads:
            self._wait(e, self.lastw.get(r))
        for w in writes:
            self._wait(e, self.lastw.get(w))
            for t in self.readers.get(w, ()):
                self._wait(e, t)

    def _commit(self, tok, reads, writes):
        for r in reads:
            lst = self.readers.setdefault(r, [])
            lst.append(tok)
            if len(lst) > 12:
                best = {}
                for k, v in lst:
                    best[k] = max(best.get(k, 0), v)
                self.readers[r] = list(best.items())
        for w in writes:
            self.lastw[w] = tok
            self.readers[w] = []

    def op(self, e, fn, reads=(), writes=()):
        self._deps(e, reads, writes)
        ins = fn(self.eng[e])
        self.cnt[e] += 1
        ins.then_inc(self.esem[e], 1)
        tok = (("e", e), self.cnt[e])
        self._commit(tok, reads, writes)
        self.ninst += 1
        return tok

    def mm(self, out, lhsT, rhs, start, stop, reads=(), writes=(), last=True):
        e = "pe"
        self._deps(e, reads, writes)
        ins = self.nc.tensor.matmul(out, lhsT, rhs, start=start, stop=stop)
        self.ninst += 1
        if last:
            self.cnt[e] += 1
            ins.then_inc(self.esem[e], 1)
            tok = (("e", e), self.cnt[e])
        else:
            tok = (("e", e), self.cnt[e] + 1)
        self._commit(tok, reads, writes)
        return tok

    def dma(self, q, out, in_, slot, reads=(), writes=()):
        if slot not in self.dsem:
            self.dsem[slot] = self.nc.alloc_semaphore("d_" + slot)
            self.dcnt[slot] = 0
        self._deps(q, reads, writes)
        ins = self.eng[q].dma_start(out=out, in_=in_)
        self.dcnt[slot] += 16
        ins.then_inc(self.dsem[slot], 16)
        tok = (("d", slot), self.dcnt[slot])
        self._commit(tok, reads, writes)
        self.ninst += 1
        return tok

    def cc(self, fn, name, reads=(), writes=()):
        self.dsem[name] = self.nc.alloc_semaphore("cc_" + name)
        self._deps("pool", reads, writes)
        ins = fn(self.eng["pool"])
        ins.then_inc(self.dsem[name], 1)
        self.dcnt[name] = 1
        tok = (("d", name), 1)
        self._commit(tok, reads, writes)
        self.ninst += 1
        return tok

    def barrier(self):
        for e in self.eng:
            for e2 in self.esem:
                if e2 != e and self.cnt[e2] > 0:
                    self._wait(e, (("e", e2), self.cnt[e2]))
            for s, v in self.dcnt.items():
                if v > 0:
                    self._wait(e, (("d", s), v))
        self.lastw.clear()
        self.readers.clear()


def V(t, off, dims, p0=0, npart=128):
    base = t[:]
    ps = base.ap[0][0]
    return bass.AP(base.tensor, base.offset + p0 * ps + off, [[ps, npart]] + [[int(s), int(c)] for s, c in dims])


class Prog:
    def __init__(self, LG, nl=2, dbg=(), phases=None, NR=1):
        L = LG // NR
        self.LG, self.NR = LG, NR
        self.L, self.NB, self.NL = L, L // 128, nl
        self.NTA = 17 + NR
        self.NTB = [n + NR - 1 for n in NBLK_G]
        self.OFFB = [0, self.NTB[0], self.NTB[0] + self.NTB[1]]
        self.NRING = [n - 1 + 2 * NR for n in NBLK_G]
        self.O_KIT, self.O_KB, self.O_CKVA = 2 * L, 3 * L, 15 * L
        self.O_VB = 15 * L + (L // 128) * 256
        self.KCOLS = 15 * L + (L // 128) * 1792
        self.QT = min(1024, L)
        self.dbg = set(dbg)
        self.phases = phases
        nc = self.nc = bass.Bass("TRN2", target_bir_lowering=False)
        self.kb = KB(nc)
        self.ev_i = 0
        self.wr_i = 0
        NB = self.NB

        def din(name, shape):
            return nc.dram_tensor(name, list(shape), F32, kind="ExternalInput").ap()

        def dscr(name, shape, dt):
            kind = "ExternalOutput" if name in self.dbg else "Internal"
            return nc.dram_tensor(name, list(shape), dt, kind=kind).ap()

        self.d_xT = din("xT", [KC, 128, L])
        self.d_c = din("c_pk", [128, KC])
        self.d_wada = din("w_ada", [nl, D, 6 * D])
        self.d_bada = din("b_adaT", [nl, 128, 96])
        self.d_n1g = din("n1g", [nl, 128, KC])
        self.d_n2g = din("n2g", [nl, 128, KC])
        self.d_fg = din("fg", [128, KC])
        self.d_win = din("w_in", [nl, D, NIN])
        self.d_kvg = din("kvg", [nl, 128, 256])
        self.d_ilg = din("ilg", [nl, 128, 64])
        self.d_ilb = din("ilb", [nl, 128, 64])
        self.d_wuk = din("w_uk", [nl, 8, 128, 256])
        self.d_wuv = din("w_uv", [nl, 8, 256, 128])
        self.d_waup = din("w_a_up", [nl, 1024, D])
        self.d_wbup = din("w_b_up", [nl, 512, D])
        self.d_wout = din("w_out", [nl, D, D])
        self.d_wfg = din("w_ffg", [nl, D, DFF])
        self.d_wfu = din("w_ffu", [nl, D, DFF])
        self.d_wfd = din("w_ffd", [nl, DFF, D])
        self.d_ropeC = din("ropeC", [128, NB, 16])
        self.d_ropeS = din("ropeS", [128, NB, 16])
        self.d_bta = din("bta", [128, self.NTA * 8 * 128])
        self.d_btb = din("btb", [128, sum(self.NTB) * 4 * 128])
        self.d_cm = din("cmr", [128, NR * 128])
        self.d_ident = din("ident", [128, 128])
        self.d_out = nc.dram_tensor("outT", [KC, 128, L], F32, kind="ExternalOutput").ap()
        self.d_xs = dscr("xs", [KC, 128, L], F32)
        self.d_qlat = dscr("qlat", [128, 16, L], BF16)
        self.d_qiT = dscr("qiT", [128, 8, L], BF16)
        self.d_wi = dscr("wi", [NB, 128, 16], F32)
        self.d_qb = dscr("qb", [128, 12, L], BF16)
        self.d_gates = dscr("gates", [128, 32, L], BF16)
        def kt(name, cols):
            loc = dscr(name + "_l", [128, cols], BF16)
            full = loc if NR == 1 else dscr(name + "_f", [NR * 128, cols], BF16)
            return loc, full
        self.kA = [kt("kA%d" % l, 3 * L) for l in range(nl)]
        self.kC = [kt("kC%d" % l, NB * 256) for l in range(nl)]
        self.kK = [[kt("kK%d_%d" % (l, g), 4 * L) for g in range(3)] for l in range(nl)]
        self.kV = [[kt("kV%d_%d" % (l, g), NB * 512) for g in range(3)] for l in range(nl)]
        self.d_ya = dscr("yaT", [128, 8, L], BF16)
        self.d_yb = dscr("ybT", [128, 4, L], BF16)

        def sbp(name, f, dt):
            return nc.alloc_sbuf_tensor(name, [128, f], dt)

        self.ident = sbp("identb", 128, BF16)
        self.ones = sbp("onesb", 128, BF16)
        self.cm = sbp("cmf", NR * 128, F32)
        self.ropeC = sbp("ropeCs", NB * 16, F32)
        self.ropeS = sbp("ropeSs", NB * 16, F32)
        self.mod = [sbp("mod%d" % l, 96, F32) for l in range(nl)]
        self.A1 = [sbp("A1_%d" % l, 16, F32) for l in range(nl)]
        self.A2 = [sbp("A2_%d" % l, 16, F32) for l in range(nl)]
        self.fg = sbp("fgs", 16, F32)
        self.wring = [sbp("wring%d" % i, 8192, BF16) for i in range(2)]
        self.ps = [nc.alloc_psum_tensor("psb%d" % i, [128, 512], F32) for i in range(8)]

    def sb(self, es, name, f, dt):
        self.sb_n = getattr(self, "sb_n", 0) + 1
        return es.enter_context(self.nc.sbuf_tensor("s%d_%s" % (self.sb_n, name), [128, f], dt))

    def evq(self):
        self.ev_i += 1
        return "act" if self.ev_i % 2 else "dve"

    def evac(self, out, in_, scale, reads, writes, eng=None):
        e = eng or self.evq()
        if e == "act":
            return self.kb.op("act", lambda g: g.activation(out=out, in_=in_, func=AF.Identity, scale=float(scale)),
                              reads=reads, writes=writes)
        if scale == 1.0:
            return self.kb.op("dve", lambda g: g.tensor_copy(out=out, in_=in_), reads=reads, writes=writes)
        return self.kb.op("dve", lambda g: g.tensor_scalar(out=out, in0=in_, scalar1=float(scale), scalar2=None,
                                                            op0=ALU.mult), reads=reads, writes=writes)

    def load_w(self, dram_ap, dims):
        s = self.wr_i % 2
        self.wr_i += 1
        key = "wring%d" % s
        view = V(self.wring[s], 0, dims)
        self.kb.dma("pool", view, dram_ap, key, writes=[key])
        return view, key, s

    def wslice(self, s, off, dims):
        return V(self.wring[s], off, dims)

    def phase_const(self):
        kb = self.kb
        kb.dma("pool", self.ident[:], self.d_ident, "c_ident", writes=["ident"])
        kb.op("pool", lambda g: g.memset(self.ones[:], 1.0), writes=["ones"])
        kb.dma("sp", self.cm[:], self.d_cm, "c_misc", writes=["cm"])
        kb.dma("sp", self.ropeC[:], self.d_ropeC.rearrange("p b f -> p (b f)"), "c_misc", writes=["ropeC"])
        kb.dma("sp", self.ropeS[:], self.d_ropeS.rearrange("p b f -> p (b f)"), "c_misc", writes=["ropeS"])
        kb.dma("sp", self.fg[:], self.d_fg, "c_misc", writes=["fg"])

    def phase_mod(self):
        kb, ps = self.kb, self.ps
        with ExitStack() as es:
            sc = self.sb(es, "sc", 16, F32)
            scb = self.sb(es, "scb", 16, BF16)
            bada = self.sb(es, "bada", 96, F32)
            ng = self.sb(es, "ng", 32, F32)
            W = [self.sb(es, "wada%d" % i, 16 * 1536, BF16) for i in range(2)]
            kb.dma("sp", sc[:], self.d_c, "m_c", writes=["sc"])
            kb.op("act", lambda g: g.activation(out=scb[:], in_=sc[:], func=AF.Silu), reads=["sc"], writes=["scb"])
            n = 0
            for l in range(self.NL):
                for cb in range(8):
                    s = n % 2
                    n += 1
                    wk = "wada%d" % s
                    kb.dma("pool", V(W[s], 0, [(1536, 16), (1, 1536)]),
                           self.d_wada[l, :, cb * 1536:(cb + 1) * 1536].rearrange("(k p) c -> p k c", p=128),
                           wk, writes=[wk])
                    for jj in range(12):
                        j = cb * 12 + jj
                        for k in range(16):
                            kb.mm(V(ps[0], j, [(1, 1)]), V(W[s], k * 1536 + jj * 128, [(1, 128)]),
                                  V(scb, k, [(1, 1)]), k == 0, k == 15, reads=[wk, "scb"], writes=["ps0"],
                                  last=(k == 15))
                kb.dma("sp", bada[:], self.d_bada[l], "m_b", writes=["bada"])
                kb.dma("sp", V(ng, 0, [(1, 16)]), self.d_n1g[l], "m_b", writes=["ng"])
                kb.dma("sp", V(ng, 16, [(1, 16)]), self.d_n2g[l], "m_b", writes=["ng"])
                mod = self.mod[l]
                kb.op("dve", lambda g: g.tensor_tensor(out=mod[:], in0=V(ps[0], 0, [(1, 96)]), in1=bada[:], op=ALU.add),
                      reads=["ps0", "bada"], writes=["mod%d" % l])
                kb.op("dve", lambda g: g.scalar_tensor_tensor(out=self.A1[l][:], in0=V(mod, 16, [(1, 16)]), scalar=1.0,
                                                              in1=V(ng, 0, [(1, 16)]), op0=ALU.add, op1=ALU.mult),
                      reads=["mod%d" % l, "ng"], writes=["A1_%d" % l])
                kb.op("dve", lambda g: g.scalar_tensor_tensor(out=self.A2[l][:], in0=V(mod, 64, [(1, 16)]), scalar=1.0,
                                                              in1=V(ng, 16, [(1, 16)]), op0=ALU.add, op1=ALU.mult),
                      reads=["mod%d" % l, "ng"], writes=["A2_%d" % l])
            kb.barrier()

    def norm_tiles(self, es, xsrc, A, shift, t0, QT, hT, final=False):
        kb, ps = self.kb, self.ps
        xt = [self.sb(es, "nx%d" % i, 16 * 512, F32) for i in range(2)]
        sq = self.sb(es, "nsq", 16 * 512, BF16)
        rs = self.sb(es, "nrs", 512, F32)
        toks = []
        for ti in range(QT // 512):
            b = ti % 2
            tt = t0 + ti * 512
            xk = "nx%d" % b
            kb.dma("sp", V(xt[b], 0, [(512, 16), (1, 512)]), xsrc[:, :, tt:tt + 512].rearrange("k p t -> p k t"),
                   xk, reads=["x_t%d" % (tt // 512)], writes=[xk])
            kb.op("act", lambda g: g.activation(out=sq[:], in_=xt[b][:], func=AF.Square), reads=[xk], writes=["nsq"])
            for k in range(16):
                kb.mm(ps[1][:], self.ones[:], V(sq, k * 512, [(1, 512)]), k == 0, k == 15, reads=["nsq", "ones"],
                      writes=["ps1"], last=(k == 15))
            kb.op("act", lambda g: g.activation(out=rs[:], in_=ps[1][:], func=AF.Sqrt, scale=1.0 / D, bias=self.epsb[:]),
                  reads=["ps1"], writes=["nrs"])
            kb.op("dve", lambda g: g.reciprocal(out=rs[:], in_=rs[:]), reads=["nrs"], writes=["nrs"])
            kb.op("dve", lambda g: g.tensor_tensor(out=V(xt[b], 0, [(512, 16), (1, 512)]),
                                                   in0=V(xt[b], 0, [(512, 16), (1, 512)]),
                                                   in1=V(rs, 0, [(0, 16), (1, 512)]), op=ALU.mult),
                  reads=[xk, "nrs"], writes=[xk])
            if final:
                for k in range(16):
                    kb.op("dve" if k % 2 else "act",
                          (lambda g: g.tensor_scalar(out=V(xt[b], k * 512, [(1, 512)]), in0=V(xt[b], k * 512, [(1, 512)]),
                                                     scalar1=V(A, k, [(1, 1)]), scalar2=None, op0=ALU.mult)) if k % 2 else
                          (lambda g: g.activation(out=V(xt[b], k * 512, [(1, 512)]), in_=V(xt[b], k * 512, [(1, 512)]),
                                                  func=AF.Identity, scale=V(A, k, [(1, 1)]))),
                          reads=[xk, "fg"], writes=[xk])
                toks.append(kb.dma("sp", self.d_out[:, :, tt:tt + 512].rearrange("k p t -> p k t"),
                                   V(xt[b], 0, [(512, 16), (1, 512)]), "outst", reads=[xk]))
                continue
            for k in range(16):
                o = V(hT, k * QT + ti * 512, [(1, 512)])
                i_ = V(xt[b], k * 512, [(1, 512)])
                if k % 2:
                    kb.op("dve", lambda g: g.tensor_scalar(out=o, in0=i_, scalar1=V(A, k, [(1, 1)]),
                                                           scalar2=V(shift[0], shift[1] + k, [(1, 1)]),
                                                           op0=ALU.mult, op1=ALU.add), reads=[xk], writes=["hT"])
                else:
                    kb.op("act", lambda g: g.activation(out=o, in_=i_, func=AF.Identity, scale=V(A, k, [(1, 1)]),
                                                        bias=V(shift[0], shift[1] + k, [(1, 1)])), reads=[xk], writes=["hT"])
        return toks

    def fm_proj(self, hT, QT, t0, wv, wk, c0, ncol, out_cb):
        kb, ps = self.kb, self.ps
        for c in range(ncol // 128):
            for ti in range(QT // 512):
                bank = 2 + (self.bank_i % 2)
                self.bank_i += 1
                pk = "ps%d" % bank
                for k in range(16):
                    kb.mm(ps[bank][:], V(wv[0], wv[1] + k * wv[2] + c * 128, [(1, 128)]),
                          V(hT, k * QT + ti * 512, [(1, 512)]), k == 0, k == 15, reads=[wk, "hT"], writes=[pk],
                          last=(k == 15))
                out_cb(c0 + c, ti, bank)

    def phase_p1(self, l, t0, QT, hT, es, part):
        kb, ps, L = self.kb, self.ps, self.L
        self.bank_i = 0
        win = self.d_win[l]
        self.d_ckvT = self.kA[l][0][:, 0:2 * L].rearrange("p (c t) -> p c t", c=2)
        self.d_kiT = self.kA[l][0][:, 2 * L:3 * L]
        self.d_ckvA = self.kC[l][0].rearrange("p (b c) -> b p c", c=256)
        kKl = [self.kK[l][g][0].rearrange("p (h t) -> p h t", h=4) for g in range(3)]
        kVl = [self.kV[l][g][0] for g in range(3)]
        stg = [self.sb(es, "p1stg%d" % i, 512, BF16) for i in range(4)]
        self.stg_i = 0

        def nstg():
            i = self.stg_i % 4
            self.stg_i += 1
            return stg[i], "p1stg%d" % i

        def wcols(c0, n):
            return win[:, c0:c0 + n].rearrange("(k p) c -> p k c", p=128)

        if part == "q":
            self._p1_q(l, t0, QT, hT, es, wcols, nstg)
        else:
            self._p1_k(l, t0, QT, hT, es, wcols, nstg, kKl, kVl)

    def _p1_q(self, l, t0, QT, hT, es, wcols, nstg):
        kb, ps, L = self.kb, self.ps, self.L
        wuk = self.sb(es, "wuk", 8 * 256, BF16)
        kb.dma("pool", V(wuk, 0, [(256, 8), (1, 256)]), self.d_wuk[l].rearrange("h d c -> d h c"), "p1wuk", writes=["wuk"])
        qa = [self.sb(es, "qa%d" % i, 512, BF16) for i in range(2)]
        self.qa_i = 0
        for grp in range(2):
            wv_, wk, s = self.load_w(wcols(C_QA + grp * 512, 512), [(512, 16), (1, 512)])

            def cb_qa(h, ti, bank):
                qi = self.qa_i % 2
                self.qa_i += 1
                qk = "qa%d" % qi
                self.evac(qa[qi][:], ps[bank][:], 1.0, ["ps%d" % bank], [qk])
                for cc in range(2):
                    b2 = 4 + cc
                    kb.mm(ps[b2][:], V(wuk, h * 256 + cc * 128, [(1, 128)]), qa[qi][:], True, True,
                          reads=["wuk", qk], writes=["ps%d" % b2])
                    st, sk = nstg()
                    self.evac(st[:], ps[b2][:], 128 ** -0.5, ["ps%d" % b2], [sk])
                    tt = t0 + ti * 512
                    kb.dma("sp", self.d_qlat[:, cc * 8 + h, tt:tt + 512], st[:], sk, reads=[sk], writes=["qlat"])

            self.fm_proj(hT, QT, t0, (self.wring[s], 0, 512), wk, grp * 4, 512, cb_qa)

        self._p1_fm(t0, QT, hT, wcols, nstg, C_QB, self.d_qb, 128 ** -0.5, "qb", None)
        self._p1_gates_qi(l, t0, QT, hT, es, wcols, nstg)

    def _p1_fm(self, t0, QT, hT, wcols, nstg, cbase, dst, scl, dk, kKl):
        kb, ps = self.kb, self.ps
        if True:
            for grp in range(3):
                wv_, wk, s = self.load_w(wcols(cbase + grp * 512, 512), [(512, 16), (1, 512)])

                def cb_b(h, ti, bank, dst=dst, scl=scl, dk=dk):
                    st, sk = nstg()
                    self.evac(st[:], ps[bank][:], scl, ["ps%d" % bank], [sk])
                    tt = t0 + ti * 512
                    dap = dst[:, h, tt:tt + 512] if dst is not None else kKl[h // 4][:, h % 4, tt:tt + 512]
                    kb.dma("sp", dap, st[:], sk, reads=[sk], writes=[dk])

                self.fm_proj(hT, QT, t0, (self.wring[s], 0, 512), wk, grp * 4, 512, cb_b)

    def _p1_gates_qi(self, l, t0, QT, hT, es, wcols, nstg):
        kb, ps, L = self.kb, self.ps, self.L
        for grp in range(8):
            wv_, wk, s = self.load_w(wcols(C_GATE + grp * 512, 512), [(512, 16), (1, 512)])

            def cb_g(c, ti, bank):
                st, sk = nstg()
                kb.op("act", lambda g: g.activation(out=st[:], in_=ps[bank][:], func=AF.Sigmoid),
                      reads=["ps%d" % bank], writes=[sk])
                tt = t0 + ti * 512
                kb.dma("sp", self.d_gates[:, c, tt:tt + 512], st[:], sk, reads=[sk], writes=["gates"])

            self.fm_proj(hT, QT, t0, (self.wring[s], 0, 512), wk, grp * 4, 512, cb_g)

        self._p1_qi(l, t0, QT, hT, es, wcols)

    def _p1_k(self, l, t0, QT, hT, es, wcols, nstg, kKl, kVl):
        kb, ps, L = self.kb, self.ps, self.L
        self._p1_fm(t0, QT, hT, wcols, nstg, C_KB, None, 1.0, "kbT", kKl)
        vst = [self.sb(es, "vst%d" % i, 512, BF16) for i in range(2)]
        n = 0
        for grp in range(3):
            wv_, wk, s = self.load_w(wcols(C_VB + grp * 512, 512), [(512, 16), (1, 512)])
            for bi in range(QT // 128):
                blk = t0 // 128 + bi
                bank = 2 + (self.bank_i % 2)
                self.bank_i += 1
                for k in range(16):
                    kb.mm(ps[bank][:], V(hT, k * QT + bi * 128, [(1, 128)]), V(self.wring[s], k * 512, [(1, 512)]),
                          k == 0, k == 15, reads=[wk, "hT"], writes=["ps%d" % bank], last=(k == 15))
                v = vst[n % 2]
                vk = "vst%d" % (n % 2)
                n += 1
                self.evac(v[:], ps[bank][:], 1.0, ["ps%d" % bank], [vk])
                kb.dma("sp", kVl[grp][:, blk * 512:(blk + 1) * 512], v[:], vk, reads=[vk], writes=["vb"])

        kvg = self.sb(es, "kvg", 256, F32)
        ilg = self.sb(es, "ilg", 128, F32)
        kb.dma("sp", kvg[:], self.d_kvg[l], "p1c", writes=["kvg"])
        kb.dma("sp", V(ilg, 0, [(1, 64)]), self.d_ilg[l], "p1c", writes=["ilg"])
        kb.dma("sp", V(ilg, 64, [(1, 64)]), self.d_ilb[l], "p1c", writes=["ilg"])
        s = self.wr_i % 2
        self.wr_i += 1
        wk = "wring%d" % s
        kb.dma("pool", V(self.wring[s], 0, [(336, 16), (1, 256)]), wcols(C_CKV, 256), wk, writes=[wk])
        kb.dma("pool", V(self.wring[s], 256, [(336, 16), (1, 80)]), wcols(C_KI, 80), wk, writes=[wk])
        junk = self.sb(es, "junk", 256, F32)
        st8 = self.sb(es, "st8", 16, F32)
        ckvb = [self.sb(es, "ckvb%d" % i, 256, BF16) for i in range(2)]
        ckt = [self.sb(es, "ckt%d" % i, 256, BF16) for i in range(2)]
        kn = self.sb(es, "kn", 64, F32)
        kt = self.sb(es, "ktmp", 64, F32)
        kr = [self.sb(es, "kr%d" % i, 128, BF16) for i in range(2)]
        kit = [self.sb(es, "kit%d" % i, 128, BF16) for i in range(2)]
        wis = [self.sb(es, "wis%d" % i, 16, F32) for i in range(2)]
        for bi in range(QT // 128):
            blk = t0 // 128 + bi
            p = bi % 2
            bank = 2 + (self.bank_i % 2)
            self.bank_i += 1
            pk = "ps%d" % bank
            for k in range(16):
                kb.mm(V(ps[bank], 0, [(1, 336)]), V(hT, k * QT + bi * 128, [(1, 128)]), V(self.wring[s], k * 336, [(1, 336)]),
                      k == 0, k == 15, reads=[wk, "hT"], writes=[pk], last=(k == 15))
            kb.op("act", lambda g: g.activation(out=junk[:], in_=V(ps[bank], 0, [(1, 256)]), func=AF.Square),
                  reads=[pk], writes=["junk"])
            kb.op("dve", lambda g: g.reduce_sum(out=V(st8, 0, [(1, 1)]), in_=junk[:], axis=mybir.AxisListType.X),
                  reads=["junk"], writes=["st8a"])
            kb.op("act", lambda g: g.activation(out=V(st8, 1, [(1, 1)]), in_=V(st8, 0, [(1, 1)]), func=AF.Sqrt,
                                                scale=1.0 / 256, bias=self.epsb[:]), reads=["st8a"], writes=["st8b"])
            kb.op("dve", lambda g: g.reciprocal(out=V(st8, 2, [(1, 1)]), in_=V(st8, 1, [(1, 1)])), reads=["st8b"], writes=["st8c"])
            cb_, ck = ckvb[p], "ckvb%d" % p
            kb.op("dve", lambda g: g.scalar_tensor_tensor(out=cb_[:], in0=V(ps[bank], 0, [(1, 256)]), scalar=V(st8, 2, [(1, 1)]),
                                                          in1=kvg[:], op0=ALU.mult, op1=ALU.mult),
                  reads=[pk, "st8c", "kvg"], writes=[ck])
            kb.dma("sp", self.d_ckvA[blk], cb_[:], ck, reads=[ck], writes=["ckvA"])
            for cc in range(2):
                kb.mm(V(ps[6], cc * 128, [(1, 128)]), V(cb_, cc * 128, [(1, 128)]), self.ident[:], True, True,
                      reads=[ck, "ident"], writes=["ps6"])
            ct, ctk = ckt[p], "ckt%d" % p
            self.evac(ct[:], V(ps[6], 0, [(1, 256)]), 1.0, ["ps6"], [ctk])
            kb.dma("sp", self.d_ckvT[:, :, blk * 128:(blk + 1) * 128], V(ct, 0, [(128, 2), (1, 128)]), ctk,
                   reads=[ctk], writes=["ckvT"])
            kb.op("dve", lambda g: g.reduce_sum(out=V(st8, 3, [(1, 1)]), in_=V(ps[bank], 256, [(1, 64)]), axis=mybir.AxisListType.X),
                  reads=[pk], writes=["st8d"])
            kb.op("act", lambda g: g.activation(out=V(junk, 0, [(1, 64)]), in_=V(ps[bank], 256, [(1, 64)]), func=AF.Square),
                  reads=[pk], writes=["junk"])
            kb.op("dve", lambda g: g.reduce_sum(out=V(st8, 4, [(1, 1)]), in_=V(junk, 0, [(1, 64)]), axis=mybir.AxisListType.X),
                  reads=["junk"], writes=["st8e"])
            kb.op("dve", lambda g: g.tensor_scalar(out=V(st8, 5, [(1, 1)]), in0=V(st8, 3, [(1, 1)]), scalar1=1.0 / 64, scalar2=None,
                                                   op0=ALU.mult), reads=["st8d"], writes=["st8f"])
            kb.op("dve", lambda g: g.tensor_tensor(out=V(st8, 6, [(1, 1)]), in0=V(st8, 5, [(1, 1)]), in1=V(st8, 5, [(1, 1)]),
                                                   op=ALU.mult), reads=["st8f"], writes=["st8g"])
            kb.op("dve", lambda g: g.scalar_tensor_tensor(out=V(st8, 7, [(1, 1)]), in0=V(st8, 4, [(1, 1)]), scalar=1.0 / 64,
                                                          in1=V(st8, 6, [(1, 1)]), op0=ALU.mult, op1=ALU.subtract),
                  reads=["st8e", "st8g"], writes=["st8h"])
            kb.op("act", lambda g: g.activation(out=V(st8, 8, [(1, 1)]), in_=V(st8, 7, [(1, 1)]), func=AF.Sqrt,
                                                bias=self.epsb[:]), reads=["st8h"], writes=["st8i"])
            kb.op("dve", lambda g: g.reciprocal(out=V(st8, 9, [(1, 1)]), in_=V(st8, 8, [(1, 1)])), reads=["st8i"], writes=["st8j"])
            kb.op("dve", lambda g: g.tensor_scalar(out=kn[:], in0=V(ps[bank], 256, [(1, 64)]), scalar1=V(st8, 5, [(1, 1)]),
                                                   scalar2=V(st8, 9, [(1, 1)]), op0=ALU.subtract, op1=ALU.mult),
                  reads=[pk, "st8f", "st8j"], writes=["kn"])
            kb.op("dve", lambda g: g.tensor_tensor(out=kn[:], in0=kn[:], in1=V(ilg, 0, [(1, 64)]), op=ALU.mult),
                  reads=["kn", "ilg"], writes=["kn"])
            kb.op("dve", lambda g: g.tensor_tensor(out=kn[:], in0=kn[:], in1=V(ilg, 64, [(1, 64)]), op=ALU.add),
                  reads=["kn", "ilg"], writes=["kn"])
            cosb = V(self.ropeC, blk * 16, [(1, 16)])
            sinb = V(self.ropeS, blk * 16, [(1, 16)])
            x1, x2 = V(kn, 0, [(1, 16)]), V(kn, 16, [(1, 16)])
            kb.op("dve", lambda g: g.tensor_tensor(out=V(kt, 0, [(1, 16)]), in0=x1, in1=cosb, op=ALU.mult), reads=["kn", "ropeC"], writes=["kt0"])
            kb.op("dve", lambda g: g.tensor_tensor(out=V(kt, 16, [(1, 16)]), in0=x2, in1=sinb, op=ALU.mult), reads=["kn", "ropeS"], writes=["kt1"])
            kb.op("dve", lambda g: g.tensor_tensor(out=V(kt, 32, [(1, 16)]), in0=x1, in1=sinb, op=ALU.mult), reads=["kn", "ropeS"], writes=["kt2"])
            kb.op("dve", lambda g: g.tensor_tensor(out=V(kt, 48, [(1, 16)]), in0=x2, in1=cosb, op=ALU.mult), reads=["kn", "ropeC"], writes=["kt3"])
            krp, krk = kr[p], "kr%d" % p
            for half in range(2):
                kb.op("dve", lambda g: g.tensor_tensor(out=V(krp, half * 64, [(1, 16)]), in0=V(kt, 0, [(1, 16)]), in1=V(kt, 16, [(1, 16)]),
                                                       op=ALU.subtract), reads=["kt0", "kt1"], writes=[krk])
                kb.op("dve", lambda g: g.tensor_tensor(out=V(krp, half * 64 + 16, [(1, 16)]), in0=V(kt, 32, [(1, 16)]), in1=V(kt, 48, [(1, 16)]),
                                                       op=ALU.add), reads=["kt2", "kt3"], writes=[krk])
                kb.op("dve", lambda g: g.tensor_copy(out=V(krp, half * 64 + 32, [(1, 32)]), in_=V(kn, 32, [(1, 32)])),
                      reads=["kn"], writes=[krk])
            kb.mm(V(ps[7], 0, [(1, 128)]), krp[:], self.ident[:], True, True, reads=[krk, "ident"], writes=["ps7"])
            kip, kik = kit[p], "kit%d" % p
            self.evac(kip[:], V(ps[7], 0, [(1, 128)]), 1.0, ["ps7"], [kik])
            kb.dma("sp", self.d_kiT[:, blk * 128:(blk + 1) * 128], kip[:], kik, reads=[kik], writes=["kiT"])
            wp, wpk = wis[p], "wis%d" % p
            kb.op("dve", lambda g: g.tensor_scalar(out=wp[:], in0=V(ps[bank], 320, [(1, 16)]), scalar1=1.0 / 32.0, scalar2=None,
                                                   op0=ALU.mult), reads=[pk], writes=[wpk])
            kb.dma("sp", self.d_wi[blk], wp[:], wpk, reads=[wpk], writes=["wi"])

    def _p1_qi(self, l, t0, QT, hT, es, wcols):
        kb, ps, L = self.kb, self.ps, self.L
        qr = [self.sb(es, "qr%d" % i, 512, BF16) for i in range(2)]
        qt4 = [self.sb(es, "qt4%d" % i, 512, F32) for i in range(1)]
        qit = [self.sb(es, "qit%d" % i, 512, BF16) for i in range(2)]
        n = 0
        for grp in range(2):
            wv_, wk, s = self.load_w(wcols(C_QI + grp * 512, 512), [(512, 16), (1, 512)])
            for bi in range(QT // 128):
                blk = t0 // 128 + bi
                bank = 2 + (self.bank_i % 2)
                self.bank_i += 1
                pk = "ps%d" % bank
                for k in range(16):
                    kb.mm(ps[bank][:], V(hT, k * QT + bi * 128, [(1, 128)]), V(self.wring[s], k * 512, [(1, 512)]),
                          k == 0, k == 15, reads=[wk, "hT"], writes=[pk], last=(k == 15))
                p = n % 2
                n += 1
                q, qk = qr[p], "qr%d" % p
                t4 = qt4[0]
                cosb = V(self.ropeC, blk * 16, [(0, 8), (1, 16)])
                sinb = V(self.ropeS, blk * 16, [(0, 8), (1, 16)])
                x1 = V(ps[bank], 0, [(64, 8), (1, 16)])
                x2 = V(ps[bank], 16, [(64, 8), (1, 16)])
                kb.op("dve", lambda g: g.tensor_tensor(out=V(t4, 0, [(16, 8), (1, 16)]), in0=x1, in1=cosb, op=ALU.mult), reads=[pk, "ropeC"], writes=["qt0"])
                kb.op("dve", lambda g: g.tensor_tensor(out=V(t4, 128, [(16, 8), (1, 16)]), in0=x2, in1=sinb, op=ALU.mult), reads=[pk, "ropeS"], writes=["qt1"])
                kb.op("dve", lambda g: g.tensor_tensor(out=V(t4, 256, [(16, 8), (1, 16)]), in0=x1, in1=sinb, op=ALU.mult), reads=[pk, "ropeS"], writes=["qt2"])
                kb.op("dve", lambda g: g.tensor_tensor(out=V(t4, 384, [(16, 8), (1, 16)]), in0=x2, in1=cosb, op=ALU.mult), reads=[pk, "ropeC"], writes=["qt3"])
                kb.op("dve", lambda g: g.tensor_tensor(out=V(q, 0, [(64, 8), (1, 16)]), in0=V(t4, 0, [(16, 8), (1, 16)]),
                                                       in1=V(t4, 128, [(16, 8), (1, 16)]), op=ALU.subtract), reads=["qt0", "qt1"], writes=[qk])
                kb.op("dve", lambda g: g.tensor_tensor(out=V(q, 16, [(64, 8), (1, 16)]), in0=V(t4, 256, [(16, 8), (1, 16)]),
                                                       in1=V(t4, 384, [(16, 8), (1, 16)]), op=ALU.add), reads=["qt2", "qt3"], writes=[qk])
                kb.op("act", lambda g: g.activation(out=V(q, 32, [(64, 8), (1, 32)]), in_=V(ps[bank], 32, [(64, 8), (1, 32)]),
                                                    func=AF.Identity), reads=[pk], writes=[qk])
                for pr in range(4):
                    kb.mm(V(ps[6], pr * 128, [(1, 128)]), V(q, pr * 128, [(1, 128)]), self.ident[:], True, True,
                          reads=[qk, "ident"], writes=["ps6"])
                qi_, qik = qit[p], "qit%d" % p
                self.evac(qi_[:], ps[6][:], 1.0, ["ps6"], [qik])
                kb.dma("sp", self.d_qiT[:, grp * 4:(grp + 1) * 4, blk * 128:(blk + 1) * 128], V(qi_, 0, [(128, 4), (1, 128)]),
                       qik, reads=[qik], writes=["qiT"])

    def phase_att_a(self, l):
        kb, ps, L, NB, NR = self.kb, self.ps, self.L, self.NB, self.NR
        kAf, kCf = self.kA[l][1], self.kC[l][1]
        NTA = self.NTA
        with ExitStack() as es:
            ckvT = self.sb(es, "ckvTs", NR * 2 * L, BF16)
            ckvA = self.sb(es, "ckvAs", NR * NB * 256, BF16)
            kiT = self.sb(es, "kiTs", NR * L, BF16)
            bta = self.sb(es, "btas", NTA * 8 * 128, BF16)
            wuv = self.sb(es, "wuvs", 8 * 2 * 128, BF16)
            for rk in range(NR):
                kb.dma("sp", V(ckvT, rk * 2 * L, [(1, 2 * L)]), kAf[rk * 128:(rk + 1) * 128, 0:2 * L], "aa_k", reads=["kfull"], writes=["ckvTs"])
                kb.dma("sp", V(kiT, rk * L, [(1, L)]), kAf[rk * 128:(rk + 1) * 128, 2 * L:3 * L], "aa_k", reads=["kfull"], writes=["kiTs"])
                kb.dma("sp", V(ckvA, rk * NB * 256, [(1, NB * 256)]), kCf[rk * 128:(rk + 1) * 128, :], "aa_k",
                       reads=["kfull"], writes=["ckvAs"])
            kb.dma("pool", bta[:], self.d_bta, "aa_c", writes=["btas"])
            kb.dma("pool", V(wuv, 0, [(256, 8), (128, 2), (1, 128)]), self.d_wuv[l].rearrange("h (cc c) d -> c h cc d", c=128),
                   "aa_c", writes=["wuvs"])
            qiT = [self.sb(es, "aqi%d" % i, 8 * 128, BF16) for i in range(2)]
            wi = [self.sb(es, "awi%d" % i, 16, F32) for i in range(2)]
            qlat = [self.sb(es, "aql%d" % i, 16 * 128, BF16) for i in range(4)]
            dg = self.sb(es, "adg", 16 * 128, BF16)
            R = [self.sb(es, "aR%d" % i, 512, BF16) for i in range(4)]
            NK = NR * L
            scs = [self.sb(es, "asc%d" % i, NK, F32) for i in range(2)]
            bss = [self.sb(es, "abs%d" % i, 8, F32) for i in range(2)]
            junks = [self.sb(es, "ajk%d" % i, NK, BF16) for i in range(2)]
            assert NK <= 4096
            negm = [(self.wring[i // 2], (i % 2) * 4096) for i in range(4)]
            Pst = [self.sb(es, "aP%d" % i, 512, BF16) for i in range(3)]
            olat = self.sb(es, "aol", 2 * 512, BF16)
            densb = self.sb(es, "adn", 512, F32)
            ysb = self.sb(es, "ays", 512, F32)
            yst = [self.sb(es, "ayst%d" % i, 512, BF16) for i in range(2)]
            st = dict(rn=0, pn=0, yn=0, ln=0)
            LB = (4, 5, 7)

            def nkeys(s):
                return NR * (s + 1) * 128

            def stage1(s):
                b = s % 2
                t0 = s * 128
                scA, sck = scs[b], "asc%d" % b
                kb.dma("sp", V(qlat[s % 4], 0, [(128, 16), (1, 128)]), self.d_qlat[:, :, t0:t0 + 128], "aa_ql%d" % (s % 4),
                       reads=["qlat"], writes=["aql%d" % (s % 4)])
                if nkeys(s) <= 256:
                    return
                kb.dma("sp", V(qiT[b], 0, [(128, 8), (1, 128)]), self.d_qiT[:, :, t0:t0 + 128], "aa_q%d" % b,
                       reads=["qiT"], writes=["aqi%d" % b])
                kb.dma("sp", wi[b][:], self.d_wi[s], "aa_q%d" % b, reads=["wi"], writes=["awi%d" % b])
                kb.op("dve", lambda g: g.tensor_tensor(out=V(dg, 0, [(128, 16), (1, 128)]), in0=V(self.ident, 0, [(0, 16), (1, 128)]),
                                                       in1=V(wi[b], 0, [(1, 16), (0, 128)]), op=ALU.mult),
                      reads=["ident", "awi%d" % b], writes=["adg"])
                nb_r = s + 1
                for rk in range(NR):
                    cbase = rk * nb_r * 128
                    for kc in range((nb_r + 3) // 4):
                        w = min(512, nb_r * 128 - kc * 512)
                        LA = 2
                        pend = []
                        for h in range(16 + LA):
                            if h < 16:
                                pr, hf = h // 2, h % 2
                                bank = LB[st["ln"] % 3]
                                st["ln"] += 1
                                kb.mm(V(ps[bank], 0, [(1, w)]), V(qiT[b], pr * 128, [(1, 128)], p0=hf * 64, npart=64),
                                      V(kiT, rk * L + kc * 512, [(1, w)], p0=hf * 64, npart=64), True, True,
                                      reads=["aqi%d" % b, "kiTs"], writes=["ps%d" % bank])
                                r = R[st["rn"] % 4]
                                rkey = "aR%d" % (st["rn"] % 4)
                                st["rn"] += 1
                                kb.op("act", lambda g: g.activation(out=V(r, 0, [(1, w)]), in_=V(ps[bank], 0, [(1, w)]), func=AF.Relu),
                                      reads=["ps%d" % bank], writes=[rkey])
                                pend.append((r, rkey))
                            if h >= LA:
                                hd = h - LA
                                r2, rkey2 = pend[hd]
                                kb.mm(V(ps[6], 0, [(1, w)]), V(dg, hd * 128, [(1, 128)]), V(r2, 0, [(1, w)]), hd == 0, hd == 15,
                                      reads=["adg", rkey2], writes=["ps6"], last=(hd == 15))
                        kb.op("act", lambda g: g.activation(out=V(scA, cbase + kc * 512, [(1, w)]), in_=V(ps[6], 0, [(1, w)]),
                                                            func=AF.Identity), reads=["ps6"], writes=[sck])
                    kb.op("dve", lambda g: g.tensor_tensor(out=V(scA, cbase + s * 128, [(1, 128)]), in0=V(scA, cbase + s * 128, [(1, 128)]),
                                                           in1=V(self.cm, rk * 128, [(1, 128)]), op=ALU.add), reads=[sck, "cm"], writes=[sck])

            def stage2(s):
                n = nkeys(s)
                nm, nk = negm[s % 4], "anegm%d" % (s % 4)
                p = s % 2
                sc, sck = scs[p], "asc%d" % p
                bs_, bk = bss[p], "abs%d" % p
                jk_, jkk = junks[p], "ajk%d" % p
                if n <= 256:
                    kb.op("pool", lambda g: g.memset(V(nm[0], nm[1], [(1, n)]), 0.0), writes=[nk])
                    return
                lo, d0, mid, cnt, g2 = [V(bs_, i, [(1, 1)]) for i in range(5)]
                scv = V(sc, 0, [(1, n)])
                kb.op("dve", lambda g: g.reduce_max(out=d0, in_=scv, axis=mybir.AxisListType.X), reads=[sck], writes=[bk])
                yield
                kb.op("dve", lambda g: g.tensor_scalar(out=d0, in0=d0, scalar1=31.0, scalar2=None, op0=ALU.add), reads=[bk], writes=[bk])
                yield
                kb.op("dve", lambda g: g.memset(lo, -30.0), reads=[bk], writes=[bk])
                yield
                kb.op("dve", lambda g: g.scalar_tensor_tensor(out=mid, in0=d0, scalar=0.5, in1=lo, op0=ALU.mult, op1=ALU.add),
                      reads=[bk], writes=[bk])
                yield
                NIT = 26
                for k in range(NIT):
                    kb.op("dve", lambda g: g.tensor_scalar(out=V(jk_, 0, [(1, n)]), in0=scv, scalar1=mid, scalar2=0.0, op0=ALU.is_ge,
                                                           op1=ALU.add, accum_out=cnt), reads=[sck, bk], writes=[bk, jkk])
                    yield
                    kb.op("dve", lambda g: g.tensor_scalar(out=g2, in0=cnt, scalar1=255.5, scalar2=2.0 ** -(k + 1), op0=ALU.is_gt,
                                                           op1=ALU.mult), reads=[bk], writes=[bk])
                    yield
                    kb.op("dve", lambda g: g.scalar_tensor_tensor(out=lo, in0=d0, scalar=g2, in1=lo, op0=ALU.mult, op1=ALU.add),
                          reads=[bk], writes=[bk])
                    yield
                    if k < NIT - 1:
                        kb.op("dve", lambda g: g.scalar_tensor_tensor(out=mid, in0=d0, scalar=2.0 ** -(k + 2), in1=lo, op0=ALU.mult,
                                                                      op1=ALU.add), reads=[bk], writes=[bk])
                        yield
                kb.op("dve", lambda g: g.tensor_scalar(out=V(nm[0], nm[1], [(1, n)]), in0=scv, scalar1=lo, scalar2=NEG, op0=ALU.is_lt,
                                                       op1=ALU.mult), reads=[sck, bk], writes=[nk])
                yield

            def inter(*gens):
                gens = list(gens)
                while gens:
                    for g_ in list(gens):
                        try:
                            next(g_)
                        except StopIteration:
                            gens.remove(g_)

            def stage3(s):
                b = s % 4
                t0 = s * 128
                nm, nk = negm[b], "anegm%d" % b
                nb_r = s + 1
                keys = [(rk, sj) for rk in range(NR) for sj in range(nb_r)]
                def emitS(g4, idx):
                    rk, sj = keys[idx]
                    j = NR * sj + rk
                    dl = min(NR * s + NR - 1 - j, NTA - 1)
                    sb_ = LB[st["pn"] % 3]
                    sk = "ps%d" % sb_
                    kb.mm(ps[sb_][:], self.ident[:], V(bta, (dl * 8 + g4 * 4) * 128, [(1, 512)]), True, False,
                          reads=["ident", "btas"], writes=[sk], last=False)
                    kb.mm(ps[sb_][:], V(nm[0], nm[1] + (rk * nb_r + sj) * 128, [(1, 128)]), V(self.ident, 0, [(0, 4), (1, 128)]), False, False,
                          reads=[nk, "ident"], writes=[sk], last=False)
                    for cc in range(2):
                        kb.mm(ps[sb_][:], V(ckvT, (rk * 2 + cc) * L + sj * 128, [(1, 128)]),
                              V(qlat[b], (cc * 8 + g4 * 4) * 128, [(1, 512)]), False, cc == 1,
                              reads=["ckvTs", "aql%d" % b], writes=[sk], last=(cc == 1))
                    pt = Pst[st["pn"] % 3]
                    pk_ = "aP%d" % (st["pn"] % 3)
                    st["pn"] += 1
                    kb.op("act", lambda g: g.activation(out=pt[:], in_=ps[sb_][:], func=AF.Exp), reads=[sk], writes=[pk_])
                    return pt, pk_

                def emitPV(g4, idx, pt, pk_):
                    rk, sj = keys[idx]
                    first, lastk = idx == 0, idx == len(keys) - 1
                    for cc in range(2):
                        kb.mm(ps[cc][:], V(ckvA, (rk * NB + sj) * 256 + cc * 128, [(1, 128)]), pt[:], first, lastk,
                              reads=["ckvAs", pk_], writes=["ps%d" % cc], last=lastk)
                    kb.mm(ps[2][:], self.ones[:], pt[:], first, lastk, reads=["ones", pk_], writes=["ps2"], last=lastk)
                    if not lastk:
                        return
                    for cc in range(2):
                        self.evac(V(olat, cc * 512, [(1, 512)]), ps[cc][:], 1.0, ["ps%d" % cc], ["aol"], eng="act")
                    kb.op("act", lambda g: g.activation(out=densb[:], in_=ps[2][:], func=AF.Ln), reads=["ps2"], writes=["adn"])
                    kb.op("act", lambda g: g.activation(out=densb[:], in_=densb[:], func=AF.Exp, scale=-1.0), reads=["adn"], writes=["adn"])
                    for h in range(4):
                        hh = g4 * 4 + h
                        for cc in range(2):
                            kb.mm(V(ps[3], h * 128, [(1, 128)]), V(wuv, hh * 256 + cc * 128, [(1, 128)]),
                                  V(olat, cc * 512 + h * 128, [(1, 128)]), cc == 0, cc == 1, reads=["wuvs", "aol"], writes=["ps3"],
                                  last=(cc == 1))
                    self.evac(ysb[:], ps[3][:], 1.0, ["ps3"], ["ays"], eng="act")
                    y = yst[st["yn"] % 2]
                    yk = "ayst%d" % (st["yn"] % 2)
                    st["yn"] += 1
                    kb.op("pool", lambda g: g.tensor_tensor(out=y[:], in0=ysb[:], in1=densb[:], op=ALU.mult),
                          reads=["ays", "adn"], writes=[yk])
                    kb.dma("sp", self.d_ya[:, g4 * 4:(g4 + 1) * 4, t0:t0 + 128], V(y, 0, [(128, 4), (1, 128)]), yk,
                           reads=[yk], writes=["yaT"])

                prev = None
                for g4 in range(2):
                    for idx in range(len(keys)):
                        cur = (g4, idx) + emitS(g4, idx)
                        if prev is not None:
                            emitPV(*prev)
                        prev = cur
                emitPV(*prev)

            stage1(0)
            stage1(1)
            inter(stage2(0), stage2(1))
            for s in range(0, NB, 2):
                if s + 2 < NB:
                    stage1(s + 2)
                    stage1(s + 3)
                stage3(s)
                stage3(s + 1)
                if s + 2 < NB:
                    inter(stage2(s + 2), stage2(s + 3))
            kb.barrier()

    def phase_att_b(self, l):
        kb, ps, L, NB, NR = self.kb, self.ps, self.L, self.NB, self.NR
        kKf = [self.kK[l][g][1] for g in range(3)]
        kVf = [self.kV[l][g][1] for g in range(3)]
        NRING, NTB, OFFB = self.NRING, self.NTB, self.OFFB
        with ExitStack() as es:
            btb = self.sb(es, "btbs", sum(NTB) * 4 * 128, BF16)
            kb.dma("pool", btb[:], self.d_btb, "ab_c", writes=["btbs"])
            kr = [self.sb(es, "bkr%d" % g, NRING[g] * 512, BF16) for g in range(3)]
            vr = [self.sb(es, "bvr%d" % g, NRING[g] * 512, BF16) for g in range(3)]
            qb = [self.sb(es, "bq%d" % i, 12 * 128, BF16) for i in range(2)]
            Pst = [self.sb(es, "bP%d" % i, 512, BF16) for i in range(5)]
            rden = self.sb(es, "brd", 512, F32)
            yst = [self.sb(es, "byst%d" % i, 512, BF16) for i in range(2)]
            pn = 0
            for s in range(NB):
                b = s % 2
                t0 = s * 128
                kb.dma("sp", V(qb[b], 0, [(128, 12), (1, 128)]), self.d_qb[:, :, t0:t0 + 128], "ab_q%d" % b, reads=["qb"],
                       writes=["bq%d" % b])
                for rk in range(NR):
                    j = NR * s + rk
                    for g in range(3):
                        sl = j % NRING[g]
                        kb.dma("sp", V(kr[g], sl * 512, [(128, 4), (1, 128)]),
                               kKf[g][rk * 128:(rk + 1) * 128, :].rearrange("p (h t) -> p h t", h=4)[:, :, t0:t0 + 128],
                               "ab_k%d_%d" % (g, s % 2), reads=["kfull"], writes=["bkr%d_%d" % (g, sl)])
                        kb.dma("sp", V(vr[g], sl * 512, [(1, 512)]), kVf[g][rk * 128:(rk + 1) * 128, s * 512:(s + 1) * 512],
                               "ab_v%d_%d" % (g, s % 2), reads=["kfull"], writes=["bvr%d_%d" % (g, sl)])
                jtop = NR * s + NR - 1
                pairs = [(g, dl) for g in range(3) for dl in range(NTB[g]) if jtop - dl >= 0]
                def emitS(idx):
                    nonlocal pn
                    g, dl = pairs[idx]
                    j = jtop - dl
                    sl = j % NRING[g]
                    sb_ = 4 + (pn % 4)
                    sk = "ps%d" % sb_
                    kb.mm(ps[sb_][:], self.ident[:], V(btb, (OFFB[g] + dl) * 512, [(1, 512)]), True, False,
                          reads=["ident", "btbs"], writes=[sk], last=False)
                    for hg in range(4):
                        kb.mm(V(ps[sb_], hg * 128, [(1, 128)]), V(kr[g], sl * 512 + hg * 128, [(1, 128)]),
                              V(qb[b], (g * 4 + hg) * 128, [(1, 128)]), False, hg == 3,
                              reads=["bkr%d_%d" % (g, sl), "bq%d" % b], writes=[sk], last=(hg == 3))
                    pt = Pst[pn % 5]
                    pk_ = "bP%d" % (pn % 5)
                    pn += 1
                    kb.op("act", lambda g_: g_.activation(out=pt[:], in_=ps[sb_][:], func=AF.Exp), reads=[sk], writes=[pk_])
                    return pt, pk_

                def emitPV(idx, pt, pk_):
                    g, dl = pairs[idx]
                    sl = (jtop - dl) % NRING[g]
                    first, lastp = idx == 0, idx == len(pairs) - 1
                    for hg in range(4):
                        kb.mm(V(ps[0], hg * 128, [(1, 128)]), V(vr[g], sl * 512 + hg * 128, [(1, 128)]),
                              V(pt, hg * 128, [(1, 128)]), first and hg == 0, lastp, reads=["bvr%d_%d" % (g, sl), pk_], writes=["ps0"],
                              last=(lastp and hg == 3))
                    kb.mm(ps[1][:], self.ones[:], pt[:], first, lastp, reads=["ones", pk_], writes=["ps1"], last=lastp)

                q_ = []
                for idx in range(len(pairs)):
                    q_.append((idx,) + emitS(idx))
                    if len(q_) > 2:
                        emitPV(*q_.pop(0))
                while q_:
                    emitPV(*q_.pop(0))
                kb.op("dve", lambda g_: g_.reciprocal(out=rden[:], in_=ps[1][:]), reads=["ps1"], writes=["brd"])
                y = yst[s % 2]
                yk = "byst%d" % (s % 2)
                kb.op("dve", lambda g_: g_.tensor_tensor(out=y[:], in0=ps[0][:], in1=rden[:], op=ALU.mult),
                      reads=["ps0", "brd"], writes=[yk])
                kb.dma("sp", self.d_yb[:, :, t0:t0 + 128], V(y, 0, [(128, 4), (1, 128)]), yk, reads=[yk], writes=["ybT"])
            kb.barrier()

    def phase_p4(self, l, xsrc, t0, QT, es):
        kb, ps, L = self.kb, self.ps, self.L
        ya = self.sb(es, "p4ya", 8 * QT, BF16)
        yb = self.sb(es, "p4yb", 4 * QT, BF16)
        mg = self.sb(es, "p4mg", 16 * QT, BF16)
        wa = self.sb(es, "p4wa", 8 * D, BF16)
        wb = self.sb(es, "p4wb", 4 * D, BF16)
        gt = [self.sb(es, "p4g%d" % i, 2 * QT, BF16) for i in range(2)]
        m1 = [self.sb(es, "p4m%d" % i, 512, F32) for i in range(2)]
        xo = [self.sb(es, "p4x%d" % i, 512, F32) for i in range(2)]
        kb.dma("sp", V(ya, 0, [(QT, 8), (1, QT)]), self.d_ya[:, :, t0:t0 + QT], "p4y", reads=["yaT"], writes=["p4ya"])
        kb.dma("sp", V(yb, 0, [(QT, 4), (1, QT)]), self.d_yb[:, :, t0:t0 + QT], "p4y", reads=["ybT"], writes=["p4yb"])
        kb.dma("pool", V(wa, 0, [(D, 8), (1, D)]), self.d_waup[l].rearrange("(h p) c -> p h c", p=128), "p4w", writes=["p4wa"])
        kb.dma("pool", V(wb, 0, [(D, 4), (1, D)]), self.d_wbup[l].rearrange("(h p) c -> p h c", p=128), "p4w", writes=["p4wb"])
        NT = QT // 512
        for oc in range(16):
            gb_ = oc % 2
            gk = "p4g%d" % gb_
            kb.dma("sp", V(gt[gb_], 0, [(1, QT)]), self.d_gates[:, oc, t0:t0 + QT], gk, reads=["gates"], writes=[gk])
            kb.dma("sp", V(gt[gb_], QT, [(1, QT)]), self.d_gates[:, 16 + oc, t0:t0 + QT], gk, reads=["gates"], writes=[gk])
            for ti in range(NT):
                for h in range(8):
                    kb.mm(ps[0][:], V(wa, h * D + oc * 128, [(1, 128)]), V(ya, h * QT + ti * 512, [(1, 512)]), h == 0, h == 7,
                          reads=["p4wa", "p4ya"], writes=["ps0"], last=(h == 7))
                for h in range(4):
                    kb.mm(ps[1][:], V(wb, h * D + oc * 128, [(1, 128)]), V(yb, h * QT + ti * 512, [(1, 512)]), h == 0, h == 3,
                          reads=["p4wb", "p4yb"], writes=["ps1"], last=(h == 3))
                kb.op("dve", lambda g: g.tensor_tensor(out=m1[0][:], in0=ps[0][:], in1=V(gt[gb_], ti * 512, [(1, 512)]), op=ALU.mult),
                      reads=["ps0", gk], writes=["p4m0"])
                kb.op("dve", lambda g: g.tensor_tensor(out=m1[1][:], in0=ps[1][:], in1=V(gt[gb_], QT + ti * 512, [(1, 512)]), op=ALU.mult),
                      reads=["ps1", gk], writes=["p4m1"])
                kb.op("pool", lambda g: g.tensor_tensor(out=V(mg, oc * QT + ti * 512, [(1, 512)]), in0=m1[0][:], in1=m1[1][:], op=ALU.add),
                      reads=["p4m0", "p4m1"], writes=["p4mg"])
        n = 0
        for grp in range(4):
            wv_, wk, s = self.load_w(self.d_wout[l][:, grp * 512:(grp + 1) * 512].rearrange("(k p) c -> p k c", p=128),
                                     [(512, 16), (1, 512)])
            for c in range(4):
                oc = grp * 4 + c
                for ti in range(NT):
                    tt = t0 + ti * 512
                    bank = 2 + n % 2
                    xb = n % 2
                    n += 1
                    xk = "p4x%d" % xb
                    kb.dma("sp", xo[xb][:], xsrc[oc, :, tt:tt + 512], xk, reads=["x_t%d" % (tt // 512)], writes=[xk])
                    for k in range(16):
                        kb.mm(ps[bank][:], V(self.wring[s], k * 512 + c * 128, [(1, 128)]), V(mg, k * QT + ti * 512, [(1, 512)]),
                              k == 0, k == 15, reads=[wk, "p4mg"], writes=["ps%d" % bank], last=(k == 15))
                    kb.op("dve", lambda g: g.scalar_tensor_tensor(out=xo[xb][:], in0=ps[bank][:], scalar=V(self.mod[l], 32 + oc, [(1, 1)]),
                                                                  in1=xo[xb][:], op0=ALU.mult, op1=ALU.add),
                          reads=["ps%d" % bank, xk], writes=[xk])
                    kb.dma("sp", self.d_xs[oc, :, tt:tt + 512], xo[xb][:], xk, reads=[xk], writes=["x_t%d" % (tt // 512)])

    def phase_ffn(self, l, t0, QT, hT, es):
        kb, ps, L = self.kb, self.ps, self.L
        act = self.sb(es, "fact", FC * QT, BF16)
        sg = [self.sb(es, "fsg%d" % i, 512, F32) for i in range(2)]
        xo = [self.sb(es, "fx%d" % i, 512, F32) for i in range(2)]
        NT = QT // 512
        n = 0
        for cg in range(DFF // 256):
            s = self.wr_i % 2
            self.wr_i += 1
            wk = "wring%d" % s
            kb.dma("pool", V(self.wring[s], 0, [(256, 16), (1, 256)]),
                   self.d_wfg[l][:, cg * 256:(cg + 1) * 256].rearrange("(k p) c -> p k c", p=128), wk, writes=[wk])
            kb.dma("pool", V(self.wring[s], 4096, [(256, 16), (1, 256)]),
                   self.d_wfu[l][:, cg * 256:(cg + 1) * 256].rearrange("(k p) c -> p k c", p=128), wk, writes=[wk])
            for c in range(2):
                f = cg * 2 + c
                for ti in range(NT):
                    bg, bu = 2 + 2 * (n % 2), 3 + 2 * (n % 2)
                    sgi = n % 2
                    n += 1
                    for k in range(16):
                        kb.mm(ps[bg][:], V(self.wring[s], k * 256 + c * 128, [(1, 128)]), V(hT, k * QT + ti * 512, [(1, 512)]),
                              k == 0, k == 15, reads=[wk, "hT"], writes=["ps%d" % bg], last=(k == 15))
                    for k in range(16):
                        kb.mm(ps[bu][:], V(self.wring[s], 4096 + k * 256 + c * 128, [(1, 128)]), V(hT, k * QT + ti * 512, [(1, 512)]),
                              k == 0, k == 15, reads=[wk, "hT"], writes=["ps%d" % bu], last=(k == 15))
                    kb.op("act", lambda g: g.activation(out=sg[sgi][:], in_=ps[bg][:], func=AF.Silu), reads=["ps%d" % bg],
                          writes=["fsg%d" % sgi])
                    kb.op("dve", lambda g: g.tensor_tensor(out=V(act, f * QT + ti * 512, [(1, 512)]), in0=ps[bu][:], in1=sg[sgi][:],
                                                           op=ALU.mult), reads=["ps%d" % bu, "fsg%d" % sgi], writes=["fact"])
        n = 0
        for oc in range(16):
            s = self.wr_i % 2
            self.wr_i += 1
            wk = "wring%d" % s
            kb.dma("pool", V(self.wring[s], 0, [(128, FC), (1, 128)]),
                   self.d_wfd[l][:, oc * 128:(oc + 1) * 128].rearrange("(f p) c -> p f c", p=128), wk, writes=[wk])
            for ti in range(NT):
                tt = t0 + ti * 512
                bank = n % 2
                xb = n % 2
                n += 1
                xk = "fx%d" % xb
                kb.dma("sp", xo[xb][:], self.d_xs[oc, :, tt:tt + 512], xk, reads=["x_t%d" % (tt // 512)], writes=[xk])
                for f in range(FC):
                    kb.mm(ps[bank][:], V(self.wring[s], f * 128, [(1, 128)]), V(act, f * QT + ti * 512, [(1, 512)]), f == 0,
                          f == FC - 1, reads=[wk, "fact"], writes=["ps%d" % bank], last=(f == FC - 1))
                kb.op("dve", lambda g: g.scalar_tensor_tensor(out=xo[xb][:], in0=ps[bank][:], scalar=V(self.mod[l], 80 + oc, [(1, 1)]),
                                                              in1=xo[xb][:], op0=ALU.mult, op1=ALU.add),
                      reads=["ps%d" % bank, xk], writes=[xk])
                kb.dma("sp", self.d_xs[oc, :, tt:tt + 512], xo[xb][:], xk, reads=[xk], writes=["x_t%d" % (tt // 512)])

    def build(self):
        kb, L, QT = self.kb, self.L, self.QT
        self.epsb = self.nc.alloc_sbuf_tensor("epsb", [128, 1], F32)
        kb.op("pool", lambda g: g.memset(self.epsb[:], EPS), writes=["epsb"])
        self.phase_const()
        self.phase_mod()
        ph = self.phases
        for l in range(self.NL):
            xsrc = self.d_xT if l == 0 else self.d_xs
            if ph is None or "p1" in ph:
                with ExitStack() as es:
                    QT1 = L
                    hT = self.sb(es, "hT", 16 * QT1, BF16)
                    with ExitStack() as es2:
                        self.norm_tiles(es2, xsrc, self.A1[l], (self.mod[l], 0), 0, QT1, hT)
                        kb.barrier()
                    with ExitStack() as es2:
                        self.phase_p1(l, 0, QT1, hT, es2, "k")
                        kb.barrier()
                    if self.NR > 1:
                        rg = [[c * self.NR + i for i in range(self.NR)] for c in range(8 // self.NR)]
                        for ci, (loc, full) in enumerate([self.kA[l], self.kC[l]] + self.kK[l] + self.kV[l]):
                            kb.cc(lambda g: g.collective_compute("AllGather", ALU.bypass, replica_groups=rg, ins=[loc], outs=[full]),
                                  "ag%d_%d" % (l, ci), reads=["kloc"], writes=["kfull"])
                    with ExitStack() as es2:
                        self.phase_p1(l, 0, QT1, hT, es2, "q")
                        kb.barrier()
            if ph is None or "atta" in ph:
                self.phase_att_a(l)
            if ph is None or "attb" in ph:
                self.phase_att_b(l)
            if ph is None or "p4" in ph:
                for q in range(L // QT):
                    with ExitStack() as es:
                        self.phase_p4(l, xsrc, q * QT, QT, es)
                        kb.barrier()
                    with ExitStack() as es:
                        hT = self.sb(es, "hT", 16 * QT, BF16)
                        with ExitStack() as es2:
                            self.norm_tiles(es2, self.d_xs, self.A2[l], (self.mod[l], 48), q * QT, QT, hT)
                            kb.barrier()
                        self.phase_ffn(l, q * QT, QT, hT, es)
                        kb.barrier()
        toks = []
        with ExitStack() as es:
            toks = self.norm_tiles(es, self.d_xs, self.fg, None, 0, L, None, final=True)
        for t in toks:
            kb._wait("sp", t)
        for nm in self.dbg:
            pass
        kb.barrier()
        return self.nc


def _rel_bucket(n):
    n = np.maximum(n, 0)
    nf = np.maximum(n, 1).astype(np.float32)
    large = 16 + (np.log(nf / np.float32(16)) / np.float32(math.log(2048 / 16)) * np.float32(16)).astype(np.int32)
    return np.where(n < 16, n, np.minimum(large, 31))


def _static_tables(LG, NR, r, rel_bias):
    L = LG // NR
    NB = L // 128
    k = np.arange(128)[:, None]
    q = np.arange(128)[None, :]
    negf = np.float32(NEG)
    sh = NR - 1 - r
    NTA = 17 + NR
    bta = np.full((128, NTA, 8, 128), negf, np.float32)
    for dl in range(NTA):
        d = dl - sh
        if d < 0:
            continue
        dist = min(d, 17) * 128 + q - k
        val = rel_bias[_rel_bucket(dist)][:, :, :8]
        val = np.where((dist >= 0)[:, :, None], val, negf)
        bta[:, dl] = np.transpose(val, (0, 2, 1))
    NTB = [n + NR - 1 for n in NBLK_G]
    btb = np.full((128, sum(NTB), 4, 128), negf, np.float32)
    off = 0
    for g, (win, dil) in enumerate(B_GROUPS):
        for dl in range(NTB[g]):
            d = dl - sh
            if 0 <= d < NBLK_G[g]:
                dist = d * 128 + q - k
                ok = (dist >= 0) & (dist <= win) & (dist % dil == 0)
                val = rel_bias[_rel_bucket(dist)][:, :, 8 + g * 4: 8 + g * 4 + 4]
                val = np.where(ok[:, :, None], val, negf)
                btb[:, off + dl] = np.transpose(val, (0, 2, 1))
        off += NTB[g]
    cm = np.where(np.arange(128)[None, :] <= np.arange(128)[:, None], np.float32(0), np.float32(-1e30)).astype(np.float32)
    cmr = np.zeros((128, NR, 128), np.float32)
    for rk in range(NR):
        if rk == r:
            cmr[:, rk] = cm
        elif rk > r:
            cmr[:, rk] = np.float32(-1e30)
    blocks = np.array([NR * s + r for s in range(NB)])
    pos = (blocks[:, None] * 128 + np.arange(128)[None, :]).astype(np.float32)
    freqs = (np.float32(10000.0) ** (-np.arange(16, dtype=np.float32) / np.float32(16))).astype(np.float32)
    ang = pos[:, :, None] * freqs[None, None, :]
    ropeC = np.cos(ang).astype(np.float32).transpose(1, 0, 2)
    ropeS = np.sin(ang).astype(np.float32).transpose(1, 0, 2)
    return dict(bta=np.ascontiguousarray(bta.reshape(128, -1)), btb=np.ascontiguousarray(btb.reshape(128, -1)),
                cmr=np.ascontiguousarray(cmr.reshape(128, -1)), ropeC=np.ascontiguousarray(ropeC),
                ropeS=np.ascontiguousarray(ropeS), ident=np.eye(128, dtype=np.float32))


def _pk(v):
    v = np.asarray(v, np.float32)
    return np.ascontiguousarray(np.swapaxes(v.reshape(v.shape[:-1] + (-1, 128)), -1, -2))


def make_in_maps(inp, LG, NR, cores):
    f = lambda a: np.ascontiguousarray(np.asarray(a, np.float32))
    shared = dict(
        w_ada=f(inp["w_ada"]), b_adaT=_pk(inp["b_ada"]), n1g=_pk(inp["norm1_g"]), n2g=_pk(inp["norm2_g"]), fg=_pk(inp["final_g"]),
        w_in=f(inp["w_in"]),
        kvg=np.ascontiguousarray(np.broadcast_to(f(inp["kv_norm_g"])[:, None, :], (len(inp["kv_norm_g"]), 128, 256))),
        ilg=np.ascontiguousarray(np.broadcast_to(f(inp["idx_ln_g"])[:, None, :], (len(inp["idx_ln_g"]), 128, 64))),
        ilb=np.ascontiguousarray(np.broadcast_to(f(inp["idx_ln_b"])[:, None, :], (len(inp["idx_ln_b"]), 128, 64))),
        w_uk=f(inp["w_uk"]), w_uv=f(inp["w_uv"]), w_a_up=f(inp["w_a_up"]), w_b_up=f(inp["w_b_up"]), w_out=f(inp["w_out"]),
        w_ffg=f(inp["w_ff_gate"]), w_ffu=f(inp["w_ff_up"]), w_ffd=f(inp["w_ff_down"]),
    )
    tabs = [_static_tables(LG, NR, r, f(inp["rel_bias"])) for r in range(NR)]
    x = f(inp["x"])
    c = f(inp["c"])
    L = LG // NR
    maps = []
    for b, r in cores:
        m = dict(shared)
        m.update(tabs[r])
        xb = x[b].reshape(LG // 128, 128, D)[r::NR].reshape(L, D)
        m["xT"] = np.ascontiguousarray(xb.T.reshape(KC, 128, L))
        m["c_pk"] = _pk(c[b])
        maps.append(m)
    return maps


_CACHE = {}
NR_FULL = 2


def kernel(**inputs):
    x = np.asarray(inputs["x"])
    B, LG, _ = x.shape
    nl = np.asarray(inputs["w_ada"]).shape[0]
    NR = NR_FULL
    key = (LG, nl, NR)
    if key not in _CACHE:
        _CACHE[key] = Prog(LG, nl, NR=NR).build()
    nc = _CACHE[key]
    cores = [((c // NR) % B, c % NR) for c in range(8)]
    in_maps = make_in_maps(inputs, LG, NR, cores)
    res = run_bass_kernel_spmd(nc, in_maps, core_ids=list(range(8)))
    L = LG // NR
    out = np.empty((B, LG // 128, 128, D), np.float32)
    for c, (b, r) in enumerate(cores):
        if c // NR >= B:
            continue
        o = np.asarray(res.results[c]["outT"]).reshape(D, L).T
        out[b, r::NR] = o.reshape(L // 128, 128, D)
    return out.reshape(B, LG, D)
```
